# Optimizing a Trainium2 kernel written in Bass

```python
import jax, jax.numpy as jnp
from jax import lax
import numpy as np

D_MODEL = 1024
BATCH = 8
SEQ = 8192
DEPTH = 2

GRID_W = 64
NORM_EPS = 1e-6
RWKV_HEADS = 8
RWKV_HEAD = 64
RWKV_W = RWKV_HEADS * RWKV_HEAD
DECAY_LORA = 64
ICLR_LORA = 64
GATE_LORA = 128
VRES_LORA = 32
RWKV_LN_EPS = 64e-5
N_DIR = 2
RWKV_COLS = 3 * RWKV_W + N_DIR * DECAY_LORA + N_DIR * ICLR_LORA + GATE_LORA
POOL_WINDOWS = (2, 4, 8, 16)
POOL_GROUPS = len(POOL_WINDOWS)
POOL_GROUP_W = 128
POOL_W = POOL_GROUPS * POOL_GROUP_W
ATT_HEADS = 8
ATT_KV_HEADS = 2
ATT_GROUP = ATT_HEADS // ATT_KV_HEADS
ATT_HEAD = 64
ATT_W = ATT_HEADS * ATT_HEAD
ATT_COLS = (ATT_HEADS + 2 * ATT_KV_HEADS) * ATT_HEAD
ROPE_AXIS_DIM = ATT_HEAD // 2
ROPE_THETA = 10000.0
Q_BLOCK = 128
N_BRANCH = 3
GATE_COLS = N_BRANCH * D_MODEL
N_PROJ = RWKV_COLS + POOL_W + ATT_COLS + GATE_COLS
N_EXPERTS = 16
EXPERT_FF = 1024
CAPACITY_FACTOR = 2

kernel_name = 'hybrid_rwkv7_pool_gqa_ecmoe_encoder'


def rms_norm(x, g, eps=NORM_EPS):
    xf = x.astype(jnp.float32)
    y = xf * lax.rsqrt(jnp.mean(xf * xf, axis=-1, keepdims=True) + eps)
    return (y * g.astype(jnp.float32)).astype(x.dtype)


def centred_token_shift(p, mu_prev, mu_next):
    zero = jnp.zeros_like(p[:, :1])
    p_prev = jnp.concatenate([zero, p[:, :-1]], axis=1)
    p_next = jnp.concatenate([p[:, 1:], zero], axis=1)
    return p + mu_prev * (p_prev - p) + mu_next * (p_next - p)


def _heads(z):
    return z.reshape(z.shape[:-1] + (RWKV_HEADS, RWKV_HEAD))


def rwkv7_bidir(p, hn, vres, v_first, w0, w2, a0, a2, g2, k_k, k_a, r_k, ln_w, ln_b):
    B, S, _ = p.shape
    f32 = jnp.float32
    dt = p.dtype
    pf = p.astype(f32)
    offs = np.cumsum([RWKV_W, RWKV_W, RWKV_W, N_DIR * DECAY_LORA, N_DIR * ICLR_LORA]).tolist()
    r, k, v, wd, ad, gd = jnp.split(pf, offs, axis=-1)
    wd = wd.reshape(B, S, N_DIR, DECAY_LORA)
    ad = ad.reshape(B, S, N_DIR, ICLR_LORA)
    w_raw = w0.astype(f32) + jnp.einsum('bsel,elc->bsec', jnp.tanh(wd), w2.astype(f32))
    decay = jnp.exp(-jnp.exp(-jax.nn.softplus(-w_raw) - 0.5))
    a = jax.nn.sigmoid(a0.astype(f32) + jnp.einsum('bsel,elc->bsec', ad, a2.astype(f32)))
    g = jnp.einsum('bsl,lc->bsc', jax.nn.sigmoid(gd), g2.astype(f32))
    if v_first is None:
        v_first = v
    else:
        v0, v1, v2 = vres
        mix = jax.nn.sigmoid(v0.astype(f32) + jnp.einsum('bsd,dl,lc->bsc', hn.astype(f32), v1.astype(f32), v2.astype(f32)))
        v = v + (v_first - v) * mix
    kk = _heads(k * k_k.astype(f32))
    kk = kk / jnp.maximum(jnp.linalg.norm(kk, axis=-1, keepdims=True), 1e-12)
    k_dir = k[:, :, None, :] * (1.0 + (a - 1.0) * k_a.astype(f32))

    def both(z):
        return jnp.stack([z, jnp.flip(z, 1)], axis=2)

    def per_dir(z):
        return jnp.stack([z[:, :, 0], jnp.flip(z[:, :, 1], 1)], axis=2)

    def time_major(z):
        return _heads(z).transpose(1, 2, 0, 3, 4)

    inputs = (time_major(both(r)), time_major(per_dir(decay)), time_major(per_dir(k_dir)),
              time_major(both(v)), both(kk).transpose(1, 2, 0, 3, 4), time_major(per_dir(a)))

    def step(state, inp):
        r_t, w_t, k_t, v_t, kk_t, a_t = inp
        sa = jnp.einsum('dbhij,dbhj->dbhi', state, -kk_t)
        state = (state * w_t[..., None, :] + sa[..., :, None] * (kk_t * a_t)[..., None, :]
                 + v_t[..., :, None] * k_t[..., None, :])
        return state, jnp.einsum('dbhij,dbhj->dbhi', state, r_t)

    state0 = jnp.zeros((N_DIR, B, RWKV_HEADS, RWKV_HEAD, RWKV_HEAD), f32)
    _, ys = lax.scan(step, state0, inputs)
    y = (ys[:, 0] + jnp.flip(ys[:, 1], 0)).transpose(1, 0, 2, 3)
    mu = jnp.mean(y, axis=-1, keepdims=True)
    var = jnp.mean(jnp.square(y - mu), axis=-1, keepdims=True)
    yn = (y - mu) * lax.rsqrt(var + RWKV_LN_EPS) * _heads(ln_w.astype(f32)) + _heads(ln_b.astype(f32))
    bonus = jnp.sum(_heads(r) * _heads(k_dir.sum(axis=2)) * _heads(r_k.astype(f32)), axis=-1, keepdims=True) * _heads(v)
    out = (yn + bonus).reshape(B, S, RWKV_W) * g
    return out.astype(dt), v_first


def multiscale_pool(z, pool_w, pool_scale):
    B, S, _ = z.shape
    zg = z.astype(jnp.float32).reshape(B, S, POOL_GROUPS, POOL_GROUP_W)
    csum = jnp.concatenate([jnp.zeros((B, 1, POOL_GROUPS, POOL_GROUP_W), jnp.float32), jnp.cumsum(zg, axis=1)], axis=1)
    t = jnp.arange(S)
    outs = []
    for gi, w in enumerate(POOL_WINDOWS):
        lo = w // 2
        hi = w - lo - 1
        start = jnp.clip(t - lo, 0, S)
        end = jnp.clip(t + hi + 1, 0, S)
        cnt = (end - start).astype(jnp.float32)
        outs.append((csum[:, end, gi] - csum[:, start, gi]) / cnt[None, :, None] - zg[:, :, gi])
    pooled = jnp.stack(outs, axis=2)
    y = jnp.einsum('bsgc,gcd->bsgd', pooled, pool_w.astype(jnp.float32)).reshape(B, S, POOL_W)
    return (y * pool_scale.astype(jnp.float32)).astype(z.dtype)


def axial_rope(seq):
    rows = seq // GRID_W
    row = jnp.repeat(jnp.arange(rows, dtype=jnp.float32), GRID_W)
    col = jnp.tile(jnp.arange(GRID_W, dtype=jnp.float32), rows)
    freqs = ROPE_THETA ** (-jnp.arange(0, ROPE_AXIS_DIM, 2, dtype=jnp.float32) / ROPE_AXIS_DIM)
    ang = jnp.concatenate([row[:, None] * freqs, col[:, None] * freqs], axis=-1)
    return jnp.cos(ang), jnp.sin(ang)


def apply_rope(x, cos, sin):
    xp = x.reshape(x.shape[:-1] + (ATT_HEAD // 2, 2))
    x0, x1 = xp[..., 0], xp[..., 1]
    c = cos[None, :, None, :]
    s = sin[None, :, None, :]
    return jnp.stack([x0 * c - x1 * s, x0 * s + x1 * c], axis=-1).reshape(x.shape)


def gqa_axial(p_att, q_norm, k_norm):
    B, S, _ = p_att.shape
    dt = p_att.dtype
    q, k, v = jnp.split(p_att, [ATT_W, ATT_W + ATT_KV_HEADS * ATT_HEAD], axis=-1)
    q = rms_norm(q.reshape(B, S, ATT_HEADS, ATT_HEAD), q_norm).astype(jnp.float32)
    k = rms_norm(k.reshape(B, S, ATT_KV_HEADS, ATT_HEAD), k_norm).astype(jnp.float32)
    cos, sin = axial_rope(S)
    q = (apply_rope(q, cos, sin) * (ATT_HEAD ** -0.5)).astype(dt)
    k = apply_rope(k, cos, sin).astype(dt)
    v = v.reshape(B, S, ATT_KV_HEADS, ATT_HEAD)
    n_blocks = S // Q_BLOCK
    q_blocks = q.reshape(B, n_blocks, Q_BLOCK, ATT_KV_HEADS, ATT_GROUP, ATT_HEAD).transpose(1, 0, 3, 4, 2, 5)
    k_t = k.transpose(0, 2, 1, 3)
    v_t = v.transpose(0, 2, 1, 3)

    def block(qb):
        s = jnp.einsum('bkgqd,bksd->bkgqs', qb, k_t).astype(jnp.float32)
        pr = jax.nn.softmax(s, axis=-1).astype(dt)
        return jnp.einsum('bkgqs,bksd->bkgqd', pr, v_t)

    o = lax.map(block, q_blocks)
    return o.transpose(1, 0, 4, 2, 3, 5).reshape(B, S, ATT_W)


def expert_choice_ffn(h, router, w_gate, w_up, w_down):
    B, S, D = h.shape
    cap = CAPACITY_FACTOR * S // N_EXPERTS
    aff = jax.nn.softmax(jnp.einsum('bsd,de->bse', h, router).astype(jnp.float32), axis=-1)
    vals, idx = lax.top_k(aff.transpose(0, 2, 1), cap)
    xg = jax.vmap(lambda hb, ib: hb[ib])(h, idx)
    hid = jax.nn.silu(jnp.einsum('becd,edf->becf', xg, w_gate)) * jnp.einsum('becd,edf->becf', xg, w_up)
    y = jnp.einsum('becf,efd->becd', hid, w_down) * vals[..., None].astype(h.dtype)
    return jax.vmap(lambda ib, yb: jnp.zeros((S, D), yb.dtype).at[ib.reshape(-1)].add(yb.reshape(-1, D)))(idx, y)


def setup_inputs(seed: int = 0) -> dict:
    key = jax.random.key(seed)
    ks = iter(jax.random.split(key, 40))

    def nrm(shape, scale):
        return jax.random.normal(next(ks), shape, jnp.float32) * scale

    def uni(shape, lo, hi):
        return jax.random.uniform(next(ks), shape, jnp.float32, lo, hi)

    L = DEPTH
    Lv = DEPTH - 1
    return {
        'x': nrm((BATCH, SEQ, D_MODEL), 1.0),
        'norm_mix': 1.0 + nrm((L, D_MODEL), 0.05),
        'w_in': nrm((L, D_MODEL, N_PROJ), D_MODEL ** -0.5),
        'shift_prev': uni((L, RWKV_COLS), 0.0, 0.5),
        'shift_next': uni((L, RWKV_COLS), 0.0, 0.5),
        'rwkv_w0': uni((L, N_DIR, RWKV_W), -5.0, 1.0),
        'rwkv_w2': nrm((L, N_DIR, DECAY_LORA, RWKV_W), 0.5 * DECAY_LORA ** -0.5),
        'rwkv_a0': nrm((L, N_DIR, RWKV_W), 0.5),
        'rwkv_a2': nrm((L, N_DIR, ICLR_LORA, RWKV_W), ICLR_LORA ** -0.5),
        'rwkv_g2': nrm((L, GATE_LORA, RWKV_W), GATE_LORA ** -0.5),
        'rwkv_k_k': 0.85 + nrm((L, RWKV_W), 0.05),
        'rwkv_k_a': 1.0 + nrm((L, RWKV_W), 0.05),
        'rwkv_r_k': nrm((L, RWKV_W), 0.1),
        'rwkv_ln_w': 1.0 + nrm((L, RWKV_W), 0.05),
        'rwkv_ln_b': nrm((L, RWKV_W), 0.02),
        'vres_v0': 0.5 + nrm((Lv, RWKV_W), 0.3),
        'vres_w1': nrm((Lv, D_MODEL, VRES_LORA), D_MODEL ** -0.5),
        'vres_w2': nrm((Lv, VRES_LORA, RWKV_W), VRES_LORA ** -0.5),
        'pool_w': nrm((L, POOL_GROUPS, POOL_GROUP_W, POOL_GROUP_W), POOL_GROUP_W ** -0.5),
        'pool_scale': 1.0 + nrm((L, POOL_W), 0.1),
        'q_norm': 1.0 + nrm((L, ATT_HEAD), 0.05),
        'k_norm': 1.0 + nrm((L, ATT_HEAD), 0.05),
        'w_branch_rwkv': nrm((L, RWKV_W, D_MODEL), RWKV_W ** -0.5),
        'w_branch_pool': nrm((L, POOL_W, D_MODEL), POOL_W ** -0.5),
        'w_branch_attn': nrm((L, ATT_W, D_MODEL), ATT_W ** -0.5),
        'w_out': nrm((L, D_MODEL, D_MODEL), D_MODEL ** -0.5),
        'norm_ffn': 1.0 + nrm((L, D_MODEL), 0.05),
        'router': nrm((L, D_MODEL, N_EXPERTS), D_MODEL ** -0.5),
        'exp_gate': nrm((L, N_EXPERTS, D_MODEL, EXPERT_FF), D_MODEL ** -0.5),
        'exp_up': nrm((L, N_EXPERTS, D_MODEL, EXPERT_FF), D_MODEL ** -0.5),
        'exp_down': nrm((L, N_EXPERTS, EXPERT_FF, D_MODEL), EXPERT_FF ** -0.5),
        'norm_final': 1.0 + nrm((D_MODEL,), 0.05),
    }


def reference(x, norm_mix, w_in, shift_prev, shift_next, rwkv_w0, rwkv_w2, rwkv_a0, rwkv_a2, rwkv_g2,
              rwkv_k_k, rwkv_k_a, rwkv_r_k, rwkv_ln_w, rwkv_ln_b, vres_v0, vres_w1, vres_w2,
              pool_w, pool_scale, q_norm, k_norm, w_branch_rwkv, w_branch_pool, w_branch_attn, w_out,
              norm_ffn, router, exp_gate, exp_up, exp_down, norm_final):
    B, S, D = x.shape
    split_at = [RWKV_COLS, RWKV_COLS + POOL_W, RWKV_COLS + POOL_W + ATT_COLS]
    v_first = None
    for l in range(DEPTH):
        hn = rms_norm(x, norm_mix[l])
        proj = jnp.einsum('bsd,dn->bsn', hn, w_in[l])
        p_rwkv, p_pool, p_att, p_gate = jnp.split(proj, split_at, axis=-1)
        p_rwkv = centred_token_shift(p_rwkv, shift_prev[l], shift_next[l])
        vres = None if l == 0 else (vres_v0[l - 1], vres_w1[l - 1], vres_w2[l - 1])
        y_a, v_first = rwkv7_bidir(p_rwkv, hn, vres, v_first, rwkv_w0[l], rwkv_w2[l], rwkv_a0[l], rwkv_a2[l],
                                   rwkv_g2[l], rwkv_k_k[l], rwkv_k_a[l], rwkv_r_k[l], rwkv_ln_w[l], rwkv_ln_b[l])
        y_b = multiscale_pool(p_pool, pool_w[l], pool_scale[l])
        y_c = gqa_axial(p_att, q_norm[l], k_norm[l])
        gates = jax.nn.sigmoid(p_gate).reshape(B, S, N_BRANCH, D)
        merged = (gates[:, :, 0] * (y_a @ w_branch_rwkv[l]) + gates[:, :, 1] * (y_b @ w_branch_pool[l])
                  + gates[:, :, 2] * (y_c @ w_branch_attn[l]))
        x = x + merged @ w_out[l]
        x = x + expert_choice_ffn(rms_norm(x, norm_ffn[l]), router[l], exp_gate[l], exp_up[l], exp_down[l])
    return rms_norm(x, norm_final)
```

```python
import numpy as np
import ml_dtypes
import concourse.bass as bass
import concourse.mybir as mybir
from concourse.bass_utils import run_bass_kernel_spmd

F32 = mybir.dt.float32
BF16 = mybir.dt.bfloat16
I32 = mybir.dt.int32
U32 = mybir.dt.uint32
AF = mybir.ActivationFunctionType
ALU = mybir.AluOpType
AX = mybir.AxisListType

D = 1024
S = 8192
NT = S // 128
NG = S // 512
NPROJ = 6272
RW = 512
EPS = 1e-6

PE, ACT, DVE, POOL, SP = "pe", "act", "dve", "pool", "sp"
ENGS = (PE, ACT, DVE, POOL, SP)
NDMASEM = {"pe": 1, "act": 6, "dve": 1, "pool": 24, "sp": 8}
NRING2 = 6


class Buf:
    __slots__ = ("name", "w", "rd")

    def __init__(self, name=""):
        self.name = name
        self.w = None
        self.rd = []


class Op:
    __slots__ = ("eng", "fn", "dma", "deps", "needs_inc", "count", "slot", "slotcount", "idx")


class Prog:
    def __init__(self, nc):
        self.nc = nc
        self.ops = {e: [] for e in ENGS}
        self.dma_rr = {e: 0 for e in ENGS}
        self.dma_rr2 = {}
        self.dma_last = {}
        self.dma_cnt = {}
        self.nops = 0
        self.pending = {e: [] for e in ENGS}

    def barrier(self):
        deps = []
        for e in ENGS:
            for op in reversed(self.ops[e]):
                if not op.dma:
                    deps.append(op)
                    break
        deps.extend(self.dma_last.values())
        for e in ENGS:
            self.pending[e] = list(deps)

    def add(self, eng, fn, reads=(), writes=(), dma=False, bulk=False):
        op = Op()
        op.eng, op.fn, op.dma = eng, fn, dma
        op.needs_inc = False
        op.count = 0
        op.idx = self.nops
        self.nops += 1
        deps = []
        for b in reads:
            if b.w is not None:
                for w_ in b.w:
                    deps.append((w_, "raw"))
        for b in writes:
            if b.w is not None:
                for w_ in b.w:
                    deps.append((w_, "waw"))
            for r in b.rd:
                deps.append((r, "war"))
        for d in self.pending[eng]:
            deps.append((d, "bar"))
        self.pending[eng] = []
        if dma:
            if bulk:
                k2 = self.dma_rr2.get(eng, 0)
                self.dma_rr2[eng] = (k2 + 1) % NRING2
                k = NDMASEM[eng] + k2
            else:
                k = self.dma_rr[eng]
                self.dma_rr[eng] = (k + 1) % NDMASEM[eng]
            op.slot = k
            prev = self.dma_last.get((eng, k))
            if prev is not None:
                deps.append((prev, "slot"))
            self.dma_last[(eng, k)] = op
            c = self.dma_cnt.get((eng, k), 0) + 16
            self.dma_cnt[(eng, k)] = c
            op.slotcount = c
        fin = []
        for d, kind in deps:
            if d is op:
                continue
            if not d.dma and d.eng == eng:
                if eng == PE:
                    continue
                if kind == "bar" and not dma:
                    continue
            if not d.dma:
                d.needs_inc = True
            fin.append(d)
        op.deps = fin
        for b in reads:
            b.rd.append(op)
        for b in writes:
            if dma and b.w is not None and all(w_.dma for w_ in b.w):
                keep = [w_ for w_ in b.w if not (w_.eng == eng and w_.slot == op.slot)]
                b.w = keep + [op]
            else:
                b.w = [op]
            b.rd = []
        self.ops[eng].append(op)
        return op

    def emit(self, sems, dsems, engs):
        for e in ENGS:
            c = 0
            for op in self.ops[e]:
                if not op.dma and op.needs_inc:
                    c += 1
                    op.count = c
        for e in ENGS:
            eng = engs[e]
            waited = {}
            for op in self.ops[e]:
                need = {}
                for d in op.deps:
                    if d.dma:
                        key = ("d", d.eng, d.slot)
                        v = d.slotcount
                    else:
                        key = ("c", d.eng)
                        v = d.count
                    if need.get(key, 0) < v:
                        need[key] = v
                for key, v in need.items():
                    if waited.get(key, 0) >= v:
                        continue
                    waited[key] = v
                    sem = dsems[key[1]][key[2]] if key[0] == "d" else sems[key[1]]
                    eng.wait_ge(sem, v)
                ins = op.fn(eng)
                if op.dma:
                    ins.then_inc(dsems[e][op.slot], 16)
                elif op.needs_inc:
                    ins.then_inc(sems[e], 1)

    def final_wait(self, sems, dsems, engs):
        eng = engs[SP]
        for (e, k), c in self.dma_cnt.items():
            eng.wait_ge(dsems[e][k], c)


class Ctx:
    def __init__(self, nc):
        self.nc = nc
        self.P = Prog(nc)
        self.stack = None
        self.uid = 0

    def name(self, base):
        self.uid += 1
        return f"{base}_{self.uid}"

    def sb(self, st, shape, dt, name="t"):
        t = st.enter_context(self.nc.sbuf_tensor(self.name(name), list(shape), dt))
        return t, Buf(name)

    def ps(self, st, shape, dt, name="p"):
        t = st.enter_context(self.nc.psum_tensor(self.name(name), list(shape), dt))
        return t, Buf(name)

    def dram(self, name, shape, dt, kind="Internal"):
        return self.nc.dram_tensor(name, list(shape), dt, kind=kind).ap(), Buf(name)

    def dma(self, eng, out, in_, reads, writes, bulk=False, **kw):
        def fn(e):
            return e.dma_start(out=out, in_=in_, **kw)
        return self.P.add(eng, fn, reads, writes, dma=True, bulk=bulk and eng == POOL)

    def op(self, eng, fn, reads, writes):
        return self.P.add(eng, fn, reads, writes)


from contextlib import ExitStack


def act(func, out, in_, **kw):
    return lambda e: e.activation(out=out, in_=in_, func=func, **kw)


def stage_proj(C, l, x_tok, w_in_d, norm_d, outs, vres_w1_d=None):
    nc = C.nc
    ncols = NPROJ + (32 if vres_w1_d is not None else 0)
    nchunk = (ncols + 127) // 128
    with ExitStack() as st:
        w_sb, w_b = C.sb(st, [128, 8, ncols], BF16, "w_in")
        gb, gb_b = C.sb(st, [128, D], F32, "gbc")
        C.dma(SP, gb[:], norm_d[0].partition_broadcast(128), [norm_d[1]], [gb_b])
        WP = 784
        stg = [C.sb(st, [128, 8, WP], F32, "wstg") for _ in range(2)]
        wv = w_in_d[0].rearrange("(kc p) n -> p kc n", p=128)
        k = 0
        for c0 in range(0, NPROJ, WP):
            t, tb = stg[k % 2]
            C.dma(ACT if k % 2 else SP, t[:], wv[:, :, c0:c0 + WP], [w_in_d[1]], [tb])
            C.op(POOL, lambda e, t=t, c0=c0: e.tensor_copy(out=w_sb[:, :, c0:c0 + WP], in_=t[:]), [tb], [w_b])
            k += 1
        if vres_w1_d is not None:
            t, tb = stg[k % 2]
            C.dma(SP, t[:, :, 0:32], vres_w1_d[0].rearrange("(kc p) n -> p kc n", p=128), [vres_w1_d[1]], [tb])
            C.op(POOL, lambda e, t=t: e.tensor_copy(out=w_sb[:, :, NPROJ:NPROJ + 32], in_=t[:, :, 0:32]), [tb], [w_b])
        ident, ident_b = C.sb(st, [128, 128], BF16, "ident")
        C.op(POOL, lambda e: e.memset(ident[:], 0.0), [], [ident_b])
        C.op(POOL, lambda e: e.affine_select(out=ident[:], in_=ident[:], pattern=[[-1, 128]], compare_op=ALU.not_equal,
                                             fill=1.0, base=0, channel_multiplier=1), [ident_b], [ident_b])
        xts = [C.sb(st, [128, D], F32, "xt") for _ in range(2)]
        hns = [C.sb(st, [128, D], BF16, "hn") for _ in range(2)]
        sq, sq_b = C.sb(st, [128, D], BF16, "sqj")
        sts = [C.sb(st, [128, 2], F32, "stat") for _ in range(2)]
        hnT = [C.sb(st, [128, 8, 512], BF16, "hnT") for _ in range(2)]
        tps = [C.ps(st, [128, 8, 128], BF16, "tps") for _ in range(2)]
        mps = [C.ps(st, [128, 512], F32, "mps") for _ in range(4)]
        ost = [C.sb(st, [128, 512], F32, "ost") for _ in range(4)]
        osb = [C.sb(st, [128, 512], BF16, "osb") for _ in range(4)]
        it = 0
        oc = 0
        for g in range(NG):
            hT, hT_b = hnT[g % 2]
            for s in range(4):
                xt, xt_b = xts[it % 2]
                hn, hn_b = hns[it % 2]
                stt, st_b = sts[it % 2]
                tp, tp_b = tps[it % 2]
                it += 1
                r0 = g * 512 + s * 128
                C.dma(SP, xt[:], x_tok[0][r0:r0 + 128, :], [x_tok[1]], [xt_b])
                C.op(ACT, act(AF.Square, sq[:], xt[:], accum_out=stt[:, 0:1]), [xt_b], [sq_b, st_b])
                C.op(ACT, act(AF.Sqrt, stt[:, 1:2], stt[:, 0:1], scale=1.0 / D, bias=EPS), [st_b], [st_b])
                C.op(DVE, lambda e, stt=stt: e.reciprocal(out=stt[:, 1:2], in_=stt[:, 1:2]), [st_b], [st_b])
                C.op(DVE, lambda e, stt=stt, xt=xt, hn=hn: e.scalar_tensor_tensor(
                    out=hn[:], in0=xt[:], scalar=stt[:, 1:2], in1=gb[:], op0=ALU.mult, op1=ALU.mult),
                    [st_b, xt_b, gb_b], [hn_b])
                for kc in range(8):
                    C.op(PE, lambda e, tp=tp, hn=hn, kc=kc: e.transpose(out=tp[:, kc, :], in_=hn[:, kc * 128:(kc + 1) * 128],
                                                                       identity=ident[:]), [hn_b, ident_b], [tp_b])
                C.op(ACT, lambda e, tp=tp, hT=hT, s=s: e.copy(out=hT[:, :, s * 128:(s + 1) * 128], in_=tp[:]), [tp_b], [hT_b])
            for c in range(nchunk):
                m = min(128, ncols - c * 128)
                mp, mp_b = mps[oc % 4]
                for kc in range(8):
                    C.op(PE, lambda e, mp=mp, c=c, kc=kc, hT=hT, m=m: e.matmul(
                        out=mp[0:m, :], lhsT=w_sb[:, kc, c * 128:c * 128 + m], rhs=hT[:, kc, :],
                        start=(kc == 0), stop=(kc == 7)), [w_b, hT_b], [mp_b])
                col = c * 128
                tsl = slice(g * 512, (g + 1) * 512)
                if col < 1920:
                    dst, off = outs["prw"], col
                elif col < 2432:
                    dst, off = outs["ppool"], col - 1920
                elif col < 3200:
                    dst, off = outs["patt"], col - 2432
                elif col < NPROJ:
                    dst, off = outs["pgate"], col - 3200
                else:
                    dst, off = outs["hv1"], 0
                if dst is outs["pgate"]:
                    o, o_b = osb[oc % 4]
                    C.op(ACT, act(AF.Sigmoid, o[:], mp[:]), [mp_b], [o_b])
                else:
                    o, o_b = ost[oc % 4]
                    C.op(DVE, lambda e, o=o, mp=mp, m=m: e.tensor_copy(out=o[0:m, :], in_=mp[0:m, :]), [mp_b], [o_b])
                C.dma(SP if oc % 2 else POOL, dst[0][off:off + m, tsl], o[0:m, :], [o_b], [dst[1]])
                oc += 1


def emit_program(C):
    nc = C.nc
    with ExitStack() as st:
        sems = {e: st.enter_context(nc.semaphore(f"s_{e}")) for e in ENGS}
        dsems = {e: [st.enter_context(nc.semaphore(f"d_{e}{k}")) for k in range(NDMASEM[e] + (NRING2 if e == POOL else 0))] for e in ENGS}
        engs = {PE: nc.tensor, ACT: nc.scalar, DVE: nc.vector, POOL: nc.gpsimd, SP: nc.sync}
        C.P.emit(sems, dsems, engs)
        C.P.final_wait(sems, dsems, engs)


def host_consts():
    t = np.arange(S)
    row = (t // 64).astype(np.float32)
    col = (t % 64).astype(np.float32)
    freqs = (10000.0 ** (-np.arange(0, 32, 2, dtype=np.float32) / 32)).astype(np.float32)
    ang = np.concatenate([row[:, None] * freqs, col[:, None] * freqs], axis=-1).astype(np.float32)
    cos = np.cos(ang).astype(np.float32)
    sin = np.sin(ang).astype(np.float32)
    pidx = (np.arange(128) % 64) // 2
    ctab = np.ascontiguousarray(cos[:, pidx].T)
    stab = np.ascontiguousarray(sin[:, pidx].T)
    prot = np.zeros((128, 128), np.float32)
    for i in range(64):
        prot[2 * i + 1, 2 * i] = -1.0
        prot[2 * i, 2 * i + 1] = 1.0
    blk = np.zeros((128, 128), np.float32)
    blk[:64, :64] = 1.0
    blk[64:, 64:] = 1.0
    return {"ctab": ctab, "stab": stab, "prot": prot, "blk": blk}


def stage_attn(C, patt, qn_d, kn_d, cst, ycT):
    with ExitStack() as st:
        qT, qT_b = C.sb(st, [128, 4, S], BF16, "qT")
        kT2, kT_b = C.sb(st, [128, 2, 2, S], BF16, "kTz")
        Vx, Vx_b = C.sb(st, [128, NT, 2, 65], BF16, "Vx")
        ones, ones_b = C.sb(st, [128, 128], F32, "ones")
        C.op(POOL, lambda e: e.memset(ones[:], 1.0), [], [ones_b])
        C.op(POOL, lambda e: e.memset(Vx[:, :, :, 0:1], 1.0), [], [Vx_b])
        C.op(POOL, lambda e: e.memset(kT2[:], 0.0), [], [kT_b])
        with ExitStack() as s1:
            blk, blk_b = C.sb(s1, [128, 128], F32, "blk")
            prot, prot_b = C.sb(s1, [128, 128], F32, "prot")
            identf, identf_b = C.sb(s1, [128, 128], F32, "identf")
            gq, gq_b = C.sb(s1, [128, 2], F32, "gqk")
            C.dma(SP, blk[:], cst["blk"][0], [cst["blk"][1]], [blk_b])
            C.dma(SP, prot[:], cst["prot"][0], [cst["prot"][1]], [prot_b])
            for hh in range(2):
                C.dma(SP, gq[hh * 64:(hh + 1) * 64, 0:1], qn_d[0].rearrange("(p o) -> p o", o=1), [qn_d[1]], [gq_b])
                C.dma(SP, gq[hh * 64:(hh + 1) * 64, 1:2], kn_d[0].rearrange("(p o) -> p o", o=1), [kn_d[1]], [gq_b])
            C.op(POOL, lambda e: e.memset(identf[:], 0.0), [], [identf_b])
            C.op(POOL, lambda e: e.affine_select(out=identf[:], in_=identf[:], pattern=[[-1, 128]], compare_op=ALU.not_equal,
                                                 fill=1.0, base=0, channel_multiplier=1), [identf_b], [identf_b])
            qcs = [C.sb(s1, [128, 512], F32, "qc") for _ in range(2)]
            ctb = [C.sb(s1, [128, 2, 512], F32, "cs") for _ in range(2)]
            sq, sq_b = C.sb(s1, [128, 512], F32, "sq")
            rs, rs_b = C.sb(s1, [128, 512], F32, "rs")
            qn, qn_b = C.sb(s1, [128, 512], F32, "qn")
            o1, o1_b = C.sb(s1, [128, 512], F32, "o1")
            o2, o2_b = C.sb(s1, [128, 512], F32, "o2")
            ssp = [C.ps(s1, [128, 512], F32, "ssp") for _ in range(2)]
            rtp = [C.ps(s1, [128, 512], F32, "rtp") for _ in range(2)]
            vtp = [C.ps(s1, [128, 128], F32, "vtp") for _ in range(2)]
            it = 0
            vi = 0
            for g in range(NG):
                tsl = slice(g * 512, (g + 1) * 512)
                cs, cs_b = ctb[g % 2]
                C.dma(SP, cs[:, 0, :], cst["ctab"][0][:, tsl], [cst["ctab"][1]], [cs_b])
                C.dma(SP, cs[:, 1, :], cst["stab"][0][:, tsl], [cst["stab"][1]], [cs_b])
                for ch in range(6):
                    qc, qc_b = qcs[it % 2]
                    sp_, sp_b = ssp[it % 2]
                    rp, rp_b = rtp[it % 2]
                    it += 1
                    if ch < 4:
                        C.dma(ACT, qc[:], patt[0][ch * 128:(ch + 1) * 128, tsl], [patt[1]], [qc_b])
                        gcol = gq[:, 0:1]
                        dst = qT[:, ch, tsl]
                        dst_b = qT_b
                    else:
                        r0 = 512 + (ch - 4) * 64
                        C.dma(ACT, qc[0:64, :], patt[0][r0:r0 + 64, tsl], [patt[1]], [qc_b])
                        C.dma(ACT, qc[64:128, :], patt[0][r0:r0 + 64, tsl], [patt[1]], [qc_b])
                        gcol = gq[:, 1:2]
                        dst = None
                        dst_b = kT_b
                    C.op(ACT, act(AF.Square, sq[:], qc[:]), [qc_b], [sq_b])
                    C.op(PE, lambda e, sp_=sp_: e.matmul(out=sp_[:], lhsT=blk[:], rhs=sq[:], start=True, stop=True),
                         [blk_b, sq_b], [sp_b])
                    C.op(ACT, act(AF.Sqrt, rs[:], sp_[:], scale=1.0 / 64, bias=EPS), [sp_b], [rs_b])
                    C.op(DVE, lambda e: e.reciprocal(out=rs[:], in_=rs[:]), [rs_b], [rs_b])
                    C.op(DVE, lambda e, qc=qc, gcol=gcol: e.scalar_tensor_tensor(out=qn[:], in0=qc[:], scalar=gcol, in1=rs[:],
                                                                                 op0=ALU.mult, op1=ALU.mult),
                         [qc_b, gq_b, rs_b], [qn_b])
                    C.op(PE, lambda e, rp=rp: e.matmul(out=rp[:], lhsT=prot[:], rhs=qn[:], start=True, stop=True),
                         [prot_b, qn_b], [rp_b])
                    C.op(POOL, lambda e, cs=cs: e.tensor_tensor(out=o1[:], in0=qn[:], in1=cs[:, 0, :], op=ALU.mult),
                         [qn_b, cs_b], [o1_b])
                    C.op(DVE, lambda e, cs=cs, rp=rp: e.tensor_tensor(out=o2[:], in0=rp[:], in1=cs[:, 1, :], op=ALU.mult),
                         [rp_b, cs_b], [o2_b])
                    if dst is not None:
                        C.op(POOL, lambda e, dst=dst: e.tensor_tensor(out=dst, in0=o1[:], in1=o2[:], op=ALU.add),
                             [o1_b, o2_b], [dst_b])
                    else:
                        for v_ in range(2):
                            psl = slice(v_ * 64, (v_ + 1) * 64)
                            _tt(C, POOL, kT2[psl, ch - 4, v_, tsl], o1[psl, :], o2[psl, :], ALU.add, [o1_b, o2_b], [dst_b])
                qc, qc_b = qcs[it % 2]
                it += 1
                C.dma(ACT, qc[:], patt[0][640:768, tsl], [patt[1]], [qc_b])
                for s in range(4):
                    vp, vp_b = vtp[vi % 2]
                    vi += 1
                    C.op(PE, lambda e, vp=vp, qc=qc, s=s: e.transpose(out=vp[:], in_=qc[:, s * 128:(s + 1) * 128],
                                                                      identity=identf[:]), [qc_b, identf_b], [vp_b])
                    ti = g * 4 + s
                    C.op(DVE, lambda e, vp=vp, ti=ti: e.tensor_copy(
                        out=Vx[:, ti, :, 1:65], in_=vp[:].rearrange("p (k d) -> p k d", k=2)), [vp_b], [Vx_b])
        C.P.barrier()
        with ExitStack() as s2:
            sps = [C.ps(s2, [128, 1024], F32, "sps") for _ in range(2)]
            ots = [C.ps(s2, [128, 512], F32, "ot") for _ in range(2)]
            bcp, bcp_b = C.ps(s2, [128, 512], F32, "bcp")
            pts = [C.sb(s2, [128, 1024], BF16, "pt") for _ in range(2)]
            osbs = [C.sb(s2, [128, 512], F32, "osb") for _ in range(2)]
            rec, rec_b = C.sb(s2, [1, 512], F32, "rec")
            ysb = [C.sb(s2, [128, 512], BF16, "ysb") for _ in range(2)]
            oi = 0
            iters = [(h, q2, stl) for h in range(8) for q2 in range(8) for stl in range(NT)]

            def emit_S(n):
                h, q2, stl = iters[n]
                kvh, ch, base = h // 4, h // 2, (h % 2) * 64
                q0 = q2 * 1024
                sp_, sp_b = sps[n % 2]
                for half in range(2):
                    C.op(PE, lambda e, sp_=sp_, half=half, stl=stl, kvh=kvh, ch=ch, base=base, q0=q0: e.matmul(
                        out=sp_[:, half * 512:(half + 1) * 512],
                        lhsT=kT2[:, kvh, base // 64, stl * 128:(stl + 1) * 128],
                        rhs=qT[:, ch, q0 + half * 512:q0 + (half + 1) * 512],
                        start=True, stop=True), [kT_b, qT_b], [sp_b])

            emit_S(0)
            for n, (h, q2, stl) in enumerate(iters):
                kvh = h // 4
                q0 = q2 * 1024
                if n + 1 < len(iters):
                    emit_S(n + 1)
                sp_, sp_b = sps[n % 2]
                pt, pt_b = pts[n % 2]
                C.op(ACT, act(AF.Exp, pt[:], sp_[:], scale=0.125), [sp_b], [pt_b])
                for half in range(2):
                    ot, ot_b = ots[half]
                    C.op(PE, lambda e, ot=ot, pt=pt, half=half, stl=stl, kvh=kvh: e.matmul(
                        out=ot[0:65, :], lhsT=Vx[:, stl, kvh, :], rhs=pt[:, half * 512:(half + 1) * 512],
                        start=(stl == 0), stop=(stl == NT - 1)), [Vx_b, pt_b], [ot_b])
                if stl == NT - 1:
                    for half in range(2):
                        ot, ot_b = ots[half]
                        osb, osb_b = osbs[oi % 2]
                        y, y_b = ysb[oi % 2]
                        oi += 1
                        C.op(DVE, lambda e, osb=osb, ot=ot: e.tensor_copy(out=osb[0:65, :], in_=ot[0:65, :]), [ot_b], [osb_b])
                        C.op(DVE, lambda e, osb=osb: e.reciprocal(out=rec[:], in_=osb[0:1, :]), [osb_b], [rec_b])
                        C.op(PE, lambda e: e.matmul(out=bcp[0:65, :], lhsT=ones[0:1, 0:65], rhs=rec[:], start=True, stop=True),
                             [ones_b, rec_b], [bcp_b])
                        C.op(DVE, lambda e, y=y, osb=osb: e.tensor_tensor(out=y[0:65, :], in0=osb[0:65, :], in1=bcp[0:65, :],
                                                                          op=ALU.mult), [osb_b, bcp_b], [y_b])
                        c0 = q0 + half * 512
                        C.dma(SP, ycT[0][h * 64:(h + 1) * 64, c0:c0 + 512], y[1:65, :], [y_b], [ycT[1]])
        C.P.barrier()


def _tt(C, eng, out, in0, in1, op, R, W):
    return C.op(eng, lambda e: e.tensor_tensor(out=out, in0=in0, in1=in1, op=op), R, W)


def _ts(C, eng, out, in0, s1, s2, op0, op1, R, W):
    if s2 is None:
        return C.op(eng, lambda e: e.tensor_scalar(out=out, in0=in0, scalar1=s1, scalar2=None, op0=op0), R, W)
    return C.op(eng, lambda e: e.tensor_scalar(out=out, in0=in0, scalar1=s1, scalar2=s2, op0=op0, op1=op1), R, W)


def _stt(C, eng, out, in0, scalar, in1, op0, op1, R, W):
    return C.op(eng, lambda e: e.scalar_tensor_tensor(out=out, in0=in0, scalar=scalar, in1=in1, op0=op0, op1=op1), R, W)


def _act(C, func, out, in_, R, W, **kw):
    return C.op(ACT, lambda e: e.activation(out=out, in_=in_, func=func, **kw), R, W)


def _mm(C, out, lhsT, rhs, R, W, start=True, stop=True):
    return C.op(PE, lambda e: e.matmul(out=out, lhsT=lhsT, rhs=rhs, start=start, stop=stop), R, W)


def _cp(C, eng, out, in_, R, W):
    if eng == ACT:
        return C.op(ACT, lambda e: e.copy(out=out, in_=in_), R, W)
    return C.op(eng, lambda e: e.tensor_copy(out=out, in_=in_), R, W)


PAR_COLS = 74
LAM = 0.6065306597126334


def pack_par(inp, l):
    def pc(v):
        return np.asarray(v, np.float32).reshape(-1, 128).T
    cols = [pc(inp["shift_prev"][l]), pc(inp["shift_next"][l]), pc(inp["rwkv_k_k"][l]), pc(inp["rwkv_k_a"][l]),
            pc(inp["rwkv_r_k"][l]), pc(inp["rwkv_w0"][l][0]), pc(inp["rwkv_w0"][l][1]), pc(inp["rwkv_a0"][l][0]),
            pc(inp["rwkv_a0"][l][1]),
            pc(inp["vres_v0"][l - 1]) if l > 0 else np.zeros((128, 4), np.float32),
            pc(inp["rwkv_ln_w"][l]), pc(inp["rwkv_ln_b"][l]), pc(inp["pool_scale"][l])]
    return np.ascontiguousarray(np.concatenate(cols, axis=1))


def stage_rwkv_prep(C, l, prw, par_d, w2_d, a2_d, g2_d, blk_d, RO, vres=None):
    with ExitStack() as st:
        par, par_b = C.sb(st, [128, PAR_COLS], F32, "par")
        dv, dv_b = C.sb(st, [128, 19], F32, "dv")
        w2s, w2_b = C.sb(st, [128, 512], F32, "w2s")
        a2s, a2_b = C.sb(st, [128, 512], F32, "a2s")
        g2s, g2_b = C.sb(st, [128, 512], F32, "g2s")
        blk, blk_b = C.sb(st, [128, 128], F32, "blk")
        C.dma(SP, par[:], par_d[0], [par_d[1]], [par_b])
        C.dma(SP, w2s[:], w2_d[0].rearrange("d l c -> (d l) c"), [w2_d[1]], [w2_b])
        C.dma(SP, a2s[:], a2_d[0].rearrange("d l c -> (d l) c"), [a2_d[1]], [a2_b])
        C.dma(SP, g2s[:], g2_d[0], [g2_d[1]], [g2_b])
        C.dma(SP, blk[:], blk_d[0], [blk_d[1]], [blk_b])
        if vres is not None:
            vw2, vw2_b = C.sb(st, [32, 512], F32, "vw2")
            C.dma(SP, vw2[:], vres["w2"][0], [vres["w2"][1]], [vw2_b])
        _tt(C, DVE, dv[:, 0:15], par[:, 0:15], par[:, 15:30], ALU.add, [par_b], [dv_b])
        _ts(C, DVE, dv[:, 0:15], dv[:, 0:15], -1.0, 1.0, ALU.mult, ALU.add, [dv_b], [dv_b])
        _ts(C, DVE, dv[:, 15:19], par[:, 34:38], -1.0, 1.0, ALU.mult, ALU.add, [par_b], [dv_b])
        dqc = [0]

        def grp_gen(stream):
            raws = [C.sb(st, [128, 514], F32, "raw") for _ in range(3)]
            sh = [C.sb(st, [128, 512], F32, "sh") for _ in range(15)]
            tmp = [C.sb(st, [128, 512], F32, "tmp") for _ in range(6)]
            outb = [C.sb(st, [128, 512], F32, "outb") for _ in range(8)]
            pss = [C.ps(st, [128, 512], F32, "rps") for _ in range(4)]
            cnt = {"raw": 0, "tmp": 0, "out": 0, "ps": 0}

            def nxt(lst, key):
                x = lst[cnt[key] % len(lst)]
                cnt[key] += 1
                return x

            def store(name, c, tsl, t, t_b):
                eng = (SP, ACT, POOL)[dqc[0] % 3]
                dqc[0] += 1
                C.dma(eng, RO[name][0][c * 128:(c + 1) * 128, tsl], t[:], [t_b], [RO[name][1]])

            for g in range(stream, NG, 2):
                t0 = g * 512
                tsl = slice(t0, t0 + 512)
                for c in range(15):
                    raw, raw_b = nxt(raws, "raw")
                    lo = max(t0 - 1, 0)
                    hi = min(t0 + 513, S)
                    if g == 0:
                        C.op(POOL, lambda e, raw=raw: e.memset(raw[:, 0:1], 0.0), [], [raw_b])
                    if g == NG - 1:
                        C.op(POOL, lambda e, raw=raw: e.memset(raw[:, 513:514], 0.0), [], [raw_b])
                    C.dma(SP if c % 2 else ACT, raw[:, lo - (t0 - 1):hi - (t0 - 1)], prw[0][c * 128:(c + 1) * 128, lo:hi],
                          [prw[1]], [raw_b])
                    s_, s_b = sh[c]
                    _act(C, AF.Copy, s_[:], raw[:, 1:513], [raw_b, dv_b], [s_b], scale=dv[:, c:c + 1])
                    _stt(C, DVE, s_[:], raw[:, 0:512], par[:, c:c + 1], s_[:], ALU.mult, ALU.add, [raw_b, par_b, s_b], [s_b])
                    _stt(C, DVE, s_[:], raw[:, 2:514], par[:, 15 + c:16 + c], s_[:], ALU.mult, ALU.add, [raw_b, par_b, s_b], [s_b])
                    yield
                twd, twd_b = sh[12]
                sad, sad_b = sh[13]
                sgd, sgd_b = sh[14]
                _act(C, AF.Tanh, twd[:], twd[:], [twd_b], [twd_b])
                _act(C, AF.Sigmoid, sgd[:], sgd[:], [sgd_b], [sgd_b])
                for c in range(4):
                    csl = slice(c * 128, (c + 1) * 128)
                    r_, r_b = sh[c]
                    k_, k_b = sh[4 + c]
                    v_, v_b = sh[8 + c]
                    store("r", c, tsl, r_, r_b)
                    av = []
                    for d in range(2):
                        dsl = slice(d * 64, (d + 1) * 64)
                        ps, ps_b = nxt(pss, "ps")
                        _mm(C, ps[:], w2s[dsl, csl], twd[dsl, :], [w2_b, twd_b], [ps_b])
                        o, o_b = nxt(outb, "out")
                        _act(C, AF.Sigmoid, o[:], ps[:], [ps_b, par_b], [o_b], bias=par[:, 42 + 4 * d + c:43 + 4 * d + c])
                        store(f"sg{d}", c, tsl, o, o_b)
                        ps, ps_b = nxt(pss, "ps")
                        _mm(C, ps[:], a2s[dsl, csl], sad[dsl, :], [a2_b, sad_b], [ps_b])
                        o, o_b = nxt(outb, "out")
                        _act(C, AF.Sigmoid, o[:], ps[:], [ps_b, par_b], [o_b], bias=par[:, 50 + 4 * d + c:51 + 4 * d + c])
                        store(f"a{d}", c, tsl, o, o_b)
                        av.append((o, o_b))
                    ps, ps_b = nxt(pss, "ps")
                    _mm(C, ps[:], g2s[:, csl], sgd[:], [g2_b, sgd_b], [ps_b])
                    o, o_b = nxt(outb, "out")
                    _cp(C, ACT, o[:], ps[:], [ps_b], [o_b])
                    store("g", c, tsl, o, o_b)
                    yield
                    if vres is not None:
                        hv, hv_b = nxt(tmp, "tmp")
                        C.dma(SP, hv[0:32, :], vres["hv1"][0][:, tsl], [vres["hv1"][1]], [hv_b])
                        vf, vf_b = nxt(tmp, "tmp")
                        C.dma(ACT, vf[:], vres["vfirst"][0][csl, tsl], [vres["vfirst"][1]], [vf_b])
                        ps, ps_b = nxt(pss, "ps")
                        _mm(C, ps[:], vw2[:, csl], hv[0:32, :], [vw2_b, hv_b], [ps_b])
                        mx, mx_b = nxt(tmp, "tmp")
                        _act(C, AF.Sigmoid, mx[:], ps[:], [ps_b, par_b], [mx_b], bias=par[:, 58 + c:59 + c])
                        _tt(C, DVE, vf[:], vf[:], v_[:], ALU.subtract, [vf_b, v_b], [vf_b])
                        _tt(C, DVE, vf[:], vf[:], mx[:], ALU.mult, [vf_b, mx_b], [vf_b])
                        _tt(C, DVE, v_[:], v_[:], vf[:], ALU.add, [v_b, vf_b], [v_b])
                    else:
                        store("vfirst", c, tsl, v_, v_b)
                    store("v", c, tsl, v_, v_b)
                    sq, sq_b = nxt(tmp, "tmp")
                    _act(C, AF.Square, sq[:], k_[:], [k_b, par_b], [sq_b], scale=par[:, 30 + c:31 + c])
                    ps, ps_b = nxt(pss, "ps")
                    _mm(C, ps[:], blk[:], sq[:], [blk_b, sq_b], [ps_b])
                    nr, nr_b = nxt(tmp, "tmp")
                    _act(C, AF.Sqrt, nr[:], ps[:], [ps_b], [nr_b])
                    _ts(C, DVE, nr[:], nr[:], 1e-12, None, ALU.max, None, [nr_b], [nr_b])
                    C.op(DVE, lambda e, nr=nr: e.reciprocal(out=nr[:], in_=nr[:]), [nr_b], [nr_b])
                    o, o_b = nxt(outb, "out")
                    _stt(C, DVE, o[:], k_[:], par[:, 30 + c:31 + c], nr[:], ALU.mult, ALU.mult, [k_b, par_b, nr_b], [o_b])
                    store("kk", c, tsl, o, o_b)
                    yield
                    kds = []
                    for d in range(2):
                        a_, a_b = av[d]
                        o, o_b = nxt(outb, "out")
                        _ts(C, POOL, o[:], a_[:], par[:, 34 + c:35 + c], dv[:, 15 + c:16 + c], ALU.mult, ALU.add,
                            [a_b, par_b, dv_b], [o_b])
                        _tt(C, POOL, o[:], o[:], k_[:], ALU.mult, [o_b, k_b], [o_b])
                        store(f"kd{d}", c, tsl, o, o_b)
                        kds.append((o, o_b))
                    ks, ks_b = nxt(tmp, "tmp")
                    _tt(C, DVE, ks[:], kds[0][0][:], kds[1][0][:], ALU.add, [kds[0][1], kds[1][1]], [ks_b])
                    _stt(C, DVE, ks[:], r_[:], par[:, 38 + c:39 + c], ks[:], ALU.mult, ALU.mult, [r_b, par_b, ks_b], [ks_b])
                    ps, ps_b = nxt(pss, "ps")
                    _mm(C, ps[:], blk[:], ks[:], [blk_b, ks_b], [ps_b])
                    o, o_b = nxt(outb, "out")
                    _tt(C, DVE, o[:], ps[:], v_[:], ALU.mult, [ps_b, v_b], [o_b])
                    store("bonus", c, tsl, o, o_b)
                    yield

        gens = [grp_gen(0), grp_gen(1)]
        while gens:
            for gq in list(gens):
                try:
                    next(gq)
                except StopIteration:
                    gens.remove(gq)
    C.P.barrier()


RNAMES = ("r", "v", "g", "bonus", "kk", "kd0", "kd1", "a0", "a1", "sg0", "sg1")


def scan_consts():
    i = np.arange(64)
    lo = (i[None, :] < i[:, None]).astype(np.float32)
    up = (i[None, :] > i[:, None]).astype(np.float32)
    loi = (i[None, :] <= i[:, None]).astype(np.float32)
    upi = (i[None, :] >= i[:, None]).astype(np.float32)
    idn = np.eye(64, dtype=np.float32)
    return np.ascontiguousarray(np.stack([lo, up, loi, upi, idn], axis=1))


def stage_rwkv_scan(C, RO, msk_d, y_d):
    GT = 128
    with ExitStack() as st:
        msk, msk_b = C.sb(st, [64, 5, 64], F32, "msk")
        C.dma(SP, msk[:], msk_d[0], [msk_d[1]], [msk_b])
        idnb, idnb_b = C.sb(st, [64, 64], BF16, "idnb")
        _cp(C, DVE, idnb[:], msk[:, 4, :], [msk_b], [idnb_b])
        rmask, rmask_b = C.sb(st, [64, 8, GT], F32, "rmask")
        C.op(POOL, lambda e: e.memset(rmask[:], 1.0), [], [rmask_b])
        C.op(POOL, lambda e: e.memset(rmask[:].rearrange("p h (c t) -> p (h c) t", t=64)[:, :, 0:1], 0.0), [rmask_b], [rmask_b])
        pss = [C.ps(st, [64, 8, 64], F32, "sps") for _ in range(6)]
        tpss = [C.ps(st, [64, 8, 64], BF16, "tps") for _ in range(2)]
        pc = [0, 0]
        NCH = GT // 64

        def nps():
            x = pss[pc[0] % 6]
            pc[0] += 1
            return x

        def ntps():
            x = tpss[pc[1] % 2]
            pc[1] += 1
            return x

        def mm8(lhs_fn, rhs_fn, R):
            p, p_b = nps()
            for h in range(8):
                _mm(C, p[:, h, :], lhs_fn(h), rhs_fn(h), R, [p_b])
            return p, p_b

        def flat(t):
            return t[:].rearrange("p h t -> p (h t)")

        def v4(t):
            return t[:].rearrange("p h (c t) -> p (h c) t", t=64)

        bufs = []
        for d in range(2):
            H = C.sb(st, [64, 8, 64], F32, "H")
            Hb = C.sb(st, [64, 8, 64], BF16, "Hb")
            names = ["r", "v", "kk", "kd", "a", "sg", "cs", "e2", "Gi"] + (["cs2"] if d == 1 else [])
            G = {n: C.sb(st, [64, 8, GT], F32, "g_" + n) for n in names}
            Gb = {n: C.sb(st, [64, 8, GT], BF16, "gb_" + n) for n in ("At", "Bt", "Kt", "Rt", "Bh", "Kh", "vb")}
            Cb = {n: C.sb(st, [64, 8, 64], BF16, "c_" + n) for n in
                  ("Vt", "Bht", "Kht", "Pa", "PTa", "Pb", "PTb", "TT", "AakT", "ArbT", "ArkT", "W", "U")}
            Cf = {n: C.sb(st, [64, 8, 64], F32, "c_" + n) for n in ("WV", "YV", "ZV", "Yo", "Ht")}
            bufs.append((H, Hb, G, Gb, Cb, Cf))

        def dir_gen(d):
            (H, H_b), (Hb, Hb_b), G, Gb, Cb, Cf = bufs[d]
            NS = NCH * 8
            mN = msk[:, 0 if d == 0 else 1, :].unsqueeze(1).to_broadcast([64, 8, 64])
            mNT = msk[:, 1 if d == 0 else 0, :].unsqueeze(1).to_broadcast([64, 8, 64])
            mI = msk[:, 3 if d == 0 else 2, :].unsqueeze(1).to_broadcast([64, 8, 64])
            idb = msk[:, 4, :].unsqueeze(1).to_broadcast([64, 8, 64])
            dq_e = (SP, ACT) if d == 0 else (ACT, SP)
            C.op(DVE, lambda e: e.memset(H[:], 0.0), [], [H_b])
            C.op(DVE, lambda e: e.memset(Hb[:], 0.0), [], [Hb_b])
            for gi in range(S // GT):
                g = gi if d == 0 else S // GT - 1 - gi
                t0 = g * GT
                dq = 0
                for n, src in (("r", "r"), ("v", "v"), ("kk", "kk"), ("kd", f"kd{d}"), ("a", f"a{d}"), ("sg", f"sg{d}")):
                    t, t_b = G[n]
                    C.dma(dq_e[dq % 2], t[:], RO[src][0][:, t0:t0 + GT].rearrange("(h j) t -> j h t", j=64),
                          [RO[src][1]], [t_b])
                    dq += 1
                r_, r_b = G["r"]; v_, v_b = G["v"]; kk_, kk_b = G["kk"]; kd_, kd_b = G["kd"]; a_, a_b = G["a"]
                sg_, sg_b = G["sg"]; cs_, cs_b = G["cs"]; e2_, e2_b = G["e2"]; Gi_, Gi_b = G["Gi"]
                At_, At_b = Gb["At"]; Bt_, Bt_b = Gb["Bt"]; Kt_, Kt_b = Gb["Kt"]; Rt_, Rt_b = Gb["Rt"]
                Bh_, Bh_b = Gb["Bh"]; Kh_, Kh_b = Gb["Kh"]; vb_, vb_b = Gb["vb"]
                yield
                C.op(DVE, lambda e: e.tensor_tensor_scan(out=flat(cs_), data0=flat(rmask), data1=flat(sg_), initial=0.0,
                                                         op0=ALU.mult, op1=ALU.add), [rmask_b, sg_b], [cs_b])
                if d == 0:
                    c2_, c2_b = cs_, cs_b
                    tot = v4(cs_)[:, :, 63:64].to_broadcast([64, NS, 64])
                else:
                    c2_, c2_b = G["cs2"]
                    _tt(C, DVE, c2_[:], sg_[:], cs_[:], ALU.subtract, [sg_b, cs_b], [c2_b])
                    _tt(C, DVE, v4(c2_), v4(c2_), v4(cs_)[:, :, 63:64].to_broadcast([64, NS, 64]), ALU.add, [c2_b, cs_b], [c2_b])
                    tot = v4(c2_)[:, :, 0:1].to_broadcast([64, NS, 64])
                _tt(C, DVE, v4(e2_), tot, v4(c2_), ALU.subtract, [c2_b], [e2_b])
                _tt(C, POOL, sg_[:], c2_[:], sg_[:], ALU.subtract, [c2_b, sg_b], [sg_b])
                yield
                _act(C, AF.Exp, Gi_[:], c2_[:], [c2_b], [Gi_b], scale=LAM)
                _act(C, AF.Exp, c2_[:], c2_[:], [c2_b, e2_b], [c2_b], scale=-LAM)
                Gm_, Gm_b = c2_, c2_b
                _act(C, AF.Exp, sg_[:], sg_[:], [sg_b], [sg_b], scale=-LAM)
                _act(C, AF.Exp, e2_[:], e2_[:], [e2_b], [e2_b], scale=-LAM)
                _cp(C, ACT, vb_[:], v_[:], [v_b], [vb_b])
                yield
                _tt(C, POOL, a_[:], kk_[:], a_[:], ALU.mult, [kk_b, a_b], [a_b])
                _stt(C, DVE, At_[:], kk_[:], -1.0, sg_[:], ALU.mult, ALU.mult, [kk_b, sg_b], [At_b])
                _tt(C, POOL, Bt_[:], a_[:], Gi_[:], ALU.mult, [a_b, Gi_b], [Bt_b])
                _tt(C, DVE, Kt_[:], kd_[:], Gi_[:], ALU.mult, [kd_b, Gi_b], [Kt_b])
                yield
                _tt(C, POOL, Rt_[:], r_[:], Gm_[:], ALU.mult, [r_b, Gm_b], [Rt_b])
                _tt(C, DVE, Bh_[:], a_[:], e2_[:], ALU.mult, [a_b, e2_b], [Bh_b])
                _tt(C, POOL, Kh_[:], kd_[:], e2_[:], ALU.mult, [kd_b, e2_b], [Kh_b])
                yield
                for ci in range(NCH):
                    cc = ci if d == 0 else NCH - 1 - ci
                    ts_ = slice(cc * 64, cc * 64 + 64)
                    Vt, Vt_b = Cb["Vt"]; Bht, Bht_b = Cb["Bht"]; Kht, Kht_b = Cb["Kht"]
                    for src, src_b, dst, dst_b in ((vb_, vb_b, Vt, Vt_b), (Bh_, Bh_b, Bht, Bht_b), (Kh_, Kh_b, Kht, Kht_b)):
                        p, p_b = ntps()
                        for h in range(8):
                            C.op(PE, lambda e, p=p, h=h, src=src, ts_=ts_: e.transpose(out=p[:, h, :], in_=src[:, h, ts_],
                                                                                       identity=idnb[:]), [src_b, idnb_b], [p_b])
                        _cp(C, ACT, dst[:], p[:], [p_b], [dst_b])
                        yield
                    P_, P_b = Cb["Pa"]; PT_, PT_b = Cb["PTa"]; TT, TT_b = Cb["TT"]
                    p, p_b = mm8(lambda h: At_[:, h, ts_], lambda h: Bt_[:, h, ts_], [At_b, Bt_b])
                    _tt(C, DVE, P_[:], p[:], mN, ALU.mult, [p_b, msk_b], [P_b])
                    yield
                    p, p_b = mm8(lambda h: Bt_[:, h, ts_], lambda h: At_[:, h, ts_], [At_b, Bt_b])
                    _tt(C, DVE, PT_[:], p[:], mNT, ALU.mult, [p_b, msk_b], [PT_b])
                    _tt(C, DVE, TT[:], PT_[:], idb, ALU.add, [PT_b, msk_b], [TT_b])
                    yield
                    AakT, AakT_b = Cb["AakT"]; ArbT, ArbT_b = Cb["ArbT"]; ArkT, ArkT_b = Cb["ArkT"]
                    p, p_b = mm8(lambda h: Kt_[:, h, ts_], lambda h: At_[:, h, ts_], [Kt_b, At_b])
                    _tt(C, DVE, AakT[:], p[:], mNT, ALU.mult, [p_b, msk_b], [AakT_b])
                    yield
                    p, p_b = mm8(lambda h: Bt_[:, h, ts_], lambda h: Rt_[:, h, ts_], [Bt_b, Rt_b])
                    _tt(C, DVE, ArbT[:], p[:], mI, ALU.mult, [p_b, msk_b], [ArbT_b])
                    yield
                    p, p_b = mm8(lambda h: Kt_[:, h, ts_], lambda h: Rt_[:, h, ts_], [Kt_b, Rt_b])
                    _tt(C, DVE, ArkT[:], p[:], mI, ALU.mult, [p_b, msk_b], [ArkT_b])
                    yield
                    cur = (P_, P_b, PT_, PT_b)
                    for rd in range(5):
                        Pc, Pc_b, PTc, PTc_b = cur
                        Pn, Pn_b = Cb["Pb"] if rd % 2 == 0 else Cb["Pa"]
                        PTn, PTn_b = Cb["PTb"] if rd % 2 == 0 else Cb["PTa"]
                        p, p_b = mm8(lambda h: PTc[:, h, :], lambda h: Pc[:, h, :], [Pc_b, PTc_b])
                        _cp(C, ACT, Pn[:], p[:], [p_b], [Pn_b])
                        yield
                        if rd < 4:
                            p, p_b = mm8(lambda h: Pc[:, h, :], lambda h: PTc[:, h, :], [Pc_b, PTc_b])
                            _cp(C, ACT, PTn[:], p[:], [p_b], [PTn_b])
                            yield
                        p, p_b = mm8(lambda h: Pn[:, h, :], lambda h: TT[:, h, :], [Pn_b, TT_b])
                        _tt(C, DVE, TT[:], p[:], TT[:], ALU.add, [p_b, TT_b], [TT_b])
                        yield
                        cur = (Pn, Pn_b, PTn, PTn_b)
                    WV, WV_b = Cf["WV"]; YV, YV_b = Cf["YV"]; ZV, ZV_b = Cf["ZV"]
                    p, p_b = mm8(lambda h: AakT[:, h, :], lambda h: Vt[:, h, :], [AakT_b, Vt_b])
                    _cp(C, ACT, WV[:], p[:], [p_b], [WV_b])
                    yield
                    p, p_b = mm8(lambda h: ArkT[:, h, :], lambda h: Vt[:, h, :], [ArkT_b, Vt_b])
                    _cp(C, ACT, YV[:], p[:], [p_b], [YV_b])
                    yield
                    p, p_b = mm8(lambda h: Kht[:, h, :], lambda h: Vt[:, h, :], [Kht_b, Vt_b])
                    _cp(C, ACT, ZV[:], p[:], [p_b], [ZV_b])
                    yield
                    W, W_b = Cb["W"]; U, U_b = Cb["U"]; Yo, Yo_b = Cf["Yo"]; Ht, Ht_b = Cf["Ht"]
                    p, p_b = mm8(lambda h: At_[:, h, ts_], lambda h: Hb[:, h, :], [At_b, Hb_b])
                    _tt(C, DVE, W[:], p[:], WV[:], ALU.add, [p_b, WV_b], [W_b])
                    yield
                    p, p_b = mm8(lambda h: TT[:, h, :], lambda h: W[:, h, :], [TT_b, W_b])
                    _cp(C, ACT, U[:], p[:], [p_b], [U_b])
                    yield
                    p, p_b = nps()
                    for h in range(8):
                        _mm(C, p[:, h, :], Rt_[:, h, ts_], Hb[:, h, :], [Rt_b, Hb_b], [p_b], start=True, stop=False)
                        _mm(C, p[:, h, :], ArbT[:, h, :], U[:, h, :], [ArbT_b, U_b], [p_b], start=False, stop=True)
                    _tt(C, DVE, Yo[:], p[:], YV[:], ALU.add, [p_b, YV_b], [Yo_b])
                    r0 = t0 + cc * 64
                    C.dma(SP, y_d[d][0][r0:r0 + 64, :], Yo[:].rearrange("p h i -> p (h i)"), [Yo_b], [y_d[d][1]])
                    yield
                    p, p_b = mm8(lambda h: Bht[:, h, :], lambda h: U[:, h, :], [Bht_b, U_b])
                    gidx = cc * 64 + (63 if d == 0 else 0)
                    gl = Gm_[:, :, gidx:gidx + 1].to_broadcast([64, 8, 64])
                    _tt(C, POOL, Ht[:], H[:], gl, ALU.mult, [H_b, Gm_b], [Ht_b])
                    _tt(C, POOL, Ht[:], Ht[:], ZV[:], ALU.add, [Ht_b, ZV_b], [Ht_b])
                    _tt(C, DVE, H[:], p[:], Ht[:], ALU.add, [p_b, Ht_b], [H_b])
                    _cp(C, ACT, Hb[:], H[:], [H_b], [Hb_b])
                    yield

        gens = [dir_gen(0), dir_gen(1)]
        while gens:
            for gq in list(gens):
                try:
                    next(gq)
                except StopIteration:
                    gens.remove(gq)
    C.P.barrier()


def stage_rwkv_out(C, y_d, RO, par_d, yaT):
    with ExitStack() as st:
        par, par_b = C.sb(st, [128, PAR_COLS], F32, "par")
        C.dma(SP, par[:], par_d[0], [par_d[1]], [par_b])
        identf, identf_b = C.sb(st, [128, 128], F32, "identf")
        C.op(POOL, lambda e: e.memset(identf[:], 0.0), [], [identf_b])
        C.op(POOL, lambda e: e.affine_select(out=identf[:], in_=identf[:], pattern=[[-1, 128]], compare_op=ALU.not_equal,
                                             fill=1.0, base=0, channel_multiplier=1), [identf_b], [identf_b])
        y0s = [C.sb(st, [128, 8, 64], F32, "y0") for _ in range(2)]
        y1s = [C.sb(st, [128, 8, 64], F32, "y1") for _ in range(2)]
        yhs = [C.sb(st, [128, 8, 64], F32, "yh") for _ in range(8)]
        sqt, sqt_b = C.sb(st, [128, 8, 64], F32, "sqt")
        sts = [C.sb(st, [128, 3, 8], F32, "st") for _ in range(2)]
        pts = [C.ps(st, [128, 512], F32, "pt") for _ in range(4)]
        bgs = [C.sb(st, [128, 2, 512], F32, "bg") for _ in range(2)]
        fms = [C.sb(st, [128, 512], F32, "fm") for _ in range(2)]
        obs = [C.sb(st, [128, 512], BF16, "ob") for _ in range(2)]
        it = 0
        oc = 0
        for g in range(NG):
            tsl = slice(g * 512, (g + 1) * 512)
            yh4 = []
            for s in range(4):
                y0, y0_b = y0s[it % 2]
                y1, y1_b = y1s[it % 2]
                sv, sv_b = sts[it % 2]
                yh, yh_b = yhs[it % 8]
                it += 1
                r0 = g * 512 + s * 128
                C.dma(SP, y0[:], y_d[0][0][r0:r0 + 128, :].rearrange("p (h i) -> p h i", i=64), [y_d[0][1]], [y0_b])
                C.dma(ACT, y1[:], y_d[1][0][r0:r0 + 128, :].rearrange("p (h i) -> p h i", i=64), [y_d[1][1]], [y1_b])
                _tt(C, DVE, y0[:], y0[:], y1[:], ALU.add, [y0_b, y1_b], [y0_b])
                C.op(DVE, lambda e, sv=sv, y0=y0: e.tensor_reduce(out=sv[:, 0, :], in_=y0[:], axis=AX.X, op=ALU.add), [y0_b], [sv_b])
                _ts(C, DVE, sv[:, 0, :], sv[:, 0, :], 1.0 / 64, None, ALU.mult, None, [sv_b], [sv_b])
                _tt(C, DVE, y0[:], y0[:], sv[:, 0, :].unsqueeze(2).to_broadcast([128, 8, 64]), ALU.subtract, [y0_b, sv_b], [y0_b])
                _tt(C, POOL, sqt[:], y0[:], y0[:], ALU.mult, [y0_b], [sqt_b])
                C.op(DVE, lambda e, sv=sv: e.tensor_reduce(out=sv[:, 1, :], in_=sqt[:], axis=AX.X, op=ALU.add), [sqt_b], [sv_b])
                _act(C, AF.Sqrt, sv[:, 2, :], sv[:, 1, :], [sv_b], [sv_b], scale=1.0 / 64, bias=64e-5)
                C.op(DVE, lambda e, sv=sv: e.reciprocal(out=sv[:, 2, :], in_=sv[:, 2, :]), [sv_b], [sv_b])
                _tt(C, DVE, yh[:], y0[:], sv[:, 2, :].unsqueeze(2).to_broadcast([128, 8, 64]), ALU.mult, [y0_b, sv_b], [yh_b])
                yh4.append((yh, yh_b))
            for c in range(4):
                pt, pt_b = pts[oc % 4]
                bg, bg_b = bgs[oc % 2]
                fm, fm_b = fms[oc % 2]
                ob, ob_b = obs[oc % 2]
                oc += 1
                C.dma(SP, bg[:, 0, :], RO["bonus"][0][c * 128:(c + 1) * 128, tsl], [RO["bonus"][1]], [bg_b])
                C.dma(ACT, bg[:, 1, :], RO["g"][0][c * 128:(c + 1) * 128, tsl], [RO["g"][1]], [bg_b])
                for s in range(4):
                    yh, yh_b = yh4[s]
                    C.op(PE, lambda e, pt=pt, yh=yh, s=s, c=c: e.transpose(
                        out=pt[:, s * 128:(s + 1) * 128], in_=yh[:].rearrange("p h i -> p (h i)")[:, c * 128:(c + 1) * 128],
                        identity=identf[:]), [yh_b, identf_b], [pt_b])
                _act(C, AF.Identity, fm[:], pt[:], [pt_b, par_b], [fm_b], scale=par[:, 62 + c:63 + c], bias=par[:, 66 + c:67 + c])
                _tt(C, DVE, fm[:], fm[:], bg[:, 0, :], ALU.add, [fm_b, bg_b], [fm_b])
                _tt(C, DVE, ob[:], fm[:], bg[:, 1, :], ALU.mult, [fm_b, bg_b], [ob_b])
                C.dma(POOL, yaT[0][c * 128:(c + 1) * 128, tsl], ob[:], [ob_b], [yaT[1]])
    C.P.barrier()


LSTN = 2304


def moe_consts():
    t = np.arange(S)
    inv = np.zeros((4, S), np.float32)
    for gi, w in enumerate((2, 4, 8, 16)):
        lo = w // 2
        hi = w - lo - 1
        start = np.clip(t - lo, 0, S)
        end = np.clip(t + hi + 1, 0, S)
        inv[gi] = 1.0 / (end - start).astype(np.float32)
    tri = (np.arange(128)[:, None] < np.arange(128)[None, :]).astype(np.float32)
    tb = np.zeros((128, 65), np.float32)
    tb[:, :64] = 128.0 * np.arange(64, dtype=np.float32)[None, :]
    tb[:, 64] = np.arange(128, dtype=np.float32)
    eoff = np.ascontiguousarray(np.broadcast_to((np.arange(16, dtype=np.float32) * LSTN)[None, :], (128, 16)))
    return {"invcnt": inv, "tri": tri, "tbase": tb, "eoff": eoff}


def load_cast(C, st, eng_cast, dst, dst_b, src_ap, src_b, stg, k0):
    t, t_b = stg[k0 % len(stg)]
    a, n = src_ap.shape[1], src_ap.shape[2]
    C.dma((SP, ACT)[k0 % 2], t[:, 0:a, 0:n], src_ap, [src_b], [t_b])
    _cp(C, eng_cast, dst, t[:, 0:a, 0:n], [t_b], [dst_b])


def stage_pool(C, ppool, pw_d, par_d, inv_d, ybT):
    with ExitStack() as st:
        W = S + 32
        par, par_b = C.sb(st, [128, PAR_COLS], F32, "par")
        C.dma(SP, par[:], par_d[0], [par_d[1]], [par_b])
        zp, zp_b = C.sb(st, [128, W], F32, "zp")
        sa, sa_b = C.sb(st, [128, W], F32, "sa")
        sb_, sb_b = C.sb(st, [128, W], F32, "sb")
        inv, inv_b = C.sb(st, [128, S], F32, "inv")
        pl, pl_b = C.sb(st, [128, S], BF16, "pl")
        pwf, pwf_b = C.sb(st, [128, 128], F32, "pwf")
        pw, pw_b = C.sb(st, [128, 128], BF16, "pw")
        pss = [C.ps(st, [128, 512], F32, "pps") for _ in range(2)]
        obs = [C.sb(st, [128, 512], BF16, "pob") for _ in range(2)]
        C.op(POOL, lambda e: e.memset(zp[:], 0.0), [], [zp_b])
        C.op(POOL, lambda e: e.memset(sa[:], 0.0), [], [sa_b])
        C.op(POOL, lambda e: e.memset(sb_[:], 0.0), [], [sb_b])
        k = 0
        for gi in range(4):
            C.dma(SP, zp[:, 16:16 + S], ppool[0][gi * 128:(gi + 1) * 128, :], [ppool[1]], [zp_b])
            C.dma(ACT, inv[:], inv_d[0][gi:gi + 1, :].partition_broadcast(128) if False else inv_d[0][gi].partition_broadcast(128),
                  [inv_d[1]], [inv_b])
            C.dma(SP, pwf[:], pw_d[0][gi], [pw_d[1]], [pwf_b])
            _cp(C, POOL, pw[:], pwf[:], [pwf_b], [pw_b])
            _tt(C, DVE, sa[:, 1:W], zp[:, 1:W], zp[:, 0:W - 1], ALU.add, [zp_b], [sa_b])
            cur, cur_b, oth, oth_b = sa, sa_b, sb_, sb_b
            sh = 1
            lo_, hi_ = 1, W
            for lvl in range(gi):
                lo_, hi_ = lo_ + sh, hi_ - sh
                _tt(C, DVE, oth[:, lo_:hi_], cur[:, lo_ + sh:hi_ + sh], cur[:, lo_ - sh:hi_ - sh], ALU.add, [cur_b], [oth_b])
                cur, cur_b, oth, oth_b = oth, oth_b, cur, cur_b
                sh *= 2
            _tt(C, DVE, oth[:, 16:16 + S], cur[:, 16:16 + S], inv[:], ALU.mult, [cur_b, inv_b], [oth_b])
            _tt(C, POOL, pl[:], oth[:, 16:16 + S], zp[:, 16:16 + S], ALU.subtract, [oth_b, zp_b], [pl_b])
            for g in range(NG):
                tsl = slice(g * 512, (g + 1) * 512)
                ps, ps_b = pss[k % 2]
                ob, ob_b = obs[k % 2]
                k += 1
                _mm(C, ps[:], pw[:], pl[:, tsl], [pw_b, pl_b], [ps_b])
                _act(C, AF.Copy, ob[:], ps[:], [ps_b, par_b], [ob_b], scale=par[:, 70 + gi:71 + gi])
                C.dma(SP, ybT[0][gi * 128:(gi + 1) * 128, tsl], ob[:], [ob_b], [ybT[1]])
    C.P.barrier()


def stage_merge(C, x_in, x_tok, yT, pgate, wbr_d, wout_d, nffn_d, router_d, h_tok, aff_tok, affT):
    with ExitStack() as st:
        stg = [C.sb(st, [128, 4, 1024], F32, "mstg") for _ in range(2)]
        Wb = [C.sb(st, [128, 4, 1024], BF16, "Wb") for _ in range(3)]
        Wo, Wo_b = C.sb(st, [128, 8, 1024], BF16, "Wo")
        k0 = 0
        for b in range(3):
            load_cast(C, st, POOL, Wb[b][0][:], Wb[b][1], wbr_d[b][0].rearrange("(kc p) n -> p kc n", p=128), wbr_d[b][1], stg, k0)
            k0 += 1
        for hf in range(2):
            load_cast(C, st, POOL, Wo[:, hf * 4:(hf + 1) * 4, :], Wo_b,
                      wout_d[0][hf * 512:(hf + 1) * 512, :].rearrange("(kc p) n -> p kc n", p=128), wout_d[1], stg, k0)
            k0 += 1
        gb, gb_b = C.sb(st, [128, D], F32, "gbf")
        C.dma(SP, gb[:], nffn_d[0].partition_broadcast(128), [nffn_d[1]], [gb_b])
        rt, rt_b = C.sb(st, [128, 8, 16], F32, "rt")
        C.dma(SP, rt[:], router_d[0].rearrange("(kc p) e -> p kc e", p=128), [router_d[1]], [rt_b])
        identf, identf_b = C.sb(st, [128, 128], F32, "identf")
        C.op(POOL, lambda e: e.memset(identf[:], 0.0), [], [identf_b])
        C.op(POOL, lambda e: e.affine_select(out=identf[:], in_=identf[:], pattern=[[-1, 128]], compare_op=ALU.not_equal,
                                             fill=1.0, base=0, channel_multiplier=1), [identf_b], [identf_b])
        ys = [C.sb(st, [128, 4, 512], BF16, "ys") for _ in range(3)]
        gt, gt_b = C.sb(st, [128, 24, 512], BF16, "gt")
        mg, mg_b = C.sb(st, [128, 8, 512], BF16, "mg")
        tmps = [C.sb(st, [128, 512], F32, "mt") for _ in range(3)]
        xts = [C.sb(st, [128, D], F32, "xt") for _ in range(2)]
        hf_, hf_b = C.sb(st, [128, D], F32, "hf")
        hb, hb_b = C.sb(st, [128, D], BF16, "hb")
        sq, sq_b = C.sb(st, [128, D], BF16, "sqj")
        hT, hT_b = C.sb(st, [128, 8, 128], F32, "hT32")
        sv, sv_b = C.sb(st, [128, 8], F32, "sv")
        lg, lg_b = C.sb(st, [128, 16], F32, "lg")
        af, af_b = C.sb(st, [128, 16], F32, "af")
        aT, aT_b = C.sb(st, [16, 128], F32, "aT")
        pm = [C.ps(st, [128, 512], F32, "pm") for _ in range(3)]
        px = [C.ps(st, [128, 512], F32, "px") for _ in range(2)]
        ptr, ptr_b = C.ps(st, [128, 8, 128], F32, "ptr")
        psm, psm_b = C.ps(st, [128, 512], F32, "psm")
        it = 0
        for g in range(NG):
            tsl = slice(g * 512, (g + 1) * 512)
            for b in range(3):
                C.dma((SP, ACT, SP)[b], ys[b][0][:], yT[b][0][:, tsl].rearrange("(kc p) t -> p kc t", p=128), [yT[b][1]], [ys[b][1]])
            C.dma(ACT, gt[:], pgate[0][:, tsl].rearrange("(c p) t -> p c t", p=128), [pgate[1]], [gt_b])
            for dc in range(8):
                for b in range(3):
                    ps, ps_b = pm[b]
                    for kc in range(4):
                        _mm(C, ps[:], Wb[b][0][:, kc, dc * 128:(dc + 1) * 128], ys[b][0][:, kc, :], [Wb[b][1], ys[b][1]], [ps_b],
                            start=(kc == 0), stop=(kc == 3))
                    _tt(C, DVE, tmps[b][0][:], ps[:], gt[:, b * 8 + dc, :], ALU.mult, [ps_b, gt_b], [tmps[b][1]])
                _tt(C, POOL, tmps[0][0][:], tmps[0][0][:], tmps[1][0][:], ALU.add, [tmps[0][1], tmps[1][1]], [tmps[0][1]])
                _tt(C, POOL, mg[:, dc, :], tmps[0][0][:], tmps[2][0][:], ALU.add, [tmps[0][1], tmps[2][1]], [mg_b])
            for s in range(4):
                xt, xt_b = xts[it % 2]
                it += 1
                r0 = g * 512 + s * 128
                C.dma(SP, xt[:], x_in[0][r0:r0 + 128, :], [x_in[1]], [xt_b])
                for half in range(2):
                    ps, ps_b = px[half]
                    for kc in range(8):
                        _mm(C, ps[:], mg[:, kc, s * 128:(s + 1) * 128], Wo[:, kc, half * 512:(half + 1) * 512], [mg_b, Wo_b], [ps_b],
                            start=(kc == 0), stop=(kc == 7))
                    _tt(C, DVE, xt[:, half * 512:(half + 1) * 512], ps[:], xt[:, half * 512:(half + 1) * 512], ALU.add,
                        [ps_b, xt_b], [xt_b])
                C.dma(ACT, x_tok[0][r0:r0 + 128, :], xt[:], [xt_b], [x_tok[1]])
                _act(C, AF.Square, sq[:], xt[:], [xt_b], [sq_b, sv_b], accum_out=sv[:, 0:1])
                _act(C, AF.Sqrt, sv[:, 1:2], sv[:, 0:1], [sv_b], [sv_b], scale=1.0 / D, bias=EPS)
                C.op(DVE, lambda e: e.reciprocal(out=sv[:, 1:2], in_=sv[:, 1:2]), [sv_b], [sv_b])
                _stt(C, DVE, hf_[:], xt[:], sv[:, 1:2], gb[:], ALU.mult, ALU.mult, [xt_b, sv_b, gb_b], [hf_b])
                _cp(C, POOL, hb[:], hf_[:], [hf_b], [hb_b])
                C.dma(SP, h_tok[0][r0:r0 + 128, :], hb[:], [hb_b], [h_tok[1]])
                for kc in range(8):
                    C.op(PE, lambda e, kc=kc: e.transpose(out=ptr[:, kc, :], in_=hf_[:, kc * 128:(kc + 1) * 128], identity=identf[:]),
                         [hf_b, identf_b], [ptr_b])
                _cp(C, ACT, hT[:], ptr[:], [ptr_b], [hT_b])
                for kc in range(8):
                    _mm(C, psm[:, 0:16], hT[:, kc, :], rt[:, kc, :], [hT_b, rt_b], [psm_b], start=(kc == 0), stop=(kc == 7))
                _cp(C, DVE, lg[:], psm[:, 0:16], [psm_b], [lg_b])
                C.op(DVE, lambda e: e.tensor_reduce(out=sv[:, 2:3], in_=lg[:], axis=AX.X, op=ALU.max), [lg_b], [sv_b])
                _ts(C, DVE, sv[:, 2:3], sv[:, 2:3], -1.0, None, ALU.mult, None, [sv_b], [sv_b])
                _act(C, AF.Exp, af[:], lg[:], [lg_b, sv_b], [af_b, sv_b], bias=sv[:, 2:3], accum_out=sv[:, 3:4])
                C.op(DVE, lambda e: e.reciprocal(out=sv[:, 4:5], in_=sv[:, 3:4]), [sv_b], [sv_b])
                _ts(C, DVE, af[:], af[:], sv[:, 4:5], None, ALU.mult, None, [af_b, sv_b], [af_b])
                C.dma(SP, aff_tok[0][r0:r0 + 128, :], af[:], [af_b], [aff_tok[1]])
                C.op(PE, lambda e: e.transpose(out=psm[0:16, 128:256], in_=af[:], identity=identf[:]), [af_b, identf_b], [psm_b])
                _cp(C, ACT, aT[:], psm[0:16, 128:256], [psm_b], [aT_b])
                C.dma(ACT, affT[0][:, r0:r0 + 128], aT[:], [aT_b], [affT[1]])
    C.P.barrier()


def stage_moe(C, x_tok, h_tok, aff_tok, affT, wg_d, wu_d, wd_d, mc, msk_d, lst_d):
    CAP = 1024
    NE = 16
    with ExitStack() as st:
        thrb, thrb_b = C.sb(st, [128, 16], F32, "thrb")
        with ExitStack() as s1:
            aT, aT_b = C.sb(s1, [16, S], F32, "aT")
            jk, jk_b = C.sb(s1, [16, S], F32, "jk")
            C.dma(SP, aT[:], affT[0], [affT[1]], [aT_b])
            sv, sv_b = C.sb(s1, [16, 8], F32, "bs")
            id16, id16_b = C.sb(s1, [16, 16], F32, "id16")
            dg, dg_b = C.sb(s1, [16, 16], F32, "dg")
            on16, on16_b = C.sb(s1, [16, 128], F32, "on16")
            pb, pb_b = C.ps(s1, [128, 16], F32, "pb")
            C.dma(ACT, id16[:], msk_d[0][0:16, 4, 0:16], [msk_d[1]], [id16_b])
            C.op(POOL, lambda e: e.memset(on16[:], 1.0), [], [on16_b])
            C.op(DVE, lambda e: e.memset(sv[:, 0:1], 0.0), [], [sv_b])
            C.op(DVE, lambda e: e.memset(sv[:, 1:2], 1.0), [sv_b], [sv_b])
            for itn in range(34):
                _tt(C, DVE, sv[:, 2:3], sv[:, 0:1], sv[:, 1:2], ALU.add, [sv_b], [sv_b])
                _ts(C, DVE, sv[:, 2:3], sv[:, 2:3], 0.5, None, ALU.mult, None, [sv_b], [sv_b])
                _ts(C, DVE, jk[:], aT[:], sv[:, 2:3], None, ALU.is_ge, None, [aT_b, sv_b], [jk_b])
                C.op(DVE, lambda e: e.tensor_reduce(out=sv[:, 3:4], in_=jk[:], axis=AX.X, op=ALU.add), [jk_b], [sv_b])
                _ts(C, DVE, sv[:, 4:5], sv[:, 3:4], CAP - 0.5, None, ALU.is_ge, None, [sv_b], [sv_b])
                _tt(C, DVE, sv[:, 5:6], sv[:, 2:3], sv[:, 0:1], ALU.subtract, [sv_b], [sv_b])
                _tt(C, DVE, sv[:, 6:7], sv[:, 1:2], sv[:, 2:3], ALU.subtract, [sv_b], [sv_b])
                _stt(C, DVE, sv[:, 0:1], sv[:, 5:6], sv[:, 4:5], sv[:, 0:1], ALU.mult, ALU.add, [sv_b], [sv_b])
                _stt(C, DVE, sv[:, 1:2], sv[:, 6:7], sv[:, 4:5], sv[:, 2:3], ALU.mult, ALU.add, [sv_b], [sv_b])
            _ts(C, DVE, dg[:], id16[:], sv[:, 0:1], None, ALU.mult, None, [id16_b, sv_b], [dg_b])
            _mm(C, pb[:], on16[:], dg[:], [on16_b, dg_b], [pb_b])
            _cp(C, DVE, thrb[:], pb[:], [pb_b], [thrb_b])
        C.P.barrier()
        af, af_b = C.sb(st, [128, 64, 16], F32, "af")
        C.dma(SP, af[:], aff_tok[0].rearrange("(i p) e -> p i e", p=128), [aff_tok[1]], [af_b])
        posi, posi_b = C.sb(st, [128, 64, 16], I32, "posi")
        srcf, src_b = C.sb(st, [128, 64 * 16 * 2 + 16], F32, "src")
        src = srcf[:, 0:2048].rearrange("p (i e t) -> p i e t", e=16, t=2)
        C.op(POOL, lambda e: e.memset(srcf[:, 2048:2064], 0.0), [], [src_b])
        identb, identb_b = C.sb(st, [128, 128], BF16, "identb")
        C.op(POOL, lambda e: e.memset(identb[:], 0.0), [], [identb_b])
        C.op(POOL, lambda e: e.affine_select(out=identb[:], in_=identb[:], pattern=[[-1, 128]], compare_op=ALU.not_equal,
                                             fill=1.0, base=0, channel_multiplier=1), [identb_b], [identb_b])
        with ExitStack() as s2:
            mk, mk_b = C.sb(s2, [128, 64, 16], F32, "mk")
            posm, posm_b = C.sb(s2, [128, 64, 16], F32, "posm")
            tri, tri_b = C.sb(s2, [128, 128], F32, "tri")
            ones, ones_b = C.sb(s2, [128, 128], F32, "ones")
            tb, tb_b = C.sb(s2, [128, 66], F32, "tb")
            eo, eo_b = C.sb(s2, [128, 16], F32, "eo")
            totT, totT_b = C.sb(s2, [128, 16, 64], F32, "totT")
            inc, inc_b = C.sb(s2, [128, 16, 64], F32, "inc")
            rm2, rm2_b = C.sb(s2, [128, 16, 64], F32, "rm2")
            pw_ = [C.ps(s2, [128, 512], F32, "pw") for _ in range(2)]
            pt_ = [C.ps(s2, [128, 512], F32, "ptt") for _ in range(2)]
            C.dma(SP, tri[:], mc["tri"][0], [mc["tri"][1]], [tri_b])
            C.dma(SP, tb[:, 0:65], mc["tbase"][0], [mc["tbase"][1]], [tb_b])
            C.dma(SP, eo[:], mc["eoff"][0], [mc["eoff"][1]], [eo_b])
            _ts(C, DVE, tb[:, 65:66], tb[:, 64:65], 2048.0, None, ALU.add, None, [tb_b], [tb_b])
            C.op(POOL, lambda e: e.memset(ones[:], 1.0), [], [ones_b])
            C.op(POOL, lambda e: e.memset(rm2[:], 1.0), [], [rm2_b])
            C.op(POOL, lambda e: e.memset(rm2[:, :, 0:1], 0.0), [rm2_b], [rm2_b])
            _tt(C, DVE, mk[:], af[:], thrb[:].unsqueeze(1).to_broadcast([128, 64, 16]), ALU.is_ge, [af_b, thrb_b], [mk_b])
            mkf = mk[:].rearrange("p i e -> p (i e)")
            for half in range(2):
                _mm(C, pw_[half][0][:], tri[:], mkf[:, half * 512:(half + 1) * 512], [tri_b, mk_b], [pw_[half][1]])
                _mm(C, pt_[half][0][:], ones[:], mkf[:, half * 512:(half + 1) * 512], [ones_b, mk_b], [pt_[half][1]])
                _cp(C, DVE, totT[:].rearrange("p e i -> p i e")[:, half * 32:(half + 1) * 32, :],
                    pt_[half][0][:].rearrange("p (i e) -> p i e", e=16), [pt_[half][1]], [totT_b])
            C.op(DVE, lambda e: e.tensor_tensor_scan(out=inc[:].rearrange("p e i -> p (e i)"), data0=rm2[:].rearrange("p e i -> p (e i)"),
                                                     data1=totT[:].rearrange("p e i -> p (e i)"), initial=0.0, op0=ALU.mult, op1=ALU.add),
                 [rm2_b, totT_b], [inc_b])
            _tt(C, DVE, inc[:], inc[:], totT[:], ALU.subtract, [inc_b, totT_b], [inc_b])
            for half in range(2):
                _tt(C, DVE, posm[:, half * 32:(half + 1) * 32, :], pw_[half][0][:].rearrange("p (i e) -> p i e", e=16),
                    inc[:].rearrange("p e i -> p i e")[:, half * 32:(half + 1) * 32, :], ALU.add, [pw_[half][1], inc_b], [posm_b])
            _ts(C, DVE, posm[:], posm[:], tb[:, 65:66], None, ALU.subtract, None, [posm_b, tb_b], [posm_b])
            _tt(C, DVE, posm[:], posm[:], mk[:], ALU.mult, [posm_b, mk_b], [posm_b])
            _ts(C, DVE, posm[:], posm[:], tb[:, 65:66], None, ALU.add, None, [posm_b, tb_b], [posm_b])
            _tt(C, DVE, posm[:], posm[:], eo[:].unsqueeze(1).to_broadcast([128, 64, 16]), ALU.add, [posm_b, eo_b], [posm_b])
            _cp(C, DVE, posi[:], posm[:], [posm_b], [posi_b])
            _ts(C, POOL, src[:, :, :, 0], tb[:, 0:64].unsqueeze(2).to_broadcast([128, 64, 16]), tb[:, 64:65], None, ALU.add, None,
                [tb_b], [src_b])
            _cp(C, POOL, src[:, :, :, 1], af[:], [af_b], [src_b])
        C.P.barrier()
        sc_bufs = [[Buf("sc") for _ in range(64)] for _ in range(NE)]

        def scatter(e_):
            for i in range(64):
                _idma(C, lambda e, e_=e_, i=i: e.indirect_dma_start(
                    out=lst_d[0], out_offset=bass.IndirectOffsetOnAxis(ap=posi[:, i, e_:e_ + 1], axis=0),
                    in_=srcf[:, (i * 16 + e_) * 2:(i * 16 + e_) * 2 + 16], in_offset=None), [posi_b, src_b], [sc_bufs[e_][i]])
        civs = [C.sb(st, [128, 8, 2], F32, "civ") for _ in range(2)]
        idxs = [C.sb(st, [128, 8], I32, "idx") for _ in range(2)]
        xg, xg_b = C.sb(st, [128, 8, 1024], BF16, "xg")
        xgT, xgT_b = C.sb(st, [128, 8, 1024], BF16, "xgT")
        hid, hid_b = C.sb(st, [128, 8, 1024], BF16, "hid")
        Wgs = [C.sb(st, [128, 8, 1024], BF16, "Wg") for _ in range(2)]
        Wus = [C.sb(st, [128, 8, 1024], BF16, "Wu") for _ in range(2)]
        Wds = [C.sb(st, [128, 8, 1024], BF16, "Wd") for _ in range(2)]
        sgs = [C.sb(st, [128, 512], F32, "sg") for _ in range(2)]
        yvs = [C.sb(st, [128, 1024], F32, "yv") for _ in range(4)]
        pxt, pxt_b = C.ps(st, [128, 8, 128], BF16, "pxt")
        pgs = [C.ps(st, [128, 512], F32, "pg") for _ in range(2)]
        pus = [C.ps(st, [128, 512], F32, "pu") for _ in range(2)]
        pys = [C.ps(st, [128, 512], F32, "py") for _ in range(2)]
        kk = [0, 0]

        def load_w(W_, W_b, srcw, e_):
            for hf in range(2):
                C.dma(POOL, W_[:, hf * 4:(hf + 1) * 4, :],
                      srcw[0][e_, hf * 512:(hf + 1) * 512, :].rearrange("(kc p) n -> p kc n", p=128), [srcw[1]], [W_b], bulk=True)

        def gather(e_):
            civ, civ_b = civs[e_ % 2]
            idx, idx_b = idxs[e_ % 2]
            C.dma(SP, civ[:], lst_d[0][e_ * LSTN:e_ * LSTN + 1024, 0:2].rearrange("(cb p) two -> p cb two", p=128),
                  sc_bufs[e_], [civ_b])
            _cp(C, DVE, idx[:], civ[:, :, 0], [civ_b], [idx_b])
            for cb in range(8):
                _idma(C, lambda e, cb=cb, idx=idx: e.indirect_dma_start(
                    out=xg[:, cb, :], out_offset=None, in_=h_tok[0],
                    in_offset=bass.IndirectOffsetOnAxis(ap=idx[:, cb:cb + 1], axis=0)), [idx_b, h_tok[1]], [xg_b])

        def transposes():
            for cb in range(8):
                for kc in range(8):
                    C.op(PE, lambda e, cb=cb, kc=kc: e.transpose(out=pxt[:, kc, :], in_=xg[:, cb, kc * 128:(kc + 1) * 128],
                                                                 identity=identb[:]), [xg_b, identb_b], [pxt_b])
                _cp(C, (ACT, DVE)[cb % 2], xgT[:, :, cb * 128:(cb + 1) * 128], pxt[:], [pxt_b], [xgT_b])

        def load_all(e_):
            load_w(*Wgs[e_ % 2], wg_d, e_)
            load_w(*Wus[e_ % 2], wu_d, e_)
            load_w(*Wds[e_ % 2], wd_d, e_)

        load_all(0)
        scatter(0)
        scatter(1)
        scatter(2)
        gather(0)
        for e_ in range(NE):
            civ, civ_b = civs[e_ % 2]
            idx, idx_b = idxs[e_ % 2]
            Wg, Wg_b = Wgs[e_ % 2]
            Wu, Wu_b = Wus[e_ % 2]
            Wd, Wd_b = Wds[e_ % 2]
            if e_ + 1 < NE:
                load_all(e_ + 1)
            transposes()
            if e_ + 1 < NE:
                gather(e_ + 1)
            for fc in range(8):
                for half in range(2):
                    hsl = slice(half * 512, (half + 1) * 512)
                    pg, pg_b = pgs[kk[1] % 2]
                    pu, pu_b = pus[kk[1] % 2]
                    sg, sg_b = sgs[kk[1] % 2]
                    kk[1] += 1
                    for kc in range(8):
                        _mm(C, pg[:], Wg[:, kc, fc * 128:(fc + 1) * 128], xgT[:, kc, hsl], [Wg_b, xgT_b], [pg_b], start=(kc == 0), stop=(kc == 7))
                    for kc in range(8):
                        _mm(C, pu[:], Wu[:, kc, fc * 128:(fc + 1) * 128], xgT[:, kc, hsl], [Wu_b, xgT_b], [pu_b], start=(kc == 0), stop=(kc == 7))
                    _act(C, AF.Silu, sg[:], pg[:], [pg_b], [sg_b])
                    _tt(C, DVE, hid[:, fc, hsl], pu[:], sg[:], ALU.mult, [pu_b, sg_b], [hid_b])
            for cb in range(8):
                yv, yv_b = yvs[cb % 4]
                for half in range(2):
                    py, py_b = pys[half]
                    for fc in range(8):
                        _mm(C, py[:], hid[:, fc, cb * 128:(cb + 1) * 128], Wd[:, fc, half * 512:(half + 1) * 512], [hid_b, Wd_b], [py_b],
                            start=(fc == 0), stop=(fc == 7))
                    _ts(C, DVE, yv[:, half * 512:(half + 1) * 512], py[:], civ[:, cb, 1:2], None, ALU.mult, None,
                        [py_b, civ_b], [yv_b])
                _idma(C, lambda e, cb=cb, yv=yv, idx=idx: e.indirect_dma_start(
                    out=x_tok[0], out_offset=bass.IndirectOffsetOnAxis(ap=idx[:, cb:cb + 1], axis=0), in_=yv[:], in_offset=None,
                    compute_op=ALU.add), [idx_b, yv_b, x_tok[1]], [x_tok[1]])
            if e_ + 3 < NE:
                scatter(e_ + 3)
    C.P.barrier()


def _idma(C, fn, R, W):
    return C.P.add(POOL, fn, R, W, dma=True)


def stage_final(C, x_tok, nf_d, out_d):
    with ExitStack() as st:
        gb, gb_b = C.sb(st, [128, D], F32, "gbf")
        C.dma(SP, gb[:], nf_d[0].partition_broadcast(128), [nf_d[1]], [gb_b])
        xts = [C.sb(st, [128, D], F32, "xt") for _ in range(3)]
        sq, sq_b = C.sb(st, [128, D], BF16, "sqj")
        svs = [C.sb(st, [128, 2], F32, "sv") for _ in range(3)]
        for i in range(NT):
            xt, xt_b = xts[i % 3]
            sv, sv_b = svs[i % 3]
            C.dma(SP, xt[:], x_tok[0][i * 128:(i + 1) * 128, :], [x_tok[1]], [xt_b])
            _act(C, AF.Square, sq[:], xt[:], [xt_b], [sq_b, sv_b], accum_out=sv[:, 0:1])
            _act(C, AF.Sqrt, sv[:, 1:2], sv[:, 0:1], [sv_b], [sv_b], scale=1.0 / D, bias=EPS)
            C.op(DVE, lambda e, sv=sv: e.reciprocal(out=sv[:, 1:2], in_=sv[:, 1:2]), [sv_b], [sv_b])
            _stt(C, DVE, xt[:], xt[:], sv[:, 1:2], gb[:], ALU.mult, ALU.mult, [xt_b, sv_b, gb_b], [xt_b])
            C.dma(ACT, out_d[0][i * 128:(i + 1) * 128, :], xt[:], [xt_b], [out_d[1]])


W_NAMES = ("norm_mix", "w_in", "rwkv_w2", "rwkv_a2", "rwkv_g2", "vres_w1", "vres_w2", "pool_w", "q_norm", "k_norm",
           "w_branch_rwkv", "w_branch_pool", "w_branch_attn", "w_out", "norm_ffn", "router", "exp_gate", "exp_up", "exp_down",
           "norm_final")
W_SHAPES = {"norm_mix": [2, D], "w_in": [2, D, NPROJ], "rwkv_w2": [2, 2, 64, 512], "rwkv_a2": [2, 2, 64, 512],
            "rwkv_g2": [2, 128, 512], "vres_w1": [1, D, 32], "vres_w2": [1, 32, 512], "pool_w": [2, 4, 128, 128],
            "q_norm": [2, 64], "k_norm": [2, 64], "w_branch_rwkv": [2, 512, D], "w_branch_pool": [2, 512, D],
            "w_branch_attn": [2, 512, D], "w_out": [2, D, D], "norm_ffn": [2, D], "router": [2, D, 16],
            "exp_gate": [2, 16, D, D], "exp_up": [2, 16, D, D], "exp_down": [2, 16, D, D], "norm_final": [D]}


def build_full(nlayers=2, do_final=True):
    nc = bass.Bass("TRN2", target_bir_lowering=False)
    C = Ctx(nc)
    x = C.dram("x", [S, D], F32, kind="ExternalInput")
    Wd = {n: C.dram(n, W_SHAPES[n], F32, kind="ExternalInput") for n in W_NAMES}
    pars = [C.dram(f"par{l}", [128, PAR_COLS], F32, kind="ExternalInput") for l in range(2)]
    hc = host_consts()
    mcn = moe_consts()
    cst = {k: C.dram(k, list(v.shape), F32, kind="ExternalInput") for k, v in hc.items()}
    mc = {k: C.dram(k, list(v.shape), F32, kind="ExternalInput") for k, v in mcn.items()}
    msk = C.dram("msk", [64, 5, 64], F32, kind="ExternalInput")
    out = C.dram("out", [S, D], F32, kind="ExternalOutput")
    x_tok = C.dram("x_tok", [S, D], F32)
    outs = {"prw": C.dram("prw", [1920, S], F32), "ppool": C.dram("ppool", [512, S], F32), "patt": C.dram("patt", [768, S], F32),
            "pgate": C.dram("pgate", [3072, S], BF16), "hv1": C.dram("hv1", [32, S], F32)}
    RO = {n: C.dram("o_" + n, [512, S], F32) for n in RNAMES + ("vfirst",)}
    y_d = [C.dram(f"ydir{d}", [S, 512], F32) for d in range(2)]
    yT = [C.dram(n, [512, S], BF16) for n in ("yaT", "ybT", "ycT")]
    h_tok = C.dram("h_tok", [S, D], BF16)
    aff_tok = C.dram("aff_tok", [S, 16], F32)
    affT = C.dram("affT", [16, S], F32)
    lst_d = C.dram("ranklist", [16 * LSTN, 16], F32)

    def sub(d, *idx):
        ap = d[0]
        for i in idx:
            ap = ap[i]
        return (ap, d[1])

    for l in range(nlayers):
        x_src = x if l == 0 else x_tok
        stage_proj(C, l, x_src, sub(Wd["w_in"], l), sub(Wd["norm_mix"], l), outs, sub(Wd["vres_w1"], 0) if l == 1 else None)
        C.P.barrier()
        vres = None
        if l == 1:
            vres = {"w2": sub(Wd["vres_w2"], 0), "hv1": outs["hv1"], "vfirst": RO["vfirst"]}
        stage_rwkv_prep(C, l, outs["prw"], pars[l], sub(Wd["rwkv_w2"], l), sub(Wd["rwkv_a2"], l), sub(Wd["rwkv_g2"], l),
                        cst["blk"], RO, vres)
        stage_rwkv_scan(C, RO, msk, y_d)
        stage_rwkv_out(C, y_d, RO, pars[l], yT[0])
        stage_pool(C, outs["ppool"], sub(Wd["pool_w"], l), pars[l], mc["invcnt"], yT[1])
        stage_attn(C, outs["patt"], sub(Wd["q_norm"], l), sub(Wd["k_norm"], l), cst, yT[2])
        stage_merge(C, x_src, x_tok, yT, outs["pgate"],
                    [sub(Wd["w_branch_rwkv"], l), sub(Wd["w_branch_pool"], l), sub(Wd["w_branch_attn"], l)],
                    sub(Wd["w_out"], l), sub(Wd["norm_ffn"], l), sub(Wd["router"], l), h_tok, aff_tok, affT)
        stage_moe(C, x_tok, h_tok, aff_tok, affT, sub(Wd["exp_gate"], l), sub(Wd["exp_up"], l), sub(Wd["exp_down"], l), mc, msk, lst_d)
    if do_final:
        stage_final(C, x_tok, Wd["norm_final"], out)
    emit_program(C)
    consts = dict(hc)
    consts.update(mcn)
    consts["msk"] = scan_consts()
    return nc, consts


def make_in_maps(inputs, cores):
    inp = {k: np.asarray(v) for k, v in inputs.items()}
    shared = {n: np.ascontiguousarray(inp[n], dtype=np.float32) for n in W_NAMES}
    shared["par0"] = pack_par(inp, 0)
    shared["par1"] = pack_par(inp, 1)
    maps = []
    for b in cores:
        m = dict(shared)
        m["x"] = np.ascontiguousarray(inp["x"][b], dtype=np.float32)
        maps.append(m)
    return maps


def kernel(**inputs):
    nc, consts = build_full()
    maps = make_in_maps(inputs, list(range(8)))
    for m in maps:
        m.update(consts)
    res = run_bass_kernel_spmd(nc, maps, core_ids=list(range(8)))
    return np.stack([np.asarray(r["out"], dtype=np.float32) for r in res.results], axis=0)
```

```python
import numpy as np
import ml_dtypes
import concourse.bass as bass
import concourse.mybir as mybir
from concourse.bass_utils import run_bass_kernel_spmd

F32 = mybir.dt.float32
BF16 = mybir.dt.bfloat16
I32 = mybir.dt.int32
U32 = mybir.dt.uint32
AF = mybir.ActivationFunctionType
ALU = mybir.AluOpType
AX = mybir.AxisListType

D = 1024
S = 8192
NT = S // 128
NG = S // 512
NPROJ = 6272
RW = 512
EPS = 1e-6

PE, ACT, DVE, POOL, SP = "pe", "act", "dve", "pool", "sp"
ENGS = (PE, ACT, DVE, POOL, SP)
NDMASEM = {"pe": 1, "act": 6, "dve": 1, "pool": 24, "sp": 8}
NRING2 = 6


class Buf:
    __slots__ = ("name", "w", "rd")

    def __init__(self, name=""):
        self.name = name
        self.w = None
        self.rd = []


class Op:
    __slots__ = ("eng", "fn", "dma", "deps", "needs_inc", "count", "slot", "slotcount", "idx")


class Prog:
    def __init__(self, nc):
        self.nc = nc
        self.ops = {e: [] for e in ENGS}
        self.dma_rr = {e: 0 for e in ENGS}
        self.dma_rr2 = {}
        self.dma_last = {}
        self.dma_cnt = {}
        self.nops = 0
        self.pending = {e: [] for e in ENGS}

    def barrier(self):
        deps = []
        for e in ENGS:
            for op in reversed(self.ops[e]):
                if not op.dma:
                    deps.append(op)
                    break
        deps.extend(self.dma_last.values())
        for e in ENGS:
            self.pending[e] = list(deps)

    def add(self, eng, fn, reads=(), writes=(), dma=False, bulk=False):
        op = Op()
        op.eng, op.fn, op.dma = eng, fn, dma
        op.needs_inc = False
        op.count = 0
        op.idx = self.nops
        self.nops += 1
        deps = []
        for b in reads:
            if b.w is not None:
                for w_ in b.w:
                    deps.append((w_, "raw"))
        for b in writes:
            if b.w is not None:
                for w_ in b.w:
                    deps.append((w_, "waw"))
            for r in b.rd:
                deps.append((r, "war"))
        for d in self.pending[eng]:
            deps.append((d, "bar"))
        self.pending[eng] = []
        if dma:
            if bulk:
                k2 = self.dma_rr2.get(eng, 0)
                self.dma_rr2[eng] = (k2 + 1) % NRING2
                k = NDMASEM[eng] + k2
            else:
                k = self.dma_rr[eng]
                self.dma_rr[eng] = (k + 1) % NDMASEM[eng]
            op.slot = k
            prev = self.dma_last.get((eng, k))
            if prev is not None:
                deps.append((prev, "slot"))
            self.dma_last[(eng, k)] = op
            c = self.dma_cnt.get((eng, k), 0) + 16
            self.dma_cnt[(eng, k)] = c
            op.slotcount = c
        fin = []
        for d, kind in deps:
            if d is op:
                continue
            if not d.dma and d.eng == eng:
                if eng == PE:
                    continue
                if kind == "bar" and not dma:
                    continue
            if not d.dma:
                d.needs_inc = True
            fin.append(d)
        op.deps = fin
        for b in reads:
            b.rd.append(op)
        for b in writes:
            if dma and b.w is not None and all(w_.dma for w_ in b.w):
                keep = [w_ for w_ in b.w if not (w_.eng == eng and w_.slot == op.slot)]
                b.w = keep + [op]
            else:
                b.w = [op]
            b.rd = []
        self.ops[eng].append(op)
        return op

    def emit(self, sems, dsems, engs):
        for e in ENGS:
            c = 0
            for op in self.ops[e]:
                if not op.dma and op.needs_inc:
                    c += 1
                    op.count = c
        for e in ENGS:
            eng = engs[e]
            waited = {}
            for op in self.ops[e]:
                need = {}
                for d in op.deps:
                    if d.dma:
                        key = ("d", d.eng, d.slot)
                        v = d.slotcount
                    else:
                        key = ("c", d.eng)
                        v = d.count
                    if need.get(key, 0) < v:
                        need[key] = v
                for key, v in need.items():
                    if waited.get(key, 0) >= v:
                        continue
                    waited[key] = v
                    sem = dsems[key[1]][key[2]] if key[0] == "d" else sems[key[1]]
                    eng.wait_ge(sem, v)
                ins = op.fn(eng)
                if op.dma:
                    ins.then_inc(dsems[e][op.slot], 16)
                elif op.needs_inc:
                    ins.then_inc(sems[e], 1)

    def final_wait(self, sems, dsems, engs):
        eng = engs[SP]
        for (e, k), c in self.dma_cnt.items():
            eng.wait_ge(dsems[e][k], c)


class Ctx:
    def __init__(self, nc):
        self.nc = nc
        self.P = Prog(nc)
        self.stack = None
        self.uid = 0

    def name(self, base):
        self.uid += 1
        return f"{base}_{self.uid}"

    def sb(self, st, shape, dt, name="t"):
        t = st.enter_context(self.nc.sbuf_tensor(self.name(name), list(shape), dt))
        return t, Buf(name)

    def ps(self, st, shape, dt, name="p"):
        t = st.enter_context(self.nc.psum_tensor(self.name(name), list(shape), dt))
        return t, Buf(name)

    def dram(self, name, shape, dt, kind="Internal"):
        return self.nc.dram_tensor(name, list(shape), dt, kind=kind).ap(), Buf(name)

    def dma(self, eng, out, in_, reads, writes, bulk=False, **kw):
        def fn(e):
            return e.dma_start(out=out, in_=in_, **kw)
        return self.P.add(eng, fn, reads, writes, dma=True, bulk=bulk and eng == POOL)

    def op(self, eng, fn, reads, writes):
        return self.P.add(eng, fn, reads, writes)


from contextlib import ExitStack


def act(func, out, in_, **kw):
    return lambda e: e.activation(out=out, in_=in_, func=func, **kw)


def stage_proj(C, l, x_tok, w_in_d, norm_d, outs, vres_w1_d=None):
    nc = C.nc
    ncols = NPROJ + (32 if vres_w1_d is not None else 0)
    nchunk = (ncols + 127) // 128
    with ExitStack() as st:
        w_sb, w_b = C.sb(st, [128, 8, ncols], BF16, "w_in")
        gb, gb_b = C.sb(st, [128, D], F32, "gbc")
        C.dma(SP, gb[:], norm_d[0].partition_broadcast(128), [norm_d[1]], [gb_b])
        WP = 784
        stg = [C.sb(st, [128, 8, WP], F32, "wstg") for _ in range(2)]
        wv = w_in_d[0].rearrange("(kc p) n -> p kc n", p=128)
        k = 0
        for c0 in range(0, NPROJ, WP):
            t, tb = stg[k % 2]
            C.dma(ACT if k % 2 else SP, t[:], wv[:, :, c0:c0 + WP], [w_in_d[1]], [tb])
            C.op(POOL, lambda e, t=t, c0=c0: e.tensor_copy(out=w_sb[:, :, c0:c0 + WP], in_=t[:]), [tb], [w_b])
            k += 1
        if vres_w1_d is not None:
            t, tb = stg[k % 2]
            C.dma(SP, t[:, :, 0:32], vres_w1_d[0].rearrange("(kc p) n -> p kc n", p=128), [vres_w1_d[1]], [tb])
            C.op(POOL, lambda e, t=t: e.tensor_copy(out=w_sb[:, :, NPROJ:NPROJ + 32], in_=t[:, :, 0:32]), [tb], [w_b])
        ident, ident_b = C.sb(st, [128, 128], BF16, "ident")
        C.op(POOL, lambda e: e.memset(ident[:], 0.0), [], [ident_b])
        C.op(POOL, lambda e: e.affine_select(out=ident[:], in_=ident[:], pattern=[[-1, 128]], compare_op=ALU.not_equal,
                                             fill=1.0, base=0, channel_multiplier=1), [ident_b], [ident_b])
        xts = [C.sb(st, [128, D], F32, "xt") for _ in range(2)]
        hns = [C.sb(st, [128, D], BF16, "hn") for _ in range(2)]
        sq, sq_b = C.sb(st, [128, D], BF16, "sqj")
        sts = [C.sb(st, [128, 2], F32, "stat") for _ in range(2)]
        hnT = [C.sb(st, [128, 8, 512], BF16, "hnT") for _ in range(2)]
        tps = [C.ps(st, [128, 8, 128], BF16, "tps") for _ in range(2)]
        mps = [C.ps(st, [128, 512], F32, "mps") for _ in range(4)]
        ost = [C.sb(st, [128, 512], F32, "ost") for _ in range(4)]
        osb = [C.sb(st, [128, 512], BF16, "osb") for _ in range(4)]
        it = 0
        oc = 0
        for g in range(NG):
            hT, hT_b = hnT[g % 2]
            for s in range(4):
                xt, xt_b = xts[it % 2]
                hn, hn_b = hns[it % 2]
                stt, st_b = sts[it % 2]
                tp, tp_b = tps[it % 2]
                it += 1
                r0 = g * 512 + s * 128
                C.dma(SP, xt[:], x_tok[0][r0:r0 + 128, :], [x_tok[1]], [xt_b])
                C.op(ACT, act(AF.Square, sq[:], xt[:], accum_out=stt[:, 0:1]), [xt_b], [sq_b, st_b])
                C.op(ACT, act(AF.Sqrt, stt[:, 1:2], stt[:, 0:1], scale=1.0 / D, bias=EPS), [st_b], [st_b])
                C.op(DVE, lambda e, stt=stt: e.reciprocal(out=stt[:, 1:2], in_=stt[:, 1:2]), [st_b], [st_b])
                C.op(DVE, lambda e, stt=stt, xt=xt, hn=hn: e.scalar_tensor_tensor(
                    out=hn[:], in0=xt[:], scalar=stt[:, 1:2], in1=gb[:], op0=ALU.mult, op1=ALU.mult),
                    [st_b, xt_b, gb_b], [hn_b])
                for kc in range(8):
                    C.op(PE, lambda e, tp=tp, hn=hn, kc=kc: e.transpose(out=tp[:, kc, :], in_=hn[:, kc * 128:(kc + 1) * 128],
                                                                       identity=ident[:]), [hn_b, ident_b], [tp_b])
                C.op(ACT, lambda e, tp=tp, hT=hT, s=s: e.copy(out=hT[:, :, s * 128:(s + 1) * 128], in_=tp[:]), [tp_b], [hT_b])
            for c in range(nchunk):
                m = min(128, ncols - c * 128)
                mp, mp_b = mps[oc % 4]
                for kc in range(8):
                    C.op(PE, lambda e, mp=mp, c=c, kc=kc, hT=hT, m=m: e.matmul(
                        out=mp[0:m, :], lhsT=w_sb[:, kc, c * 128:c * 128 + m], rhs=hT[:, kc, :],
                        start=(kc == 0), stop=(kc == 7)), [w_b, hT_b], [mp_b])
                col = c * 128
                tsl = slice(g * 512, (g + 1) * 512)
                if col < 1920:
                    dst, off = outs["prw"], col
                elif col < 2432:
                    dst, off = outs["ppool"], col - 1920
                elif col < 3200:
                    dst, off = outs["patt"], col - 2432
                elif col < NPROJ:
                    dst, off = outs["pgate"], col - 3200
                else:
                    dst, off = outs["hv1"], 0
                if dst is outs["pgate"]:
                    o, o_b = osb[oc % 4]
                    C.op(ACT, act(AF.Sigmoid, o[:], mp[:]), [mp_b], [o_b])
                else:
                    o, o_b = ost[oc % 4]
                    C.op(DVE, lambda e, o=o, mp=mp, m=m: e.tensor_copy(out=o[0:m, :], in_=mp[0:m, :]), [mp_b], [o_b])
                C.dma(SP if oc % 2 else POOL, dst[0][off:off + m, tsl], o[0:m, :], [o_b], [dst[1]])
                oc += 1


def emit_program(C):
    nc = C.nc
    with ExitStack() as st:
        sems = {e: st.enter_context(nc.semaphore(f"s_{e}")) for e in ENGS}
        dsems = {e: [st.enter_context(nc.semaphore(f"d_{e}{k}")) for k in range(NDMASEM[e] + (NRING2 if e == POOL else 0))] for e in ENGS}
        engs = {PE: nc.tensor, ACT: nc.scalar, DVE: nc.vector, POOL: nc.gpsimd, SP: nc.sync}
        C.P.emit(sems, dsems, engs)
        C.P.final_wait(sems, dsems, engs)


def host_consts():
    t = np.arange(S)
    row = (t // 64).astype(np.float32)
    col = (t % 64).astype(np.float32)
    freqs = (10000.0 ** (-np.arange(0, 32, 2, dtype=np.float32) / 32)).astype(np.float32)
    ang = np.concatenate([row[:, None] * freqs, col[:, None] * freqs], axis=-1).astype(np.float32)
    cos = np.cos(ang).astype(np.float32)
    sin = np.sin(ang).astype(np.float32)
    pidx = (np.arange(128) % 64) // 2
    ctab = np.ascontiguousarray(cos[:, pidx].T)
    stab = np.ascontiguousarray(sin[:, pidx].T)
    prot = np.zeros((128, 128), np.float32)
    for i in range(64):
        prot[2 * i + 1, 2 * i] = -1.0
        prot[2 * i, 2 * i + 1] = 1.0
    blk = np.zeros((128, 128), np.float32)
    blk[:64, :64] = 1.0
    blk[64:, 64:] = 1.0
    return {"ctab": ctab, "stab": stab, "prot": prot, "blk": blk}


def stage_attn(C, patt, qn_d, kn_d, cst, ycT):
    with ExitStack() as st:
        qT, qT_b = C.sb(st, [128, 4, S], BF16, "qT")
        kT2, kT_b = C.sb(st, [128, 2, 2, S], BF16, "kTz")
        Vx, Vx_b = C.sb(st, [128, NT, 2, 65], BF16, "Vx")
        ones, ones_b = C.sb(st, [128, 128], F32, "ones")
        C.op(POOL, lambda e: e.memset(ones[:], 1.0), [], [ones_b])
        C.op(POOL, lambda e: e.memset(Vx[:, :, :, 0:1], 1.0), [], [Vx_b])
        C.op(POOL, lambda e: e.memset(kT2[:], 0.0), [], [kT_b])
        with ExitStack() as s1:
            blk, blk_b = C.sb(s1, [128, 128], F32, "blk")
            prot, prot_b = C.sb(s1, [128, 128], F32, "prot")
            identf, identf_b = C.sb(s1, [128, 128], F32, "identf")
            gq, gq_b = C.sb(s1, [128, 2], F32, "gqk")
            C.dma(SP, blk[:], cst["blk"][0], [cst["blk"][1]], [blk_b])
            C.dma(SP, prot[:], cst["prot"][0], [cst["prot"][1]], [prot_b])
            for hh in range(2):
                C.dma(SP, gq[hh * 64:(hh + 1) * 64, 0:1], qn_d[0].rearrange("(p o) -> p o", o=1), [qn_d[1]], [gq_b])
                C.dma(SP, gq[hh * 64:(hh + 1) * 64, 1:2], kn_d[0].rearrange("(p o) -> p o", o=1), [kn_d[1]], [gq_b])
            C.op(POOL, lambda e: e.memset(identf[:], 0.0), [], [identf_b])
            C.op(POOL, lambda e: e.affine_select(out=identf[:], in_=identf[:], pattern=[[-1, 128]], compare_op=ALU.not_equal,
                                                 fill=1.0, base=0, channel_multiplier=1), [identf_b], [identf_b])
            qcs = [C.sb(s1, [128, 512], F32, "qc") for _ in range(2)]
            ctb = [C.sb(s1, [128, 2, 512], F32, "cs") for _ in range(2)]
            sq, sq_b = C.sb(s1, [128, 512], F32, "sq")
            rs, rs_b = C.sb(s1, [128, 512], F32, "rs")
            qn, qn_b = C.sb(s1, [128, 512], F32, "qn")
            o1, o1_b = C.sb(s1, [128, 512], F32, "o1")
            o2, o2_b = C.sb(s1, [128, 512], F32, "o2")
            ssp = [C.ps(s1, [128, 512], F32, "ssp") for _ in range(2)]
            rtp = [C.ps(s1, [128, 512], F32, "rtp") for _ in range(2)]
            vtp = [C.ps(s1, [128, 128], F32, "vtp") for _ in range(2)]
            it = 0
            vi = 0
            for g in range(NG):
                tsl = slice(g * 512, (g + 1) * 512)
                cs, cs_b = ctb[g % 2]
                C.dma(SP, cs[:, 0, :], cst["ctab"][0][:, tsl], [cst["ctab"][1]], [cs_b])
                C.dma(SP, cs[:, 1, :], cst["stab"][0][:, tsl], [cst["stab"][1]], [cs_b])
                for ch in range(6):
                    qc, qc_b = qcs[it % 2]
                    sp_, sp_b = ssp[it % 2]
                    rp, rp_b = rtp[it % 2]
                    it += 1
                    if ch < 4:
                        C.dma(ACT, qc[:], patt[0][ch * 128:(ch + 1) * 128, tsl], [patt[1]], [qc_b])
                        gcol = gq[:, 0:1]
                        dst = qT[:, ch, tsl]
                        dst_b = qT_b
                    else:
                        r0 = 512 + (ch - 4) * 64
                        C.dma(ACT, qc[0:64, :], patt[0][r0:r0 + 64, tsl], [patt[1]], [qc_b])
                        C.dma(ACT, qc[64:128, :], patt[0][r0:r0 + 64, tsl], [patt[1]], [qc_b])
                        gcol = gq[:, 1:2]
                        dst = None
                        dst_b = kT_b
                    C.op(ACT, act(AF.Square, sq[:], qc[:]), [qc_b], [sq_b])
                    C.op(PE, lambda e, sp_=sp_: e.matmul(out=sp_[:], lhsT=blk[:], rhs=sq[:], start=True, stop=True),
                         [blk_b, sq_b], [sp_b])
                    C.op(ACT, act(AF.Sqrt, rs[:], sp_[:], scale=1.0 / 64, bias=EPS), [sp_b], [rs_b])
                    C.op(DVE, lambda e: e.reciprocal(out=rs[:], in_=rs[:]), [rs_b], [rs_b])
                    C.op(DVE, lambda e, qc=qc, gcol=gcol: e.scalar_tensor_tensor(out=qn[:], in0=qc[:], scalar=gcol, in1=rs[:],
                                                                                 op0=ALU.mult, op1=ALU.mult),
                         [qc_b, gq_b, rs_b], [qn_b])
                    C.op(PE, lambda e, rp=rp: e.matmul(out=rp[:], lhsT=prot[:], rhs=qn[:], start=True, stop=True),
                         [prot_b, qn_b], [rp_b])
                    C.op(POOL, lambda e, cs=cs: e.tensor_tensor(out=o1[:], in0=qn[:], in1=cs[:, 0, :], op=ALU.mult),
                         [qn_b, cs_b], [o1_b])
                    C.op(DVE, lambda e, cs=cs, rp=rp: e.tensor_tensor(out=o2[:], in0=rp[:], in1=cs[:, 1, :], op=ALU.mult),
                         [rp_b, cs_b], [o2_b])
                    if dst is not None:
                        C.op(POOL, lambda e, dst=dst: e.tensor_tensor(out=dst, in0=o1[:], in1=o2[:], op=ALU.add),
                             [o1_b, o2_b], [dst_b])
                    else:
                        for v_ in range(2):
                            psl = slice(v_ * 64, (v_ + 1) * 64)
                            _tt(C, POOL, kT2[psl, ch - 4, v_, tsl], o1[psl, :], o2[psl, :], ALU.add, [o1_b, o2_b], [dst_b])
                qc, qc_b = qcs[it % 2]
                it += 1
                C.dma(ACT, qc[:], patt[0][640:768, tsl], [patt[1]], [qc_b])
                for s in range(4):
                    vp, vp_b = vtp[vi % 2]
                    vi += 1
                    C.op(PE, lambda e, vp=vp, qc=qc, s=s: e.transpose(out=vp[:], in_=qc[:, s * 128:(s + 1) * 128],
                                                                      identity=identf[:]), [qc_b, identf_b], [vp_b])
                    ti = g * 4 + s
                    C.op(DVE, lambda e, vp=vp, ti=ti: e.tensor_copy(
                        out=Vx[:, ti, :, 1:65], in_=vp[:].rearrange("p (k d) -> p k d", k=2)), [vp_b], [Vx_b])
        C.P.barrier()
        with ExitStack() as s2:
            sps = [C.ps(s2, [128, 1024], F32, "sps") for _ in range(2)]
            ots = [C.ps(s2, [128, 512], F32, "ot") for _ in range(2)]
            bcp, bcp_b = C.ps(s2, [128, 512], F32, "bcp")
            pts = [C.sb(s2, [128, 1024], BF16, "pt") for _ in range(2)]
            osbs = [C.sb(s2, [128, 512], F32, "osb") for _ in range(2)]
            rec, rec_b = C.sb(s2, [1, 512], F32, "rec")
            ysb = [C.sb(s2, [128, 512], BF16, "ysb") for _ in range(2)]
            oi = 0
            iters = [(h, q2, stl) for h in range(8) for q2 in range(8) for stl in range(NT)]

            def emit_S(n):
                h, q2, stl = iters[n]
                kvh, ch, base = h // 4, h // 2, (h % 2) * 64
                q0 = q2 * 1024
                sp_, sp_b = sps[n % 2]
                for half in range(2):
                    C.op(PE, lambda e, sp_=sp_, half=half, stl=stl, kvh=kvh, ch=ch, base=base, q0=q0: e.matmul(
                        out=sp_[:, half * 512:(half + 1) * 512],
                        lhsT=kT2[:, kvh, base // 64, stl * 128:(stl + 1) * 128],
                        rhs=qT[:, ch, q0 + half * 512:q0 + (half + 1) * 512],
                        start=True, stop=True), [kT_b, qT_b], [sp_b])

            emit_S(0)
            for n, (h, q2, stl) in enumerate(iters):
                kvh = h // 4
                q0 = q2 * 1024
                if n + 1 < len(iters):
                    emit_S(n + 1)
                sp_, sp_b = sps[n % 2]
                pt, pt_b = pts[n % 2]
                C.op(ACT, act(AF.Exp, pt[:], sp_[:], scale=0.125), [sp_b], [pt_b])
                for half in range(2):
                    ot, ot_b = ots[half]
                    C.op(PE, lambda e, ot=ot, pt=pt, half=half, stl=stl, kvh=kvh: e.matmul(
                        out=ot[0:65, :], lhsT=Vx[:, stl, kvh, :], rhs=pt[:, half * 512:(half + 1) * 512],
                        start=(stl == 0), stop=(stl == NT - 1)), [Vx_b, pt_b], [ot_b])
                if stl == NT - 1:
                    for half in range(2):
                        ot, ot_b = ots[half]
                        osb, osb_b = osbs[oi % 2]
                        y, y_b = ysb[oi % 2]
                        oi += 1
                        C.op(DVE, lambda e, osb=osb, ot=ot: e.tensor_copy(out=osb[0:65, :], in_=ot[0:65, :]), [ot_b], [osb_b])
                        C.op(DVE, lambda e, osb=osb: e.reciprocal(out=rec[:], in_=osb[0:1, :]), [osb_b], [rec_b])
                        C.op(PE, lambda e: e.matmul(out=bcp[0:65, :], lhsT=ones[0:1, 0:65], rhs=rec[:], start=True, stop=True),
                             [ones_b, rec_b], [bcp_b])
                        C.op(DVE, lambda e, y=y, osb=osb: e.tensor_tensor(out=y[0:65, :], in0=osb[0:65, :], in1=bcp[0:65, :],
                                                                          op=ALU.mult), [osb_b, bcp_b], [y_b])
                        c0 = q0 + half * 512
                        C.dma(SP, ycT[0][h * 64:(h + 1) * 64, c0:c0 + 512], y[1:65, :], [y_b], [ycT[1]])
        C.P.barrier()


def _tt(C, eng, out, in0, in1, op, R, W):
    return C.op(eng, lambda e: e.tensor_tensor(out=out, in0=in0, in1=in1, op=op), R, W)


def _ts(C, eng, out, in0, s1, s2, op0, op1, R, W):
    if s2 is None:
        return C.op(eng, lambda e: e.tensor_scalar(out=out, in0=in0, scalar1=s1, scalar2=None, op0=op0), R, W)
    return C.op(eng, lambda e: e.tensor_scalar(out=out, in0=in0, scalar1=s1, scalar2=s2, op0=op0, op1=op1), R, W)


def _stt(C, eng, out, in0, scalar, in1, op0, op1, R, W):
    return C.op(eng, lambda e: e.scalar_tensor_tensor(out=out, in0=in0, scalar=scalar, in1=in1, op0=op0, op1=op1), R, W)


def _act(C, func, out, in_, R, W, **kw):
    return C.op(ACT, lambda e: e.activation(out=out, in_=in_, func=func, **kw), R, W)


def _mm(C, out, lhsT, rhs, R, W, start=True, stop=True):
    return C.op(PE, lambda e: e.matmul(out=out, lhsT=lhsT, rhs=rhs, start=start, stop=stop), R, W)


def _cp(C, eng, out, in_, R, W):
    if eng == ACT:
        return C.op(ACT, lambda e: e.copy(out=out, in_=in_), R, W)
    return C.op(eng, lambda e: e.tensor_copy(out=out, in_=in_), R, W)


PAR_COLS = 74
LAM = 0.6065306597126334


def pack_par(inp, l):
    def pc(v):
        return np.asarray(v, np.float32).reshape(-1, 128).T
    cols = [pc(inp["shift_prev"][l]), pc(inp["shift_next"][l]), pc(inp["rwkv_k_k"][l]), pc(inp["rwkv_k_a"][l]),
            pc(inp["rwkv_r_k"][l]), pc(inp["rwkv_w0"][l][0]), pc(inp["rwkv_w0"][l][1]), pc(inp["rwkv_a0"][l][0]),
            pc(inp["rwkv_a0"][l][1]),
            pc(inp["vres_v0"][l - 1]) if l > 0 else np.zeros((128, 4), np.float32),
            pc(inp["rwkv_ln_w"][l]), pc(inp["rwkv_ln_b"][l]), pc(inp["pool_scale"][l])]
    return np.ascontiguousarray(np.concatenate(cols, axis=1))


def stage_rwkv_prep(C, l, prw, par_d, w2_d, a2_d, g2_d, blk_d, RO, vres=None):
    with ExitStack() as st:
        par, par_b = C.sb(st, [128, PAR_COLS], F32, "par")
        dv, dv_b = C.sb(st, [128, 19], F32, "dv")
        w2s, w2_b = C.sb(st, [128, 512], F32, "w2s")
        a2s, a2_b = C.sb(st, [128, 512], F32, "a2s")
        g2s, g2_b = C.sb(st, [128, 512], F32, "g2s")
        blk, blk_b = C.sb(st, [128, 128], F32, "blk")
        C.dma(SP, par[:], par_d[0], [par_d[1]], [par_b])
        C.dma(SP, w2s[:], w2_d[0].rearrange("d l c -> (d l) c"), [w2_d[1]], [w2_b])
        C.dma(SP, a2s[:], a2_d[0].rearrange("d l c -> (d l) c"), [a2_d[1]], [a2_b])
        C.dma(SP, g2s[:], g2_d[0], [g2_d[1]], [g2_b])
        C.dma(SP, blk[:], blk_d[0], [blk_d[1]], [blk_b])
        if vres is not None:
            vw2, vw2_b = C.sb(st, [32, 512], F32, "vw2")
            C.dma(SP, vw2[:], vres["w2"][0], [vres["w2"][1]], [vw2_b])
        _tt(C, DVE, dv[:, 0:15], par[:, 0:15], par[:, 15:30], ALU.add, [par_b], [dv_b])
        _ts(C, DVE, dv[:, 0:15], dv[:, 0:15], -1.0, 1.0, ALU.mult, ALU.add, [dv_b], [dv_b])
        _ts(C, DVE, dv[:, 15:19], par[:, 34:38], -1.0, 1.0, ALU.mult, ALU.add, [par_b], [dv_b])
        dqc = [0]

        def grp_gen(stream):
            raws = [C.sb(st, [128, 514], F32, "raw") for _ in range(3)]
            sh = [C.sb(st, [128, 512], F32, "sh") for _ in range(15)]
            tmp = [C.sb(st, [128, 512], F32, "tmp") for _ in range(6)]
            outb = [C.sb(st, [128, 512], F32, "outb") for _ in range(8)]
            pss = [C.ps(st, [128, 512], F32, "rps") for _ in range(4)]
            cnt = {"raw": 0, "tmp": 0, "out": 0, "ps": 0}

            def nxt(lst, key):
                x = lst[cnt[key] % len(lst)]
                cnt[key] += 1
                return x

            def store(name, c, tsl, t, t_b):
                eng = (SP, ACT, POOL)[dqc[0] % 3]
                dqc[0] += 1
                C.dma(eng, RO[name][0][c * 128:(c + 1) * 128, tsl], t[:], [t_b], [RO[name][1]])

            for g in range(stream, NG, 2):
                t0 = g * 512
                tsl = slice(t0, t0 + 512)
                for c in range(15):
                    raw, raw_b = nxt(raws, "raw")
                    lo = max(t0 - 1, 0)
                    hi = min(t0 + 513, S)
                    if g == 0:
                        C.op(POOL, lambda e, raw=raw: e.memset(raw[:, 0:1], 0.0), [], [raw_b])
                    if g == NG - 1:
                        C.op(POOL, lambda e, raw=raw: e.memset(raw[:, 513:514], 0.0), [], [raw_b])
                    C.dma(SP if c % 2 else ACT, raw[:, lo - (t0 - 1):hi - (t0 - 1)], prw[0][c * 128:(c + 1) * 128, lo:hi],
                          [prw[1]], [raw_b])
                    s_, s_b = sh[c]
                    _act(C, AF.Copy, s_[:], raw[:, 1:513], [raw_b, dv_b], [s_b], scale=dv[:, c:c + 1])
                    _stt(C, DVE, s_[:], raw[:, 0:512], par[:, c:c + 1], s_[:], ALU.mult, ALU.add, [raw_b, par_b, s_b], [s_b])
                    _stt(C, DVE, s_[:], raw[:, 2:514], par[:, 15 + c:16 + c], s_[:], ALU.mult, ALU.add, [raw_b, par_b, s_b], [s_b])
                    yield
                twd, twd_b = sh[12]
                sad, sad_b = sh[13]
                sgd, sgd_b = sh[14]
                _act(C, AF.Tanh, twd[:], twd[:], [twd_b], [twd_b])
                _act(C, AF.Sigmoid, sgd[:], sgd[:], [sgd_b], [sgd_b])
                for c in range(4):
                    csl = slice(c * 128, (c + 1) * 128)
                    r_, r_b = sh[c]
                    k_, k_b = sh[4 + c]
                    v_, v_b = sh[8 + c]
                    store("r", c, tsl, r_, r_b)
                    av = []
                    for d in range(2):
                        dsl = slice(d * 64, (d + 1) * 64)
                        ps, ps_b = nxt(pss, "ps")
                        _mm(C, ps[:], w2s[dsl, csl], twd[dsl, :], [w2_b, twd_b], [ps_b])
                        o, o_b = nxt(outb, "out")
                        _act(C, AF.Sigmoid, o[:], ps[:], [ps_b, par_b], [o_b], bias=par[:, 42 + 4 * d + c:43 + 4 * d + c])
                        store(f"sg{d}", c, tsl, o, o_b)
                        ps, ps_b = nxt(pss, "ps")
                        _mm(C, ps[:], a2s[dsl, csl], sad[dsl, :], [a2_b, sad_b], [ps_b])
                        o, o_b = nxt(outb, "out")
                        _act(C, AF.Sigmoid, o[:], ps[:], [ps_b, par_b], [o_b], bias=par[:, 50 + 4 * d + c:51 + 4 * d + c])
                        store(f"a{d}", c, tsl, o, o_b)
                        av.append((o, o_b))
                    ps, ps_b = nxt(pss, "ps")
                    _mm(C, ps[:], g2s[:, csl], sgd[:], [g2_b, sgd_b], [ps_b])
                    o, o_b = nxt(outb, "out")
                    _cp(C, ACT, o[:], ps[:], [ps_b], [o_b])
                    store("g", c, tsl, o, o_b)
                    yield
                    if vres is not None:
                        hv, hv_b = nxt(tmp, "tmp")
                        C.dma(SP, hv[0:32, :], vres["hv1"][0][:, tsl], [vres["hv1"][1]], [hv_b])
                        vf, vf_b = nxt(tmp, "tmp")
                        C.dma(ACT, vf[:], vres["vfirst"][0][csl, tsl], [vres["vfirst"][1]], [vf_b])
                        ps, ps_b = nxt(pss, "ps")
                        _mm(C, ps[:], vw2[:, csl], hv[0:32, :], [vw2_b, hv_b], [ps_b])
                        mx, mx_b = nxt(tmp, "tmp")
                        _act(C, AF.Sigmoid, mx[:], ps[:], [ps_b, par_b], [mx_b], bias=par[:, 58 + c:59 + c])
                        _tt(C, DVE, vf[:], vf[:], v_[:], ALU.subtract, [vf_b, v_b], [vf_b])
                        _tt(C, DVE, vf[:], vf[:], mx[:], ALU.mult, [vf_b, mx_b], [vf_b])
                        _tt(C, DVE, v_[:], v_[:], vf[:], ALU.add, [v_b, vf_b], [v_b])
                    else:
                        store("vfirst", c, tsl, v_, v_b)
                    store("v", c, tsl, v_, v_b)
                    sq, sq_b = nxt(tmp, "tmp")
                    _act(C, AF.Square, sq[:], k_[:], [k_b, par_b], [sq_b], scale=par[:, 30 + c:31 + c])
                    ps, ps_b = nxt(pss, "ps")
                    _mm(C, ps[:], blk[:], sq[:], [blk_b, sq_b], [ps_b])
                    nr, nr_b = nxt(tmp, "tmp")
                    _act(C, AF.Sqrt, nr[:], ps[:], [ps_b], [nr_b])
                    _ts(C, DVE, nr[:], nr[:], 1e-12, None, ALU.max, None, [nr_b], [nr_b])
                    C.op(DVE, lambda e, nr=nr: e.reciprocal(out=nr[:], in_=nr[:]), [nr_b], [nr_b])
                    o, o_b = nxt(outb, "out")
                    _stt(C, DVE, o[:], k_[:], par[:, 30 + c:31 + c], nr[:], ALU.mult, ALU.mult, [k_b, par_b, nr_b], [o_b])
                    store("kk", c, tsl, o, o_b)
                    yield
                    kds = []
                    for d in range(2):
                        a_, a_b = av[d]
                        o, o_b = nxt(outb, "out")
                        _ts(C, POOL, o[:], a_[:], par[:, 34 + c:35 + c], dv[:, 15 + c:16 + c], ALU.mult, ALU.add,
                            [a_b, par_b, dv_b], [o_b])
                        _tt(C, POOL, o[:], o[:], k_[:], ALU.mult, [o_b, k_b], [o_b])
                        store(f"kd{d}", c, tsl, o, o_b)
                        kds.append((o, o_b))
                    ks, ks_b = nxt(tmp, "tmp")
                    _tt(C, DVE, ks[:], kds[0][0][:], kds[1][0][:], ALU.add, [kds[0][1], kds[1][1]], [ks_b])
                    _stt(C, DVE, ks[:], r_[:], par[:, 38 + c:39 + c], ks[:], ALU.mult, ALU.mult, [r_b, par_b, ks_b], [ks_b])
                    ps, ps_b = nxt(pss, "ps")
                    _mm(C, ps[:], blk[:], ks[:], [blk_b, ks_b], [ps_b])
                    o, o_b = nxt(outb, "out")
                    _tt(C, DVE, o[:], ps[:], v_[:], ALU.mult, [ps_b, v_b], [o_b])
                    store("bonus", c, tsl, o, o_b)
                    yield

        gens = [grp_gen(0), grp_gen(1)]
        while gens:
            for gq in list(gens):
                try:
                    next(gq)
                except StopIteration:
                    gens.remove(gq)
    C.P.barrier()


RNAMES = ("r", "v", "g", "bonus", "kk", "kd0", "kd1", "a0", "a1", "sg0", "sg1")


def scan_consts():
    i = np.arange(64)
    lo = (i[None, :] < i[:, None]).astype(np.float32)
    up = (i[None, :] > i[:, None]).astype(np.float32)
    loi = (i[None, :] <= i[:, None]).astype(np.float32)
    upi = (i[None, :] >= i[:, None]).astype(np.float32)
    idn = np.eye(64, dtype=np.float32)
    return np.ascontiguousarray(np.stack([lo, up, loi, upi, idn], axis=1))


def stage_rwkv_scan(C, RO, msk_d, y_d):
    GT = 128
    with ExitStack() as st:
        msk, msk_b = C.sb(st, [64, 5, 64], F32, "msk")
        C.dma(SP, msk[:], msk_d[0], [msk_d[1]], [msk_b])
        idnb, idnb_b = C.sb(st, [64, 64], BF16, "idnb")
        _cp(C, DVE, idnb[:], msk[:, 4, :], [msk_b], [idnb_b])
        rmask, rmask_b = C.sb(st, [64, 8, GT], F32, "rmask")
        C.op(POOL, lambda e: e.memset(rmask[:], 1.0), [], [rmask_b])
        C.op(POOL, lambda e: e.memset(rmask[:].rearrange("p h (c t) -> p (h c) t", t=64)[:, :, 0:1], 0.0), [rmask_b], [rmask_b])
        pss = [C.ps(st, [64, 8, 64], F32, "sps") for _ in range(6)]
        tpss = [C.ps(st, [64, 8, 64], BF16, "tps") for _ in range(2)]
        pc = [0, 0]
        NCH = GT // 64

        def nps():
            x = pss[pc[0] % 6]
            pc[0] += 1
            return x

        def ntps():
            x = tpss[pc[1] % 2]
            pc[1] += 1
            return x

        def mm8(lhs_fn, rhs_fn, R):
            p, p_b = nps()
            for h in range(8):
                _mm(C, p[:, h, :], lhs_fn(h), rhs_fn(h), R, [p_b])
            return p, p_b

        def flat(t):
            return t[:].rearrange("p h t -> p (h t)")

        def v4(t):
            return t[:].rearrange("p h (c t) -> p (h c) t", t=64)

        bufs = []
        for d in range(2):
            H = C.sb(st, [64, 8, 64], F32, "H")
            Hb = C.sb(st, [64, 8, 64], BF16, "Hb")
            names = ["r", "v", "kk", "kd", "a", "sg", "cs", "e2", "Gi"] + (["cs2"] if d == 1 else [])
            G = {n: C.sb(st, [64, 8, GT], F32, "g_" + n) for n in names}
            Gb = {n: C.sb(st, [64, 8, GT], BF16, "gb_" + n) for n in ("At", "Bt", "Kt", "Rt", "Bh", "Kh", "vb")}
            Cb = {n: C.sb(st, [64, 8, 64], BF16, "c_" + n) for n in
                  ("Vt", "Bht", "Kht", "Pa", "PTa", "Pb", "PTb", "TT", "AakT", "ArbT", "ArkT", "W", "U")}
            Cf = {n: C.sb(st, [64, 8, 64], F32, "c_" + n) for n in ("WV", "YV", "ZV", "Yo", "Ht")}
            bufs.append((H, Hb, G, Gb, Cb, Cf))

        def dir_gen(d):
            (H, H_b), (Hb, Hb_b), G, Gb, Cb, Cf = bufs[d]
            NS = NCH * 8
            mN = msk[:, 0 if d == 0 else 1, :].unsqueeze(1).to_broadcast([64, 8, 64])
            mNT = msk[:, 1 if d == 0 else 0, :].unsqueeze(1).to_broadcast([64, 8, 64])
            mI = msk[:, 3 if d == 0 else 2, :].unsqueeze(1).to_broadcast([64, 8, 64])
            idb = msk[:, 4, :].unsqueeze(1).to_broadcast([64, 8, 64])
            dq_e = (SP, ACT) if d == 0 else (ACT, SP)
            C.op(DVE, lambda e: e.memset(H[:], 0.0), [], [H_b])
            C.op(DVE, lambda e: e.memset(Hb[:], 0.0), [], [Hb_b])
            for gi in range(S // GT):
                g = gi if d == 0 else S // GT - 1 - gi
                t0 = g * GT
                dq = 0
                for n, src in (("r", "r"), ("v", "v"), ("kk", "kk"), ("kd", f"kd{d}"), ("a", f"a{d}"), ("sg", f"sg{d}")):
                    t, t_b = G[n]
                    C.dma(dq_e[dq % 2], t[:], RO[src][0][:, t0:t0 + GT].rearrange("(h j) t -> j h t", j=64),
                          [RO[src][1]], [t_b])
                    dq += 1
                r_, r_b = G["r"]; v_, v_b = G["v"]; kk_, kk_b = G["kk"]; kd_, kd_b = G["kd"]; a_, a_b = G["a"]
                sg_, sg_b = G["sg"]; cs_, cs_b = G["cs"]; e2_, e2_b = G["e2"]; Gi_, Gi_b = G["Gi"]
                At_, At_b = Gb["At"]; Bt_, Bt_b = Gb["Bt"]; Kt_, Kt_b = Gb["Kt"]; Rt_, Rt_b = Gb["Rt"]
                Bh_, Bh_b = Gb["Bh"]; Kh_, Kh_b = Gb["Kh"]; vb_, vb_b = Gb["vb"]
                yield
                C.op(DVE, lambda e: e.tensor_tensor_scan(out=flat(cs_), data0=flat(rmask), data1=flat(sg_), initial=0.0,
                                                         op0=ALU.mult, op1=ALU.add), [rmask_b, sg_b], [cs_b])
                if d == 0:
                    c2_, c2_b = cs_, cs_b
                    tot = v4(cs_)[:, :, 63:64].to_broadcast([64, NS, 64])
                else:
                    c2_, c2_b = G["cs2"]
                    _tt(C, DVE, c2_[:], sg_[:], cs_[:], ALU.subtract, [sg_b, cs_b], [c2_b])
                    _tt(C, DVE, v4(c2_), v4(c2_), v4(cs_)[:, :, 63:64].to_broadcast([64, NS, 64]), ALU.add, [c2_b, cs_b], [c2_b])
                    tot = v4(c2_)[:, :, 0:1].to_broadcast([64, NS, 64])
                _tt(C, DVE, v4(e2_), tot, v4(c2_), ALU.subtract, [c2_b], [e2_b])
                _tt(C, POOL, sg_[:], c2_[:], sg_[:], ALU.subtract, [c2_b, sg_b], [sg_b])
                yield
                _act(C, AF.Exp, Gi_[:], c2_[:], [c2_b], [Gi_b], scale=LAM)
                _act(C, AF.Exp, c2_[:], c2_[:], [c2_b, e2_b], [c2_b], scale=-LAM)
                Gm_, Gm_b = c2_, c2_b
                _act(C, AF.Exp, sg_[:], sg_[:], [sg_b], [sg_b], scale=-LAM)
                _act(C, AF.Exp, e2_[:], e2_[:], [e2_b], [e2_b], scale=-LAM)
                _cp(C, ACT, vb_[:], v_[:], [v_b], [vb_b])
                yield
                _tt(C, POOL, a_[:], kk_[:], a_[:], ALU.mult, [kk_b, a_b], [a_b])
                _stt(C, DVE, At_[:], kk_[:], -1.0, sg_[:], ALU.mult, ALU.mult, [kk_b, sg_b], [At_b])
                _tt(C, POOL, Bt_[:], a_[:], Gi_[:], ALU.mult, [a_b, Gi_b], [Bt_b])
                _tt(C, DVE, Kt_[:], kd_[:], Gi_[:], ALU.mult, [kd_b, Gi_b], [Kt_b])
                yield
                _tt(C, POOL, Rt_[:], r_[:], Gm_[:], ALU.mult, [r_b, Gm_b], [Rt_b])
                _tt(C, DVE, Bh_[:], a_[:], e2_[:], ALU.mult, [a_b, e2_b], [Bh_b])
                _tt(C, POOL, Kh_[:], kd_[:], e2_[:], ALU.mult, [kd_b, e2_b], [Kh_b])
                yield
                for ci in range(NCH):
                    cc = ci if d == 0 else NCH - 1 - ci
                    ts_ = slice(cc * 64, cc * 64 + 64)
                    Vt, Vt_b = Cb["Vt"]; Bht, Bht_b = Cb["Bht"]; Kht, Kht_b = Cb["Kht"]
                    for src, src_b, dst, dst_b in ((vb_, vb_b, Vt, Vt_b), (Bh_, Bh_b, Bht, Bht_b), (Kh_, Kh_b, Kht, Kht_b)):
                        p, p_b = ntps()
                        for h in range(8):
                            C.op(PE, lambda e, p=p, h=h, src=src, ts_=ts_: e.transpose(out=p[:, h, :], in_=src[:, h, ts_],
                                                                                       identity=idnb[:]), [src_b, idnb_b], [p_b])
                        _cp(C, ACT, dst[:], p[:], [p_b], [dst_b])
                        yield
                    P_, P_b = Cb["Pa"]; PT_, PT_b = Cb["PTa"]; TT, TT_b = Cb["TT"]
                    p, p_b = mm8(lambda h: At_[:, h, ts_], lambda h: Bt_[:, h, ts_], [At_b, Bt_b])
                    _tt(C, DVE, P_[:], p[:], mN, ALU.mult, [p_b, msk_b], [P_b])
                    yield
                    p, p_b = mm8(lambda h: Bt_[:, h, ts_], lambda h: At_[:, h, ts_], [At_b, Bt_b])
                    _tt(C, DVE, PT_[:], p[:], mNT, ALU.mult, [p_b, msk_b], [PT_b])
                    _tt(C, DVE, TT[:], PT_[:], idb, ALU.add, [PT_b, msk_b], [TT_b])
                    yield
                    AakT, AakT_b = Cb["AakT"]; ArbT, ArbT_b = Cb["ArbT"]; ArkT, ArkT_b = Cb["ArkT"]
                    p, p_b = mm8(lambda h: Kt_[:, h, ts_], lambda h: At_[:, h, ts_], [Kt_b, At_b])
                    _tt(C, DVE, AakT[:], p[:], mNT, ALU.mult, [p_b, msk_b], [AakT_b])
                    yield
                    p, p_b = mm8(lambda h: Bt_[:, h, ts_], lambda h: Rt_[:, h, ts_], [Bt_b, Rt_b])
                    _tt(C, DVE, ArbT[:], p[:], mI, ALU.mult, [p_b, msk_b], [ArbT_b])
                    yield
                    p, p_b = mm8(lambda h: Kt_[:, h, ts_], lambda h: Rt_[:, h, ts_], [Kt_b, Rt_b])
                    _tt(C, DVE, ArkT[:], p[:], mI, ALU.mult, [p_b, msk_b], [ArkT_b])
                    yield
                    cur = (P_, P_b, PT_, PT_b)
                    for rd in range(5):
                        Pc, Pc_b, PTc, PTc_b = cur
                        Pn, Pn_b = Cb["Pb"] if rd % 2 == 0 else Cb["Pa"]
                        PTn, PTn_b = Cb["PTb"] if rd % 2 == 0 else Cb["PTa"]
                        p, p_b = mm8(lambda h: PTc[:, h, :], lambda h: Pc[:, h, :], [Pc_b, PTc_b])
                        _cp(C, ACT, Pn[:], p[:], [p_b], [Pn_b])
                        yield
                        if rd < 4:
                            p, p_b = mm8(lambda h: Pc[:, h, :], lambda h: PTc[:, h, :], [Pc_b, PTc_b])
                            _cp(C, ACT, PTn[:], p[:], [p_b], [PTn_b])
                            yield
                        p, p_b = mm8(lambda h: Pn[:, h, :], lambda h: TT[:, h, :], [Pn_b, TT_b])
                        _tt(C, DVE, TT[:], p[:], TT[:], ALU.add, [p_b, TT_b], [TT_b])
                        yield
                        cur = (Pn, Pn_b, PTn, PTn_b)
                    WV, WV_b = Cf["WV"]; YV, YV_b = Cf["YV"]; ZV, ZV_b = Cf["ZV"]
                    p, p_b = mm8(lambda h: AakT[:, h, :], lambda h: Vt[:, h, :], [AakT_b, Vt_b])
                    _cp(C, ACT, WV[:], p[:], [p_b], [WV_b])
                    yield
                    p, p_b = mm8(lambda h: ArkT[:, h, :], lambda h: Vt[:, h, :], [ArkT_b, Vt_b])
                    _cp(C, ACT, YV[:], p[:], [p_b], [YV_b])
                    yield
                    p, p_b = mm8(lambda h: Kht[:, h, :], lambda h: Vt[:, h, :], [Kht_b, Vt_b])
                    _cp(C, ACT, ZV[:], p[:], [p_b], [ZV_b])
                    yield
                    W, W_b = Cb["W"]; U, U_b = Cb["U"]; Yo, Yo_b = Cf["Yo"]; Ht, Ht_b = Cf["Ht"]
                    p, p_b = mm8(lambda h: At_[:, h, ts_], lambda h: Hb[:, h, :], [At_b, Hb_b])
                    _tt(C, DVE, W[:], p[:], WV[:], ALU.add, [p_b, WV_b], [W_b])
                    yield
                    p, p_b = mm8(lambda h: TT[:, h, :], lambda h: W[:, h, :], [TT_b, W_b])
                    _cp(C, ACT, U[:], p[:], [p_b], [U_b])
                    yield
                    p, p_b = nps()
                    for h in range(8):
                        _mm(C, p[:, h, :], Rt_[:, h, ts_], Hb[:, h, :], [Rt_b, Hb_b], [p_b], start=True, stop=False)
                        _mm(C, p[:, h, :], ArbT[:, h, :], U[:, h, :], [ArbT_b, U_b], [p_b], start=False, stop=True)
                    _tt(C, DVE, Yo[:], p[:], YV[:], ALU.add, [p_b, YV_b], [Yo_b])
                    r0 = t0 + cc * 64
                    C.dma(SP, y_d[d][0][r0:r0 + 64, :], Yo[:].rearrange("p h i -> p (h i)"), [Yo_b], [y_d[d][1]])
                    yield
                    p, p_b = mm8(lambda h: Bht[:, h, :], lambda h: U[:, h, :], [Bht_b, U_b])
                    gidx = cc * 64 + (63 if d == 0 else 0)
                    gl = Gm_[:, :, gidx:gidx + 1].to_broadcast([64, 8, 64])
                    _tt(C, POOL, Ht[:], H[:], gl, ALU.mult, [H_b, Gm_b], [Ht_b])
                    _tt(C, POOL, Ht[:], Ht[:], ZV[:], ALU.add, [Ht_b, ZV_b], [Ht_b])
                    _tt(C, DVE, H[:], p[:], Ht[:], ALU.add, [p_b, Ht_b], [H_b])
                    _cp(C, ACT, Hb[:], H[:], [H_b], [Hb_b])
                    yield

        gens = [dir_gen(0), dir_gen(1)]
        while gens:
            for gq in list(gens):
                try:
                    next(gq)
                except StopIteration:
                    gens.remove(gq)
    C.P.barrier()


def stage_rwkv_out(C, y_d, RO, par_d, yaT):
    with ExitStack() as st:
        par, par_b = C.sb(st, [128, PAR_COLS], F32, "par")
        C.dma(SP, par[:], par_d[0], [par_d[1]], [par_b])
        identf, identf_b = C.sb(st, [128, 128], F32, "identf")
        C.op(POOL, lambda e: e.memset(identf[:], 0.0), [], [identf_b])
        C.op(POOL, lambda e: e.affine_select(out=identf[:], in_=identf[:], pattern=[[-1, 128]], compare_op=ALU.not_equal,
                                             fill=1.0, base=0, channel_multiplier=1), [identf_b], [identf_b])
        y0s = [C.sb(st, [128, 8, 64], F32, "y0") for _ in range(2)]
        y1s = [C.sb(st, [128, 8, 64], F32, "y1") for _ in range(2)]
        yhs = [C.sb(st, [128, 8, 64], F32, "yh") for _ in range(8)]
        sqt, sqt_b = C.sb(st, [128, 8, 64], F32, "sqt")
        sts = [C.sb(st, [128, 3, 8], F32, "st") for _ in range(2)]
        pts = [C.ps(st, [128, 512], F32, "pt") for _ in range(4)]
        bgs = [C.sb(st, [128, 2, 512], F32, "bg") for _ in range(2)]
        fms = [C.sb(st, [128, 512], F32, "fm") for _ in range(2)]
        obs = [C.sb(st, [128, 512], BF16, "ob") for _ in range(2)]
        it = 0
        oc = 0
        for g in range(NG):
            tsl = slice(g * 512, (g + 1) * 512)
            yh4 = []
            for s in range(4):
                y0, y0_b = y0s[it % 2]
                y1, y1_b = y1s[it % 2]
                sv, sv_b = sts[it % 2]
                yh, yh_b = yhs[it % 8]
                it += 1
                r0 = g * 512 + s * 128
                C.dma(SP, y0[:], y_d[0][0][r0:r0 + 128, :].rearrange("p (h i) -> p h i", i=64), [y_d[0][1]], [y0_b])
                C.dma(ACT, y1[:], y_d[1][0][r0:r0 + 128, :].rearrange("p (h i) -> p h i", i=64), [y_d[1][1]], [y1_b])
                _tt(C, DVE, y0[:], y0[:], y1[:], ALU.add, [y0_b, y1_b], [y0_b])
                C.op(DVE, lambda e, sv=sv, y0=y0: e.tensor_reduce(out=sv[:, 0, :], in_=y0[:], axis=AX.X, op=ALU.add), [y0_b], [sv_b])
                _ts(C, DVE, sv[:, 0, :], sv[:, 0, :], 1.0 / 64, None, ALU.mult, None, [sv_b], [sv_b])
                _tt(C, DVE, y0[:], y0[:], sv[:, 0, :].unsqueeze(2).to_broadcast([128, 8, 64]), ALU.subtract, [y0_b, sv_b], [y0_b])
                _tt(C, POOL, sqt[:], y0[:], y0[:], ALU.mult, [y0_b], [sqt_b])
                C.op(DVE, lambda e, sv=sv: e.tensor_reduce(out=sv[:, 1, :], in_=sqt[:], axis=AX.X, op=ALU.add), [sqt_b], [sv_b])
                _act(C, AF.Sqrt, sv[:, 2, :], sv[:, 1, :], [sv_b], [sv_b], scale=1.0 / 64, bias=64e-5)
                C.op(DVE, lambda e, sv=sv: e.reciprocal(out=sv[:, 2, :], in_=sv[:, 2, :]), [sv_b], [sv_b])
                _tt(C, DVE, yh[:], y0[:], sv[:, 2, :].unsqueeze(2).to_broadcast([128, 8, 64]), ALU.mult, [y0_b, sv_b], [yh_b])
                yh4.append((yh, yh_b))
            for c in range(4):
                pt, pt_b = pts[oc % 4]
                bg, bg_b = bgs[oc % 2]
                fm, fm_b = fms[oc % 2]
                ob, ob_b = obs[oc % 2]
                oc += 1
                C.dma(SP, bg[:, 0, :], RO["bonus"][0][c * 128:(c + 1) * 128, tsl], [RO["bonus"][1]], [bg_b])
                C.dma(ACT, bg[:, 1, :], RO["g"][0][c * 128:(c + 1) * 128, tsl], [RO["g"][1]], [bg_b])
                for s in range(4):
                    yh, yh_b = yh4[s]
                    C.op(PE, lambda e, pt=pt, yh=yh, s=s, c=c: e.transpose(
                        out=pt[:, s * 128:(s + 1) * 128], in_=yh[:].rearrange("p h i -> p (h i)")[:, c * 128:(c + 1) * 128],
                        identity=identf[:]), [yh_b, identf_b], [pt_b])
                _act(C, AF.Identity, fm[:], pt[:], [pt_b, par_b], [fm_b], scale=par[:, 62 + c:63 + c], bias=par[:, 66 + c:67 + c])
                _tt(C, DVE, fm[:], fm[:], bg[:, 0, :], ALU.add, [fm_b, bg_b], [fm_b])
                _tt(C, DVE, ob[:], fm[:], bg[:, 1, :], ALU.mult, [fm_b, bg_b], [ob_b])
                C.dma(POOL, yaT[0][c * 128:(c + 1) * 128, tsl], ob[:], [ob_b], [yaT[1]])
    C.P.barrier()


LSTN = 2304


def moe_consts():
    t = np.arange(S)
    inv = np.zeros((4, S), np.float32)
    for gi, w in enumerate((2, 4, 8, 16)):
        lo = w // 2
        hi = w - lo - 1
        start = np.clip(t - lo, 0, S)
        end = np.clip(t + hi + 1, 0, S)
        inv[gi] = 1.0 / (end - start).astype(np.float32)
    tri = (np.arange(128)[:, None] < np.arange(128)[None, :]).astype(np.float32)
    tb = np.zeros((128, 65), np.float32)
    tb[:, :64] = 128.0 * np.arange(64, dtype=np.float32)[None, :]
    tb[:, 64] = np.arange(128, dtype=np.float32)
    eoff = np.ascontiguousarray(np.broadcast_to((np.arange(16, dtype=np.float32) * LSTN)[None, :], (128, 16)))
    return {"invcnt": inv, "tri": tri, "tbase": tb, "eoff": eoff}


def load_cast(C, st, eng_cast, dst, dst_b, src_ap, src_b, stg, k0):
    t, t_b = stg[k0 % len(stg)]
    a, n = src_ap.shape[1], src_ap.shape[2]
    C.dma((SP, ACT)[k0 % 2], t[:, 0:a, 0:n], src_ap, [src_b], [t_b])
    _cp(C, eng_cast, dst, t[:, 0:a, 0:n], [t_b], [dst_b])


def stage_pool(C, ppool, pw_d, par_d, inv_d, ybT):
    with ExitStack() as st:
        W = S + 32
        par, par_b = C.sb(st, [128, PAR_COLS], F32, "par")
        C.dma(SP, par[:], par_d[0], [par_d[1]], [par_b])
        zp, zp_b = C.sb(st, [128, W], F32, "zp")
        sa, sa_b = C.sb(st, [128, W], F32, "sa")
        sb_, sb_b = C.sb(st, [128, W], F32, "sb")
        inv, inv_b = C.sb(st, [128, S], F32, "inv")
        pl, pl_b = C.sb(st, [128, S], BF16, "pl")
        pwf, pwf_b = C.sb(st, [128, 128], F32, "pwf")
        pw, pw_b = C.sb(st, [128, 128], BF16, "pw")
        pss = [C.ps(st, [128, 512], F32, "pps") for _ in range(2)]
        obs = [C.sb(st, [128, 512], BF16, "pob") for _ in range(2)]
        C.op(POOL, lambda e: e.memset(zp[:], 0.0), [], [zp_b])
        C.op(POOL, lambda e: e.memset(sa[:], 0.0), [], [sa_b])
        C.op(POOL, lambda e: e.memset(sb_[:], 0.0), [], [sb_b])
        k = 0
        for gi in range(4):
            C.dma(SP, zp[:, 16:16 + S], ppool[0][gi * 128:(gi + 1) * 128, :], [ppool[1]], [zp_b])
            C.dma(ACT, inv[:], inv_d[0][gi:gi + 1, :].partition_broadcast(128) if False else inv_d[0][gi].partition_broadcast(128),
                  [inv_d[1]], [inv_b])
            C.dma(SP, pwf[:], pw_d[0][gi], [pw_d[1]], [pwf_b])
            _cp(C, POOL, pw[:], pwf[:], [pwf_b], [pw_b])
            _tt(C, DVE, sa[:, 1:W], zp[:, 1:W], zp[:, 0:W - 1], ALU.add, [zp_b], [sa_b])
            cur, cur_b, oth, oth_b = sa, sa_b, sb_, sb_b
            sh = 1
            lo_, hi_ = 1, W
            for lvl in range(gi):
                lo_, hi_ = lo_ + sh, hi_ - sh
                _tt(C, DVE, oth[:, lo_:hi_], cur[:, lo_ + sh:hi_ + sh], cur[:, lo_ - sh:hi_ - sh], ALU.add, [cur_b], [oth_b])
                cur, cur_b, oth, oth_b = oth, oth_b, cur, cur_b
                sh *= 2
            _tt(C, DVE, oth[:, 16:16 + S], cur[:, 16:16 + S], inv[:], ALU.mult, [cur_b, inv_b], [oth_b])
            _tt(C, POOL, pl[:], oth[:, 16:16 + S], zp[:, 16:16 + S], ALU.subtract, [oth_b, zp_b], [pl_b])
            for g in range(NG):
                tsl = slice(g * 512, (g + 1) * 512)
                ps, ps_b = pss[k % 2]
                ob, ob_b = obs[k % 2]
                k += 1
                _mm(C, ps[:], pw[:], pl[:, tsl], [pw_b, pl_b], [ps_b])
                _act(C, AF.Copy, ob[:], ps[:], [ps_b, par_b], [ob_b], scale=par[:, 70 + gi:71 + gi])
                C.dma(SP, ybT[0][gi * 128:(gi + 1) * 128, tsl], ob[:], [ob_b], [ybT[1]])
    C.P.barrier()


def stage_merge(C, x_in, x_tok, yT, pgate, wbr_d, wout_d, nffn_d, router_d, h_tok, aff_tok, affT):
    with ExitStack() as st:
        stg = [C.sb(st, [128, 4, 1024], F32, "mstg") for _ in range(2)]
        Wb = [C.sb(st, [128, 4, 1024], BF16, "Wb") for _ in range(3)]
        Wo, Wo_b = C.sb(st, [128, 8, 1024], BF16, "Wo")
        k0 = 0
        for b in range(3):
            load_cast(C, st, POOL, Wb[b][0][:], Wb[b][1], wbr_d[b][0].rearrange("(kc p) n -> p kc n", p=128), wbr_d[b][1], stg, k0)
            k0 += 1
        for hf in range(2):
            load_cast(C, st, POOL, Wo[:, hf * 4:(hf + 1) * 4, :], Wo_b,
                      wout_d[0][hf * 512:(hf + 1) * 512, :].rearrange("(kc p) n -> p kc n", p=128), wout_d[1], stg, k0)
            k0 += 1
        gb, gb_b = C.sb(st, [128, D], F32, "gbf")
        C.dma(SP, gb[:], nffn_d[0].partition_broadcast(128), [nffn_d[1]], [gb_b])
        rt, rt_b = C.sb(st, [128, 8, 16], F32, "rt")
        C.dma(SP, rt[:], router_d[0].rearrange("(kc p) e -> p kc e", p=128), [router_d[1]], [rt_b])
        identf, identf_b = C.sb(st, [128, 128], F32, "identf")
        C.op(POOL, lambda e: e.memset(identf[:], 0.0), [], [identf_b])
        C.op(POOL, lambda e: e.affine_select(out=identf[:], in_=identf[:], pattern=[[-1, 128]], compare_op=ALU.not_equal,
                                             fill=1.0, base=0, channel_multiplier=1), [identf_b], [identf_b])
        ys = [C.sb(st, [128, 4, 512], BF16, "ys") for _ in range(3)]
        gt, gt_b = C.sb(st, [128, 24, 512], BF16, "gt")
        mg, mg_b = C.sb(st, [128, 8, 512], BF16, "mg")
        tmps = [C.sb(st, [128, 512], F32, "mt") for _ in range(3)]
        xts = [C.sb(st, [128, D], F32, "xt") for _ in range(2)]
        hf_, hf_b = C.sb(st, [128, D], F32, "hf")
        hb, hb_b = C.sb(st, [128, D], BF16, "hb")
        sq, sq_b = C.sb(st, [128, D], BF16, "sqj")
        hT, hT_b = C.sb(st, [128, 8, 128], F32, "hT32")
        sv, sv_b = C.sb(st, [128, 8], F32, "sv")
        lg, lg_b = C.sb(st, [128, 16], F32, "lg")
        af, af_b = C.sb(st, [128, 16], F32, "af")
        aT, aT_b = C.sb(st, [16, 128], F32, "aT")
        pm = [C.ps(st, [128, 512], F32, "pm") for _ in range(3)]
        px = [C.ps(st, [128, 512], F32, "px") for _ in range(2)]
        ptr, ptr_b = C.ps(st, [128, 8, 128], F32, "ptr")
        psm, psm_b = C.ps(st, [128, 512], F32, "psm")
        it = 0
        for g in range(NG):
            tsl = slice(g * 512, (g + 1) * 512)
            for b in range(3):
                C.dma((SP, ACT, SP)[b], ys[b][0][:], yT[b][0][:, tsl].rearrange("(kc p) t -> p kc t", p=128), [yT[b][1]], [ys[b][1]])
            C.dma(ACT, gt[:], pgate[0][:, tsl].rearrange("(c p) t -> p c t", p=128), [pgate[1]], [gt_b])
            for dc in range(8):
                for b in range(3):
                    ps, ps_b = pm[b]
                    for kc in range(4):
                        _mm(C, ps[:], Wb[b][0][:, kc, dc * 128:(dc + 1) * 128], ys[b][0][:, kc, :], [Wb[b][1], ys[b][1]], [ps_b],
                            start=(kc == 0), stop=(kc == 3))
                    _tt(C, DVE, tmps[b][0][:], ps[:], gt[:, b * 8 + dc, :], ALU.mult, [ps_b, gt_b], [tmps[b][1]])
                _tt(C, POOL, tmps[0][0][:], tmps[0][0][:], tmps[1][0][:], ALU.add, [tmps[0][1], tmps[1][1]], [tmps[0][1]])
                _tt(C, POOL, mg[:, dc, :], tmps[0][0][:], tmps[2][0][:], ALU.add, [tmps[0][1], tmps[2][1]], [mg_b])
            for s in range(4):
                xt, xt_b = xts[it % 2]
                it += 1
                r0 = g * 512 + s * 128
                C.dma(SP, xt[:], x_in[0][r0:r0 + 128, :], [x_in[1]], [xt_b])
                for half in range(2):
                    ps, ps_b = px[half]
                    for kc in range(8):
                        _mm(C, ps[:], mg[:, kc, s * 128:(s + 1) * 128], Wo[:, kc, half * 512:(half + 1) * 512], [mg_b, Wo_b], [ps_b],
                            start=(kc == 0), stop=(kc == 7))
                    _tt(C, DVE, xt[:, half * 512:(half + 1) * 512], ps[:], xt[:, half * 512:(half + 1) * 512], ALU.add,
                        [ps_b, xt_b], [xt_b])
                C.dma(ACT, x_tok[0][r0:r0 + 128, :], xt[:], [xt_b], [x_tok[1]])
                _act(C, AF.Square, sq[:], xt[:], [xt_b], [sq_b, sv_b], accum_out=sv[:, 0:1])
                _act(C, AF.Sqrt, sv[:, 1:2], sv[:, 0:1], [sv_b], [sv_b], scale=1.0 / D, bias=EPS)
                C.op(DVE, lambda e: e.reciprocal(out=sv[:, 1:2], in_=sv[:, 1:2]), [sv_b], [sv_b])
                _stt(C, DVE, hf_[:], xt[:], sv[:, 1:2], gb[:], ALU.mult, ALU.mult, [xt_b, sv_b, gb_b], [hf_b])
                _cp(C, POOL, hb[:], hf_[:], [hf_b], [hb_b])
                C.dma(SP, h_tok[0][r0:r0 + 128, :], hb[:], [hb_b], [h_tok[1]])
                for kc in range(8):
                    C.op(PE, lambda e, kc=kc: e.transpose(out=ptr[:, kc, :], in_=hf_[:, kc * 128:(kc + 1) * 128], identity=identf[:]),
                         [hf_b, identf_b], [ptr_b])
                _cp(C, ACT, hT[:], ptr[:], [ptr_b], [hT_b])
                for kc in range(8):
                    _mm(C, psm[:, 0:16], hT[:, kc, :], rt[:, kc, :], [hT_b, rt_b], [psm_b], start=(kc == 0), stop=(kc == 7))
                _cp(C, DVE, lg[:], psm[:, 0:16], [psm_b], [lg_b])
                C.op(DVE, lambda e: e.tensor_reduce(out=sv[:, 2:3], in_=lg[:], axis=AX.X, op=ALU.max), [lg_b], [sv_b])
                _ts(C, DVE, sv[:, 2:3], sv[:, 2:3], -1.0, None, ALU.mult, None, [sv_b], [sv_b])
                _act(C, AF.Exp, af[:], lg[:], [lg_b, sv_b], [af_b, sv_b], bias=sv[:, 2:3], accum_out=sv[:, 3:4])
                C.op(DVE, lambda e: e.reciprocal(out=sv[:, 4:5], in_=sv[:, 3:4]), [sv_b], [sv_b])
                _ts(C, DVE, af[:], af[:], sv[:, 4:5], None, ALU.mult, None, [af_b, sv_b], [af_b])
                C.dma(SP, aff_tok[0][r0:r0 + 128, :], af[:], [af_b], [aff_tok[1]])
                C.op(PE, lambda e: e.transpose(out=psm[0:16, 128:256], in_=af[:], identity=identf[:]), [af_b, identf_b], [psm_b])
                _cp(C, ACT, aT[:], psm[0:16, 128:256], [psm_b], [aT_b])
                C.dma(ACT, affT[0][:, r0:r0 + 128], aT[:], [aT_b], [affT[1]])
    C.P.barrier()


def stage_moe(C, x_tok, h_tok, aff_tok, affT, wg_d, wu_d, wd_d, mc, msk_d, lst_d):
    CAP = 1024
    NE = 16
    with ExitStack() as st:
        thrb, thrb_b = C.sb(st, [128, 16], F32, "thrb")
        with ExitStack() as s1:
            aT, aT_b = C.sb(s1, [16, S], F32, "aT")
            jk, jk_b = C.sb(s1, [16, S], F32, "jk")
            C.dma(SP, aT[:], affT[0], [affT[1]], [aT_b])
            sv, sv_b = C.sb(s1, [16, 8], F32, "bs")
            id16, id16_b = C.sb(s1, [16, 16], F32, "id16")
            dg, dg_b = C.sb(s1, [16, 16], F32, "dg")
            on16, on16_b = C.sb(s1, [16, 128], F32, "on16")
            pb, pb_b = C.ps(s1, [128, 16], F32, "pb")
            C.dma(ACT, id16[:], msk_d[0][0:16, 4, 0:16], [msk_d[1]], [id16_b])
            C.op(POOL, lambda e: e.memset(on16[:], 1.0), [], [on16_b])
            C.op(DVE, lambda e: e.memset(sv[:, 0:1], 0.0), [], [sv_b])
            C.op(DVE, lambda e: e.memset(sv[:, 1:2], 1.0), [sv_b], [sv_b])
            for itn in range(34):
                _tt(C, DVE, sv[:, 2:3], sv[:, 0:1], sv[:, 1:2], ALU.add, [sv_b], [sv_b])
                _ts(C, DVE, sv[:, 2:3], sv[:, 2:3], 0.5, None, ALU.mult, None, [sv_b], [sv_b])
                _ts(C, DVE, jk[:], aT[:], sv[:, 2:3], None, ALU.is_ge, None, [aT_b, sv_b], [jk_b])
                C.op(DVE, lambda e: e.tensor_reduce(out=sv[:, 3:4], in_=jk[:], axis=AX.X, op=ALU.add), [jk_b], [sv_b])
                _ts(C, DVE, sv[:, 4:5], sv[:, 3:4], CAP - 0.5, None, ALU.is_ge, None, [sv_b], [sv_b])
                _tt(C, DVE, sv[:, 5:6], sv[:, 2:3], sv[:, 0:1], ALU.subtract, [sv_b], [sv_b])
                _tt(C, DVE, sv[:, 6:7], sv[:, 1:2], sv[:, 2:3], ALU.subtract, [sv_b], [sv_b])
                _stt(C, DVE, sv[:, 0:1], sv[:, 5:6], sv[:, 4:5], sv[:, 0:1], ALU.mult, ALU.add, [sv_b], [sv_b])
                _stt(C, DVE, sv[:, 1:2], sv[:, 6:7], sv[:, 4:5], sv[:, 2:3], ALU.mult, ALU.add, [sv_b], [sv_b])
            _ts(C, DVE, dg[:], id16[:], sv[:, 0:1], None, ALU.mult, None, [id16_b, sv_b], [dg_b])
            _mm(C, pb[:], on16[:], dg[:], [on16_b, dg_b], [pb_b])
            _cp(C, DVE, thrb[:], pb[:], [pb_b], [thrb_b])
        C.P.barrier()
        af, af_b = C.sb(st, [128, 64, 16], F32, "af")
        C.dma(SP, af[:], aff_tok[0].rearrange("(i p) e -> p i e", p=128), [aff_tok[1]], [af_b])
        posi, posi_b = C.sb(st, [128, 64, 16], I32, "posi")
        srcf, src_b = C.sb(st, [128, 64 * 16 * 2 + 16], F32, "src")
        src = srcf[:, 0:2048].rearrange("p (i e t) -> p i e t", e=16, t=2)
        C.op(POOL, lambda e: e.memset(srcf[:, 2048:2064], 0.0), [], [src_b])
        identb, identb_b = C.sb(st, [128, 128], BF16, "identb")
        C.op(POOL, lambda e: e.memset(identb[:], 0.0), [], [identb_b])
        C.op(POOL, lambda e: e.affine_select(out=identb[:], in_=identb[:], pattern=[[-1, 128]], compare_op=ALU.not_equal,
                                             fill=1.0, base=0, channel_multiplier=1), [identb_b], [identb_b])
        with ExitStack() as s2:
            mk, mk_b = C.sb(s2, [128, 64, 16], F32, "mk")
            posm, posm_b = C.sb(s2, [128, 64, 16], F32, "posm")
            tri, tri_b = C.sb(s2, [128, 128], F32, "tri")
            ones, ones_b = C.sb(s2, [128, 128], F32, "ones")
            tb, tb_b = C.sb(s2, [128, 66], F32, "tb")
            eo, eo_b = C.sb(s2, [128, 16], F32, "eo")
            totT, totT_b = C.sb(s2, [128, 16, 64], F32, "totT")
            inc, inc_b = C.sb(s2, [128, 16, 64], F32, "inc")
            rm2, rm2_b = C.sb(s2, [128, 16, 64], F32, "rm2")
            pw_ = [C.ps(s2, [128, 512], F32, "pw") for _ in range(2)]
            pt_ = [C.ps(s2, [128, 512], F32, "ptt") for _ in range(2)]
            C.dma(SP, tri[:], mc["tri"][0], [mc["tri"][1]], [tri_b])
            C.dma(SP, tb[:, 0:65], mc["tbase"][0], [mc["tbase"][1]], [tb_b])
            C.dma(SP, eo[:], mc["eoff"][0], [mc["eoff"][1]], [eo_b])
            _ts(C, DVE, tb[:, 65:66], tb[:, 64:65], 2048.0, None, ALU.add, None, [tb_b], [tb_b])
            C.op(POOL, lambda e: e.memset(ones[:], 1.0), [], [ones_b])
            C.op(POOL, lambda e: e.memset(rm2[:], 1.0), [], [rm2_b])
            C.op(POOL, lambda e: e.memset(rm2[:, :, 0:1], 0.0), [rm2_b], [rm2_b])
            _tt(C, DVE, mk[:], af[:], thrb[:].unsqueeze(1).to_broadcast([128, 64, 16]), ALU.is_ge, [af_b, thrb_b], [mk_b])
            mkf = mk[:].rearrange("p i e -> p (i e)")
            for half in range(2):
                _mm(C, pw_[half][0][:], tri[:], mkf[:, half * 512:(half + 1) * 512], [tri_b, mk_b], [pw_[half][1]])
                _mm(C, pt_[half][0][:], ones[:], mkf[:, half * 512:(half + 1) * 512], [ones_b, mk_b], [pt_[half][1]])
                _cp(C, DVE, totT[:].rearrange("p e i -> p i e")[:, half * 32:(half + 1) * 32, :],
                    pt_[half][0][:].rearrange("p (i e) -> p i e", e=16), [pt_[half][1]], [totT_b])
            C.op(DVE, lambda e: e.tensor_tensor_scan(out=inc[:].rearrange("p e i -> p (e i)"), data0=rm2[:].rearrange("p e i -> p (e i)"),
                                                     data1=totT[:].rearrange("p e i -> p (e i)"), initial=0.0, op0=ALU.mult, op1=ALU.add),
                 [rm2_b, totT_b], [inc_b])
            _tt(C, DVE, inc[:], inc[:], totT[:], ALU.subtract, [inc_b, totT_b], [inc_b])
            for half in range(2):
                _tt(C, DVE, posm[:, half * 32:(half + 1) * 32, :], pw_[half][0][:].rearrange("p (i e) -> p i e", e=16),
                    inc[:].rearrange("p e i -> p i e")[:, half * 32:(half + 1) * 32, :], ALU.add, [pw_[half][1], inc_b], [posm_b])
            _ts(C, DVE, posm[:], posm[:], tb[:, 65:66], None, ALU.subtract, None, [posm_b, tb_b], [posm_b])
            _tt(C, DVE, posm[:], posm[:], mk[:], ALU.mult, [posm_b, mk_b], [posm_b])
            _ts(C, DVE, posm[:], posm[:], tb[:, 65:66], None, ALU.add, None, [posm_b, tb_b], [posm_b])
            _tt(C, DVE, posm[:], posm[:], eo[:].unsqueeze(1).to_broadcast([128, 64, 16]), ALU.add, [posm_b, eo_b], [posm_b])
            _cp(C, DVE, posi[:], posm[:], [posm_b], [posi_b])
            _ts(C, POOL, src[:, :, :, 0], tb[:, 0:64].unsqueeze(2).to_broadcast([128, 64, 16]), tb[:, 64:65], None, ALU.add, None,
                [tb_b], [src_b])
            _cp(C, POOL, src[:, :, :, 1], af[:], [af_b], [src_b])
        C.P.barrier()
        sc_bufs = [[Buf("sc") for _ in range(64)] for _ in range(NE)]

        def scatter(e_):
            for i in range(64):
                _idma(C, lambda e, e_=e_, i=i: e.indirect_dma_start(
                    out=lst_d[0], out_offset=bass.IndirectOffsetOnAxis(ap=posi[:, i, e_:e_ + 1], axis=0),
                    in_=srcf[:, (i * 16 + e_) * 2:(i * 16 + e_) * 2 + 16], in_offset=None), [posi_b, src_b], [sc_bufs[e_][i]])
        civs = [C.sb(st, [128, 8, 2], F32, "civ") for _ in range(2)]
        idxs = [C.sb(st, [128, 8], I32, "idx") for _ in range(2)]
        xg, xg_b = C.sb(st, [128, 8, 1024], BF16, "xg")
        xgT, xgT_b = C.sb(st, [128, 8, 1024], BF16, "xgT")
        hid, hid_b = C.sb(st, [128, 8, 1024], BF16, "hid")
        Wgs = [C.sb(st, [128, 8, 1024], BF16, "Wg") for _ in range(1)]
        Wus = [C.sb(st, [128, 8, 1024], BF16, "Wu") for _ in range(1)]
        Wds = [C.sb(st, [128, 8, 1024], BF16, "Wd") for _ in range(1)]
        stg = [C.sb(st, [128, 4, 1024], F32, "estg") for _ in range(3)]
        sgs = [C.sb(st, [128, 512], F32, "sg") for _ in range(2)]
        yvs = [C.sb(st, [128, 1024], F32, "yv") for _ in range(4)]
        pxt, pxt_b = C.ps(st, [128, 8, 128], BF16, "pxt")
        pgs = [C.ps(st, [128, 512], F32, "pg") for _ in range(2)]
        pus = [C.ps(st, [128, 512], F32, "pu") for _ in range(2)]
        pys = [C.ps(st, [128, 512], F32, "py") for _ in range(2)]
        kk = [0, 0]

        def load_w(W_, W_b, srcw, e_):
            for hf in range(2):
                t, t_b = stg[kk[0] % 3]
                C.dma((SP, ACT)[kk[0] % 2], t[:], srcw[0][e_, hf * 512:(hf + 1) * 512, :].rearrange("(kc p) n -> p kc n", p=128),
                      [srcw[1]], [t_b])
                _cp(C, ACT, W_[:, hf * 4:(hf + 1) * 4, :], t[:], [t_b], [W_b])
                kk[0] += 1

        def gather(e_):
            civ, civ_b = civs[e_ % 2]
            idx, idx_b = idxs[e_ % 2]
            C.dma(SP, civ[:], lst_d[0][e_ * LSTN:e_ * LSTN + 1024, 0:2].rearrange("(cb p) two -> p cb two", p=128),
                  sc_bufs[e_], [civ_b])
            _cp(C, DVE, idx[:], civ[:, :, 0], [civ_b], [idx_b])
            for cb in range(8):
                _idma(C, lambda e, cb=cb, idx=idx: e.indirect_dma_start(
                    out=xg[:, cb, :], out_offset=None, in_=h_tok[0],
                    in_offset=bass.IndirectOffsetOnAxis(ap=idx[:, cb:cb + 1], axis=0)), [idx_b, h_tok[1]], [xg_b])

        def transposes():
            for cb in range(8):
                for kc in range(8):
                    C.op(PE, lambda e, cb=cb, kc=kc: e.transpose(out=pxt[:, kc, :], in_=xg[:, cb, kc * 128:(kc + 1) * 128],
                                                                 identity=identb[:]), [xg_b, identb_b], [pxt_b])
                _cp(C, (ACT, DVE)[cb % 2], xgT[:, :, cb * 128:(cb + 1) * 128], pxt[:], [pxt_b], [xgT_b])

        load_w(*Wgs[0], wg_d, 0)
        load_w(*Wus[0], wu_d, 0)
        load_w(*Wds[0], wd_d, 0)
        scatter(0)
        scatter(1)
        scatter(2)
        gather(0)
        for e_ in range(NE):
            civ, civ_b = civs[e_ % 2]
            idx, idx_b = idxs[e_ % 2]
            Wg, Wg_b = Wgs[0]
            Wu, Wu_b = Wus[0]
            Wd, Wd_b = Wds[0]
            transposes()
            if e_ + 1 < NE:
                gather(e_ + 1)
            for fc in range(8):
                for half in range(2):
                    hsl = slice(half * 512, (half + 1) * 512)
                    pg, pg_b = pgs[kk[1] % 2]
                    pu, pu_b = pus[kk[1] % 2]
                    sg, sg_b = sgs[kk[1] % 2]
                    kk[1] += 1
                    for kc in range(8):
                        _mm(C, pg[:], Wg[:, kc, fc * 128:(fc + 1) * 128], xgT[:, kc, hsl], [Wg_b, xgT_b], [pg_b], start=(kc == 0), stop=(kc == 7))
                    for kc in range(8):
                        _mm(C, pu[:], Wu[:, kc, fc * 128:(fc + 1) * 128], xgT[:, kc, hsl], [Wu_b, xgT_b], [pu_b], start=(kc == 0), stop=(kc == 7))
                    _act(C, AF.Silu, sg[:], pg[:], [pg_b], [sg_b])
                    _tt(C, DVE, hid[:, fc, hsl], pu[:], sg[:], ALU.mult, [pu_b, sg_b], [hid_b])
            if e_ + 1 < NE:
                load_w(Wg, Wg_b, wg_d, e_ + 1)
                load_w(Wu, Wu_b, wu_d, e_ + 1)
            for cb in range(8):
                yv, yv_b = yvs[cb % 4]
                for half in range(2):
                    py, py_b = pys[half]
                    for fc in range(8):
                        _mm(C, py[:], hid[:, fc, cb * 128:(cb + 1) * 128], Wd[:, fc, half * 512:(half + 1) * 512], [hid_b, Wd_b], [py_b],
                            start=(fc == 0), stop=(fc == 7))
                    _ts(C, DVE, yv[:, half * 512:(half + 1) * 512], py[:], civ[:, cb, 1:2], None, ALU.mult, None,
                        [py_b, civ_b], [yv_b])
                _idma(C, lambda e, cb=cb, yv=yv, idx=idx: e.indirect_dma_start(
                    out=x_tok[0], out_offset=bass.IndirectOffsetOnAxis(ap=idx[:, cb:cb + 1], axis=0), in_=yv[:], in_offset=None,
                    compute_op=ALU.add), [idx_b, yv_b, x_tok[1]], [x_tok[1]])
            if e_ + 1 < NE:
                load_w(Wd, Wd_b, wd_d, e_ + 1)
            if e_ + 3 < NE:
                scatter(e_ + 3)
    C.P.barrier()


def _idma(C, fn, R, W):
    return C.P.add(POOL, fn, R, W, dma=True)


def stage_final(C, x_tok, nf_d, out_d):
    with ExitStack() as st:
        gb, gb_b = C.sb(st, [128, D], F32, "gbf")
        C.dma(SP, gb[:], nf_d[0].partition_broadcast(128), [nf_d[1]], [gb_b])
        xts = [C.sb(st, [128, D], F32, "xt") for _ in range(3)]
        sq, sq_b = C.sb(st, [128, D], BF16, "sqj")
        svs = [C.sb(st, [128, 2], F32, "sv") for _ in range(3)]
        for i in range(NT):
            xt, xt_b = xts[i % 3]
            sv, sv_b = svs[i % 3]
            C.dma(SP, xt[:], x_tok[0][i * 128:(i + 1) * 128, :], [x_tok[1]], [xt_b])
            _act(C, AF.Square, sq[:], xt[:], [xt_b], [sq_b, sv_b], accum_out=sv[:, 0:1])
            _act(C, AF.Sqrt, sv[:, 1:2], sv[:, 0:1], [sv_b], [sv_b], scale=1.0 / D, bias=EPS)
            C.op(DVE, lambda e, sv=sv: e.reciprocal(out=sv[:, 1:2], in_=sv[:, 1:2]), [sv_b], [sv_b])
            _stt(C, DVE, xt[:], xt[:], sv[:, 1:2], gb[:], ALU.mult, ALU.mult, [xt_b, sv_b, gb_b], [xt_b])
            C.dma(ACT, out_d[0][i * 128:(i + 1) * 128, :], xt[:], [xt_b], [out_d[1]])


W_NAMES = ("norm_mix", "w_in", "rwkv_w2", "rwkv_a2", "rwkv_g2", "vres_w1", "vres_w2", "pool_w", "q_norm", "k_norm",
           "w_branch_rwkv", "w_branch_pool", "w_branch_attn", "w_out", "norm_ffn", "router", "exp_gate", "exp_up", "exp_down",
           "norm_final")
W_SHAPES = {"norm_mix": [2, D], "w_in": [2, D, NPROJ], "rwkv_w2": [2, 2, 64, 512], "rwkv_a2": [2, 2, 64, 512],
            "rwkv_g2": [2, 128, 512], "vres_w1": [1, D, 32], "vres_w2": [1, 32, 512], "pool_w": [2, 4, 128, 128],
            "q_norm": [2, 64], "k_norm": [2, 64], "w_branch_rwkv": [2, 512, D], "w_branch_pool": [2, 512, D],
            "w_branch_attn": [2, 512, D], "w_out": [2, D, D], "norm_ffn": [2, D], "router": [2, D, 16],
            "exp_gate": [2, 16, D, D], "exp_up": [2, 16, D, D], "exp_down": [2, 16, D, D], "norm_final": [D]}


def build_full(nlayers=2, do_final=True):
    nc = bass.Bass("TRN2", target_bir_lowering=False)
    C = Ctx(nc)
    x = C.dram("x", [S, D], F32, kind="ExternalInput")
    Wd = {n: C.dram(n, W_SHAPES[n], F32, kind="ExternalInput") for n in W_NAMES}
    pars = [C.dram(f"par{l}", [128, PAR_COLS], F32, kind="ExternalInput") for l in range(2)]
    hc = host_consts()
    mcn = moe_consts()
    cst = {k: C.dram(k, list(v.shape), F32, kind="ExternalInput") for k, v in hc.items()}
    mc = {k: C.dram(k, list(v.shape), F32, kind="ExternalInput") for k, v in mcn.items()}
    msk = C.dram("msk", [64, 5, 64], F32, kind="ExternalInput")
    out = C.dram("out", [S, D], F32, kind="ExternalOutput")
    x_tok = C.dram("x_tok", [S, D], F32)
    outs = {"prw": C.dram("prw", [1920, S], F32), "ppool": C.dram("ppool", [512, S], F32), "patt": C.dram("patt", [768, S], F32),
            "pgate": C.dram("pgate", [3072, S], BF16), "hv1": C.dram("hv1", [32, S], F32)}
    RO = {n: C.dram("o_" + n, [512, S], F32) for n in RNAMES + ("vfirst",)}
    y_d = [C.dram(f"ydir{d}", [S, 512], F32) for d in range(2)]
    yT = [C.dram(n, [512, S], BF16) for n in ("yaT", "ybT", "ycT")]
    h_tok = C.dram("h_tok", [S, D], BF16)
    aff_tok = C.dram("aff_tok", [S, 16], F32)
    affT = C.dram("affT", [16, S], F32)
    lst_d = C.dram("ranklist", [16 * LSTN, 16], F32)

    def sub(d, *idx):
        ap = d[0]
        for i in idx:
            ap = ap[i]
        return (ap, d[1])

    for l in range(nlayers):
        x_src = x if l == 0 else x_tok
        stage_proj(C, l, x_src, sub(Wd["w_in"], l), sub(Wd["norm_mix"], l), outs, sub(Wd["vres_w1"], 0) if l == 1 else None)
        C.P.barrier()
        vres = None
        if l == 1:
            vres = {"w2": sub(Wd["vres_w2"], 0), "hv1": outs["hv1"], "vfirst": RO["vfirst"]}
        stage_rwkv_prep(C, l, outs["prw"], pars[l], sub(Wd["rwkv_w2"], l), sub(Wd["rwkv_a2"], l), sub(Wd["rwkv_g2"], l),
                        cst["blk"], RO, vres)
        stage_rwkv_scan(C, RO, msk, y_d)
        stage_rwkv_out(C, y_d, RO, pars[l], yT[0])
        stage_pool(C, outs["ppool"], sub(Wd["pool_w"], l), pars[l], mc["invcnt"], yT[1])
        stage_attn(C, outs["patt"], sub(Wd["q_norm"], l), sub(Wd["k_norm"], l), cst, yT[2])
        stage_merge(C, x_src, x_tok, yT, outs["pgate"],
                    [sub(Wd["w_branch_rwkv"], l), sub(Wd["w_branch_pool"], l), sub(Wd["w_branch_attn"], l)],
                    sub(Wd["w_out"], l), sub(Wd["norm_ffn"], l), sub(Wd["router"], l), h_tok, aff_tok, affT)
        stage_moe(C, x_tok, h_tok, aff_tok, affT, sub(Wd["exp_gate"], l), sub(Wd["exp_up"], l), sub(Wd["exp_down"], l), mc, msk, lst_d)
    if do_final:
        stage_final(C, x_tok, Wd["norm_final"], out)
    emit_program(C)
    consts = dict(hc)
    consts.update(mcn)
    consts["msk"] = scan_consts()
    return nc, consts


def make_in_maps(inputs, cores):
    inp = {k: np.asarray(v) for k, v in inputs.items()}
    shared = {n: np.ascontiguousarray(inp[n], dtype=np.float32) for n in W_NAMES}
    shared["par0"] = pack_par(inp, 0)
    shared["par1"] = pack_par(inp, 1)
    maps = []
    for b in cores:
        m = dict(shared)
        m["x"] = np.ascontiguousarray(inp["x"][b], dtype=np.float32)
        maps.append(m)
    return maps


def kernel(**inputs):
    nc, consts = build_full()
    maps = make_in_maps(inputs, list(range(8)))
    for m in maps:
        m.update(consts)
    res = run_bass_kernel_spmd(nc, maps, core_ids=list(range(8)))
    return np.stack([np.asarray(r["out"], dtype=np.float32) for r in res.results], axis=0)
```

```python
import numpy as np
import ml_dtypes
import concourse.bass as bass
import concourse.mybir as mybir
from concourse.bass_utils import run_bass_kernel_spmd

F32 = mybir.dt.float32
BF16 = mybir.dt.bfloat16
I32 = mybir.dt.int32
U32 = mybir.dt.uint32
AF = mybir.ActivationFunctionType
ALU = mybir.AluOpType
AX = mybir.AxisListType

D = 1024
S = 8192
NT = S // 128
NG = S // 512
NPROJ = 6272
RW = 512
EPS = 1e-6

PE, ACT, DVE, POOL, SP = "pe", "act", "dve", "pool", "sp"
ENGS = (PE, ACT, DVE, POOL, SP)
NDMASEM = {"pe": 1, "act": 6, "dve": 1, "pool": 24, "sp": 8}
NRING2 = 6


class Buf:
    __slots__ = ("name", "w", "rd")

    def __init__(self, name=""):
        self.name = name
        self.w = None
        self.rd = []


class Op:
    __slots__ = ("eng", "fn", "dma", "deps", "needs_inc", "count", "slot", "slotcount", "idx")


class Prog:
    def __init__(self, nc):
        self.nc = nc
        self.ops = {e: [] for e in ENGS}
        self.dma_rr = {e: 0 for e in ENGS}
        self.dma_rr2 = {}
        self.dma_last = {}
        self.dma_cnt = {}
        self.nops = 0
        self.pending = {e: [] for e in ENGS}

    def barrier(self):
        deps = []
        for e in ENGS:
            for op in reversed(self.ops[e]):
                if not op.dma:
                    deps.append(op)
                    break
        deps.extend(self.dma_last.values())
        for e in ENGS:
            self.pending[e] = list(deps)

    def add(self, eng, fn, reads=(), writes=(), dma=False, bulk=False):
        op = Op()
        op.eng, op.fn, op.dma = eng, fn, dma
        op.needs_inc = False
        op.count = 0
        op.idx = self.nops
        self.nops += 1
        deps = []
        for b in reads:
            if b.w is not None:
                for w_ in b.w:
                    deps.append((w_, "raw"))
        for b in writes:
            if b.w is not None:
                for w_ in b.w:
                    deps.append((w_, "waw"))
            for r in b.rd:
                deps.append((r, "war"))
        for d in self.pending[eng]:
            deps.append((d, "bar"))
        self.pending[eng] = []
        if dma:
            if bulk:
                k2 = self.dma_rr2.get(eng, 0)
                self.dma_rr2[eng] = (k2 + 1) % NRING2
                k = NDMASEM[eng] + k2
            else:
                k = self.dma_rr[eng]
                self.dma_rr[eng] = (k + 1) % NDMASEM[eng]
            op.slot = k
            prev = self.dma_last.get((eng, k))
            if prev is not None:
                deps.append((prev, "slot"))
            self.dma_last[(eng, k)] = op
            c = self.dma_cnt.get((eng, k), 0) + 16
            self.dma_cnt[(eng, k)] = c
            op.slotcount = c
        fin = []
        for d, kind in deps:
            if d is op:
                continue
            if not d.dma and d.eng == eng:
                if eng == PE:
                    continue
                if kind == "bar" and not dma:
                    continue
            if not d.dma:
                d.needs_inc = True
            fin.append(d)
        op.deps = fin
        for b in reads:
            b.rd.append(op)
        for b in writes:
            if dma and b.w is not None and all(w_.dma for w_ in b.w):
                keep = [w_ for w_ in b.w if not (w_.eng == eng and w_.slot == op.slot)]
                b.w = keep + [op]
            else:
                b.w = [op]
            b.rd = []
        self.ops[eng].append(op)
        return op

    def emit(self, sems, dsems, engs):
        for e in ENGS:
            c = 0
            for op in self.ops[e]:
                if not op.dma and op.needs_inc:
                    c += 1
                    op.count = c
        for e in ENGS:
            eng = engs[e]
            waited = {}
            for op in self.ops[e]:
                need = {}
                for d in op.deps:
                    if d.dma:
                        key = ("d", d.eng, d.slot)
                        v = d.slotcount
                    else:
                        key = ("c", d.eng)
                        v = d.count
                    if need.get(key, 0) < v:
                        need[key] = v
                for key, v in need.items():
                    if waited.get(key, 0) >= v:
                        continue
                    waited[key] = v
                    sem = dsems[key[1]][key[2]] if key[0] == "d" else sems[key[1]]
                    eng.wait_ge(sem, v)
                ins = op.fn(eng)
                if op.dma:
                    ins.then_inc(dsems[e][op.slot], 16)
                elif op.needs_inc:
                    ins.then_inc(sems[e], 1)

    def final_wait(self, sems, dsems, engs):
        eng = engs[SP]
        for (e, k), c in self.dma_cnt.items():
            eng.wait_ge(dsems[e][k], c)


class Ctx:
    def __init__(self, nc):
        self.nc = nc
        self.P = Prog(nc)
        self.stack = None
        self.uid = 0

    def name(self, base):
        self.uid += 1
        return f"{base}_{self.uid}"

    def sb(self, st, shape, dt, name="t"):
        t = st.enter_context(self.nc.sbuf_tensor(self.name(name), list(shape), dt))
        return t, Buf(name)

    def ps(self, st, shape, dt, name="p"):
        t = st.enter_context(self.nc.psum_tensor(self.name(name), list(shape), dt))
        return t, Buf(name)

    def dram(self, name, shape, dt, kind="Internal"):
        return self.nc.dram_tensor(name, list(shape), dt, kind=kind).ap(), Buf(name)

    def dma(self, eng, out, in_, reads, writes, bulk=False, **kw):
        def fn(e):
            return e.dma_start(out=out, in_=in_, **kw)
        return self.P.add(eng, fn, reads, writes, dma=True, bulk=bulk and eng == POOL)

    def op(self, eng, fn, reads, writes):
        return self.P.add(eng, fn, reads, writes)


from contextlib import ExitStack


def act(func, out, in_, **kw):
    return lambda e: e.activation(out=out, in_=in_, func=func, **kw)


def stage_proj(C, l, x_tok, w_in_d, norm_d, outs, vres_w1_d=None):
    nc = C.nc
    ncols = NPROJ + (32 if vres_w1_d is not None else 0)
    nchunk = (ncols + 127) // 128
    with ExitStack() as st:
        w_sb, w_b = C.sb(st, [128, 8, ncols], BF16, "w_in")
        gb, gb_b = C.sb(st, [128, D], F32, "gbc")
        C.dma(SP, gb[:], norm_d[0].partition_broadcast(128), [norm_d[1]], [gb_b])
        WP = 784
        stg = [C.sb(st, [128, 8, WP], F32, "wstg") for _ in range(2)]
        wv = w_in_d[0].rearrange("(kc p) n -> p kc n", p=128)
        k = 0
        for c0 in range(0, NPROJ, WP):
            t, tb = stg[k % 2]
            C.dma(ACT if k % 2 else SP, t[:], wv[:, :, c0:c0 + WP], [w_in_d[1]], [tb])
            _cp(C, (ACT, DVE)[k % 2], w_sb[:, :, c0:c0 + WP], t[:], [tb], [w_b])
            k += 1
        if vres_w1_d is not None:
            t, tb = stg[k % 2]
            C.dma(SP, t[:, :, 0:32], vres_w1_d[0].rearrange("(kc p) n -> p kc n", p=128), [vres_w1_d[1]], [tb])
            C.op(POOL, lambda e, t=t: e.tensor_copy(out=w_sb[:, :, NPROJ:NPROJ + 32], in_=t[:, :, 0:32]), [tb], [w_b])
        ident, ident_b = C.sb(st, [128, 128], BF16, "ident")
        C.op(POOL, lambda e: e.memset(ident[:], 0.0), [], [ident_b])
        C.op(POOL, lambda e: e.affine_select(out=ident[:], in_=ident[:], pattern=[[-1, 128]], compare_op=ALU.not_equal,
                                             fill=1.0, base=0, channel_multiplier=1), [ident_b], [ident_b])
        xts = [C.sb(st, [128, D], F32, "xt") for _ in range(2)]
        hns = [C.sb(st, [128, D], BF16, "hn") for _ in range(2)]
        sq, sq_b = C.sb(st, [128, D], BF16, "sqj")
        sts = [C.sb(st, [128, 2], F32, "stat") for _ in range(2)]
        hnT = [C.sb(st, [128, 8, 512], BF16, "hnT") for _ in range(2)]
        tps = [C.ps(st, [128, 8, 128], BF16, "tps") for _ in range(2)]
        mps = [C.ps(st, [128, 512], F32, "mps") for _ in range(4)]
        ost = [C.sb(st, [128, 512], F32, "ost") for _ in range(4)]
        osb = [C.sb(st, [128, 512], BF16, "osb") for _ in range(4)]
        it = 0
        oc = 0
        for g in range(NG):
            hT, hT_b = hnT[g % 2]
            for s in range(4):
                xt, xt_b = xts[it % 2]
                hn, hn_b = hns[it % 2]
                stt, st_b = sts[it % 2]
                tp, tp_b = tps[it % 2]
                it += 1
                r0 = g * 512 + s * 128
                C.dma(SP, xt[:], x_tok[0][r0:r0 + 128, :], [x_tok[1]], [xt_b])
                C.op(ACT, act(AF.Square, sq[:], xt[:], accum_out=stt[:, 0:1]), [xt_b], [sq_b, st_b])
                C.op(ACT, act(AF.Sqrt, stt[:, 1:2], stt[:, 0:1], scale=1.0 / D, bias=EPS), [st_b], [st_b])
                C.op(DVE, lambda e, stt=stt: e.reciprocal(out=stt[:, 1:2], in_=stt[:, 1:2]), [st_b], [st_b])
                C.op(DVE, lambda e, stt=stt, xt=xt, hn=hn: e.scalar_tensor_tensor(
                    out=hn[:], in0=xt[:], scalar=stt[:, 1:2], in1=gb[:], op0=ALU.mult, op1=ALU.mult),
                    [st_b, xt_b, gb_b], [hn_b])
                for kc in range(8):
                    C.op(PE, lambda e, tp=tp, hn=hn, kc=kc: e.transpose(out=tp[:, kc, :], in_=hn[:, kc * 128:(kc + 1) * 128],
                                                                       identity=ident[:]), [hn_b, ident_b], [tp_b])
                C.op(ACT, lambda e, tp=tp, hT=hT, s=s: e.copy(out=hT[:, :, s * 128:(s + 1) * 128], in_=tp[:]), [tp_b], [hT_b])
            for c in range(nchunk):
                m = min(128, ncols - c * 128)
                mp, mp_b = mps[oc % 4]
                for kc in range(8):
                    C.op(PE, lambda e, mp=mp, c=c, kc=kc, hT=hT, m=m: e.matmul(
                        out=mp[0:m, :], lhsT=w_sb[:, kc, c * 128:c * 128 + m], rhs=hT[:, kc, :],
                        start=(kc == 0), stop=(kc == 7)), [w_b, hT_b], [mp_b])
                col = c * 128
                tsl = slice(g * 512, (g + 1) * 512)
                if col < 1920:
                    dst, off = outs["prw"], col
                elif col < 2432:
                    dst, off = outs["ppool"], col - 1920
                elif col < 3200:
                    dst, off = outs["patt"], col - 2432
                elif col < NPROJ:
                    dst, off = outs["pgate"], col - 3200
                else:
                    dst, off = outs["hv1"], 0
                if dst is outs["pgate"]:
                    o, o_b = osb[oc % 4]
                    C.op(ACT, act(AF.Sigmoid, o[:], mp[:]), [mp_b], [o_b])
                else:
                    o, o_b = ost[oc % 4]
                    C.op(DVE, lambda e, o=o, mp=mp, m=m: e.tensor_copy(out=o[0:m, :], in_=mp[0:m, :]), [mp_b], [o_b])
                C.dma(SP if oc % 2 else POOL, dst[0][off:off + m, tsl], o[0:m, :], [o_b], [dst[1]])
                oc += 1


def emit_program(C):
    nc = C.nc
    with ExitStack() as st:
        sems = {e: st.enter_context(nc.semaphore(f"s_{e}")) for e in ENGS}
        dsems = {e: [st.enter_context(nc.semaphore(f"d_{e}{k}")) for k in range(NDMASEM[e] + (NRING2 if e == POOL else 0))] for e in ENGS}
        engs = {PE: nc.tensor, ACT: nc.scalar, DVE: nc.vector, POOL: nc.gpsimd, SP: nc.sync}
        C.P.emit(sems, dsems, engs)
        C.P.final_wait(sems, dsems, engs)


def host_consts():
    t = np.arange(S)
    row = (t // 64).astype(np.float32)
    col = (t % 64).astype(np.float32)
    freqs = (10000.0 ** (-np.arange(0, 32, 2, dtype=np.float32) / 32)).astype(np.float32)
    ang = np.concatenate([row[:, None] * freqs, col[:, None] * freqs], axis=-1).astype(np.float32)
    cos = np.cos(ang).astype(np.float32)
    sin = np.sin(ang).astype(np.float32)
    pidx = (np.arange(128) % 64) // 2
    ctab = np.ascontiguousarray(cos[:, pidx].T)
    stab = np.ascontiguousarray(sin[:, pidx].T)
    prot = np.zeros((128, 128), np.float32)
    for i in range(64):
        prot[2 * i + 1, 2 * i] = -1.0
        prot[2 * i, 2 * i + 1] = 1.0
    blk = np.zeros((128, 128), np.float32)
    blk[:64, :64] = 1.0
    blk[64:, 64:] = 1.0
    return {"ctab": ctab, "stab": stab, "prot": prot, "blk": blk}


def stage_attn(C, patt, qn_d, kn_d, cst, ycT):
    with ExitStack() as st:
        qT, qT_b = C.sb(st, [128, 4, S], BF16, "qT")
        kT2, kT_b = C.sb(st, [128, 2, 2, S], BF16, "kTz")
        Vx, Vx_b = C.sb(st, [128, NT, 2, 65], BF16, "Vx")
        ones, ones_b = C.sb(st, [128, 128], F32, "ones")
        C.op(POOL, lambda e: e.memset(ones[:], 1.0), [], [ones_b])
        C.op(POOL, lambda e: e.memset(Vx[:, :, :, 0:1], 1.0), [], [Vx_b])
        C.op(POOL, lambda e: e.memset(kT2[:], 0.0), [], [kT_b])
        with ExitStack() as s1:
            blk, blk_b = C.sb(s1, [128, 128], F32, "blk")
            prot, prot_b = C.sb(s1, [128, 128], F32, "prot")
            identf, identf_b = C.sb(s1, [128, 128], F32, "identf")
            gq, gq_b = C.sb(s1, [128, 2], F32, "gqk")
            C.dma(SP, blk[:], cst["blk"][0], [cst["blk"][1]], [blk_b])
            C.dma(SP, prot[:], cst["prot"][0], [cst["prot"][1]], [prot_b])
            for hh in range(2):
                C.dma(SP, gq[hh * 64:(hh + 1) * 64, 0:1], qn_d[0].rearrange("(p o) -> p o", o=1), [qn_d[1]], [gq_b])
                C.dma(SP, gq[hh * 64:(hh + 1) * 64, 1:2], kn_d[0].rearrange("(p o) -> p o", o=1), [kn_d[1]], [gq_b])
            C.op(POOL, lambda e: e.memset(identf[:], 0.0), [], [identf_b])
            C.op(POOL, lambda e: e.affine_select(out=identf[:], in_=identf[:], pattern=[[-1, 128]], compare_op=ALU.not_equal,
                                                 fill=1.0, base=0, channel_multiplier=1), [identf_b], [identf_b])
            qcs = [C.sb(s1, [128, 512], F32, "qc") for _ in range(2)]
            ctb = [C.sb(s1, [128, 2, 512], F32, "cs") for _ in range(2)]
            sq, sq_b = C.sb(s1, [128, 512], F32, "sq")
            rs, rs_b = C.sb(s1, [128, 512], F32, "rs")
            qn, qn_b = C.sb(s1, [128, 512], F32, "qn")
            o1, o1_b = C.sb(s1, [128, 512], F32, "o1")
            o2, o2_b = C.sb(s1, [128, 512], F32, "o2")
            ssp = [C.ps(s1, [128, 512], F32, "ssp") for _ in range(2)]
            rtp = [C.ps(s1, [128, 512], F32, "rtp") for _ in range(2)]
            vtp = [C.ps(s1, [128, 128], F32, "vtp") for _ in range(2)]
            it = 0
            vi = 0
            for g in range(NG):
                tsl = slice(g * 512, (g + 1) * 512)
                cs, cs_b = ctb[g % 2]
                C.dma(SP, cs[:, 0, :], cst["ctab"][0][:, tsl], [cst["ctab"][1]], [cs_b])
                C.dma(SP, cs[:, 1, :], cst["stab"][0][:, tsl], [cst["stab"][1]], [cs_b])
                for ch in range(6):
                    qc, qc_b = qcs[it % 2]
                    sp_, sp_b = ssp[it % 2]
                    rp, rp_b = rtp[it % 2]
                    it += 1
                    if ch < 4:
                        C.dma(ACT, qc[:], patt[0][ch * 128:(ch + 1) * 128, tsl], [patt[1]], [qc_b])
                        gcol = gq[:, 0:1]
                        dst = qT[:, ch, tsl]
                        dst_b = qT_b
                    else:
                        r0 = 512 + (ch - 4) * 64
                        C.dma(ACT, qc[0:64, :], patt[0][r0:r0 + 64, tsl], [patt[1]], [qc_b])
                        C.dma(ACT, qc[64:128, :], patt[0][r0:r0 + 64, tsl], [patt[1]], [qc_b])
                        gcol = gq[:, 1:2]
                        dst = None
                        dst_b = kT_b
                    C.op(ACT, act(AF.Square, sq[:], qc[:]), [qc_b], [sq_b])
                    C.op(PE, lambda e, sp_=sp_: e.matmul(out=sp_[:], lhsT=blk[:], rhs=sq[:], start=True, stop=True),
                         [blk_b, sq_b], [sp_b])
                    C.op(ACT, act(AF.Sqrt, rs[:], sp_[:], scale=1.0 / 64, bias=EPS), [sp_b], [rs_b])
                    C.op(DVE, lambda e: e.reciprocal(out=rs[:], in_=rs[:]), [rs_b], [rs_b])
                    C.op(DVE, lambda e, qc=qc, gcol=gcol: e.scalar_tensor_tensor(out=qn[:], in0=qc[:], scalar=gcol, in1=rs[:],
                                                                                 op0=ALU.mult, op1=ALU.mult),
                         [qc_b, gq_b, rs_b], [qn_b])
                    C.op(PE, lambda e, rp=rp: e.matmul(out=rp[:], lhsT=prot[:], rhs=qn[:], start=True, stop=True),
                         [prot_b, qn_b], [rp_b])
                    C.op(POOL, lambda e, cs=cs: e.tensor_tensor(out=o1[:], in0=qn[:], in1=cs[:, 0, :], op=ALU.mult),
                         [qn_b, cs_b], [o1_b])
                    C.op(DVE, lambda e, cs=cs, rp=rp: e.tensor_tensor(out=o2[:], in0=rp[:], in1=cs[:, 1, :], op=ALU.mult),
                         [rp_b, cs_b], [o2_b])
                    if dst is not None:
                        C.op(POOL, lambda e, dst=dst: e.tensor_tensor(out=dst, in0=o1[:], in1=o2[:], op=ALU.add),
                             [o1_b, o2_b], [dst_b])
                    else:
                        for v_ in range(2):
                            psl = slice(v_ * 64, (v_ + 1) * 64)
                            _tt(C, POOL, kT2[psl, ch - 4, v_, tsl], o1[psl, :], o2[psl, :], ALU.add, [o1_b, o2_b], [dst_b])
                qc, qc_b = qcs[it % 2]
                it += 1
                C.dma(ACT, qc[:], patt[0][640:768, tsl], [patt[1]], [qc_b])
                for s in range(4):
                    vp, vp_b = vtp[vi % 2]
                    vi += 1
                    C.op(PE, lambda e, vp=vp, qc=qc, s=s: e.transpose(out=vp[:], in_=qc[:, s * 128:(s + 1) * 128],
                                                                      identity=identf[:]), [qc_b, identf_b], [vp_b])
                    ti = g * 4 + s
                    C.op(DVE, lambda e, vp=vp, ti=ti: e.tensor_copy(
                        out=Vx[:, ti, :, 1:65], in_=vp[:].rearrange("p (k d) -> p k d", k=2)), [vp_b], [Vx_b])
        C.P.barrier()
        with ExitStack() as s2:
            sps = [C.ps(s2, [128, 1024], F32, "sps") for _ in range(2)]
            ots = [C.ps(s2, [128, 512], F32, "ot") for _ in range(2)]
            bcp, bcp_b = C.ps(s2, [128, 512], F32, "bcp")
            pts = [C.sb(s2, [128, 1024], BF16, "pt") for _ in range(2)]
            osbs = [C.sb(s2, [128, 512], F32, "osb") for _ in range(2)]
            rec, rec_b = C.sb(s2, [1, 512], F32, "rec")
            ysb = [C.sb(s2, [128, 512], BF16, "ysb") for _ in range(2)]
            oi = 0
            iters = [(h, q2, stl) for h in range(8) for q2 in range(8) for stl in range(NT)]

            def emit_S(n):
                h, q2, stl = iters[n]
                kvh, ch, base = h // 4, h // 2, (h % 2) * 64
                q0 = q2 * 1024
                sp_, sp_b = sps[n % 2]
                for half in range(2):
                    C.op(PE, lambda e, sp_=sp_, half=half, stl=stl, kvh=kvh, ch=ch, base=base, q0=q0: e.matmul(
                        out=sp_[:, half * 512:(half + 1) * 512],
                        lhsT=kT2[:, kvh, base // 64, stl * 128:(stl + 1) * 128],
                        rhs=qT[:, ch, q0 + half * 512:q0 + (half + 1) * 512],
                        start=True, stop=True), [kT_b, qT_b], [sp_b])

            emit_S(0)
            for n, (h, q2, stl) in enumerate(iters):
                kvh = h // 4
                q0 = q2 * 1024
                if n + 1 < len(iters):
                    emit_S(n + 1)
                sp_, sp_b = sps[n % 2]
                pt, pt_b = pts[n % 2]
                C.op(ACT, act(AF.Exp, pt[:], sp_[:], scale=0.125), [sp_b], [pt_b])
                for half in range(2):
                    ot, ot_b = ots[half]
                    C.op(PE, lambda e, ot=ot, pt=pt, half=half, stl=stl, kvh=kvh: e.matmul(
                        out=ot[0:65, :], lhsT=Vx[:, stl, kvh, :], rhs=pt[:, half * 512:(half + 1) * 512],
                        start=(stl == 0), stop=(stl == NT - 1)), [Vx_b, pt_b], [ot_b])
                if stl == NT - 1:
                    for half in range(2):
                        ot, ot_b = ots[half]
                        osb, osb_b = osbs[oi % 2]
                        y, y_b = ysb[oi % 2]
                        oi += 1
                        C.op(DVE, lambda e, osb=osb, ot=ot: e.tensor_copy(out=osb[0:65, :], in_=ot[0:65, :]), [ot_b], [osb_b])
                        C.op(DVE, lambda e, osb=osb: e.reciprocal(out=rec[:], in_=osb[0:1, :]), [osb_b], [rec_b])
                        C.op(PE, lambda e: e.matmul(out=bcp[0:65, :], lhsT=ones[0:1, 0:65], rhs=rec[:], start=True, stop=True),
                             [ones_b, rec_b], [bcp_b])
                        C.op(DVE, lambda e, y=y, osb=osb: e.tensor_tensor(out=y[0:65, :], in0=osb[0:65, :], in1=bcp[0:65, :],
                                                                          op=ALU.mult), [osb_b, bcp_b], [y_b])
                        c0 = q0 + half * 512
                        C.dma(SP, ycT[0][h * 64:(h + 1) * 64, c0:c0 + 512], y[1:65, :], [y_b], [ycT[1]])
        C.P.barrier()


def _tt(C, eng, out, in0, in1, op, R, W):
    return C.op(eng, lambda e: e.tensor_tensor(out=out, in0=in0, in1=in1, op=op), R, W)


def _ts(C, eng, out, in0, s1, s2, op0, op1, R, W):
    if s2 is None:
        return C.op(eng, lambda e: e.tensor_scalar(out=out, in0=in0, scalar1=s1, scalar2=None, op0=op0), R, W)
    return C.op(eng, lambda e: e.tensor_scalar(out=out, in0=in0, scalar1=s1, scalar2=s2, op0=op0, op1=op1), R, W)


def _stt(C, eng, out, in0, scalar, in1, op0, op1, R, W):
    return C.op(eng, lambda e: e.scalar_tensor_tensor(out=out, in0=in0, scalar=scalar, in1=in1, op0=op0, op1=op1), R, W)


def _act(C, func, out, in_, R, W, **kw):
    return C.op(ACT, lambda e: e.activation(out=out, in_=in_, func=func, **kw), R, W)


def _mm(C, out, lhsT, rhs, R, W, start=True, stop=True):
    return C.op(PE, lambda e: e.matmul(out=out, lhsT=lhsT, rhs=rhs, start=start, stop=stop), R, W)


def _cp(C, eng, out, in_, R, W):
    if eng == ACT:
        return C.op(ACT, lambda e: e.copy(out=out, in_=in_), R, W)
    return C.op(eng, lambda e: e.tensor_copy(out=out, in_=in_), R, W)


PAR_COLS = 74
LAM = 0.6065306597126334


def pack_par(inp, l):
    def pc(v):
        return np.asarray(v, np.float32).reshape(-1, 128).T
    cols = [pc(inp["shift_prev"][l]), pc(inp["shift_next"][l]), pc(inp["rwkv_k_k"][l]), pc(inp["rwkv_k_a"][l]),
            pc(inp["rwkv_r_k"][l]), pc(inp["rwkv_w0"][l][0]), pc(inp["rwkv_w0"][l][1]), pc(inp["rwkv_a0"][l][0]),
            pc(inp["rwkv_a0"][l][1]),
            pc(inp["vres_v0"][l - 1]) if l > 0 else np.zeros((128, 4), np.float32),
            pc(inp["rwkv_ln_w"][l]), pc(inp["rwkv_ln_b"][l]), pc(inp["pool_scale"][l])]
    return np.ascontiguousarray(np.concatenate(cols, axis=1))


def stage_rwkv_prep(C, l, prw, par_d, w2_d, a2_d, g2_d, blk_d, RO, vres=None):
    with ExitStack() as st:
        par, par_b = C.sb(st, [128, PAR_COLS], F32, "par")
        dv, dv_b = C.sb(st, [128, 19], F32, "dv")
        w2s, w2_b = C.sb(st, [128, 512], F32, "w2s")
        a2s, a2_b = C.sb(st, [128, 512], F32, "a2s")
        g2s, g2_b = C.sb(st, [128, 512], F32, "g2s")
        blk, blk_b = C.sb(st, [128, 128], F32, "blk")
        C.dma(SP, par[:], par_d[0], [par_d[1]], [par_b])
        C.dma(SP, w2s[:], w2_d[0].rearrange("d l c -> (d l) c"), [w2_d[1]], [w2_b])
        C.dma(SP, a2s[:], a2_d[0].rearrange("d l c -> (d l) c"), [a2_d[1]], [a2_b])
        C.dma(SP, g2s[:], g2_d[0], [g2_d[1]], [g2_b])
        C.dma(SP, blk[:], blk_d[0], [blk_d[1]], [blk_b])
        if vres is not None:
            vw2, vw2_b = C.sb(st, [32, 512], F32, "vw2")
            C.dma(SP, vw2[:], vres["w2"][0], [vres["w2"][1]], [vw2_b])
        _tt(C, DVE, dv[:, 0:15], par[:, 0:15], par[:, 15:30], ALU.add, [par_b], [dv_b])
        _ts(C, DVE, dv[:, 0:15], dv[:, 0:15], -1.0, 1.0, ALU.mult, ALU.add, [dv_b], [dv_b])
        _ts(C, DVE, dv[:, 15:19], par[:, 34:38], -1.0, 1.0, ALU.mult, ALU.add, [par_b], [dv_b])
        dqc = [0]

        def grp_gen(stream):
            raws = [C.sb(st, [128, 514], F32, "raw") for _ in range(3)]
            sh = [C.sb(st, [128, 512], F32, "sh") for _ in range(15)]
            tmp = [C.sb(st, [128, 512], F32, "tmp") for _ in range(6)]
            outb = [C.sb(st, [128, 512], F32, "outb") for _ in range(8)]
            pss = [C.ps(st, [128, 512], F32, "rps") for _ in range(4)]
            cnt = {"raw": 0, "tmp": 0, "out": 0, "ps": 0}

            def nxt(lst, key):
                x = lst[cnt[key] % len(lst)]
                cnt[key] += 1
                return x

            def store(name, c, tsl, t, t_b):
                eng = (SP, ACT, POOL)[dqc[0] % 3]
                dqc[0] += 1
                C.dma(eng, RO[name][0][c * 128:(c + 1) * 128, tsl], t[:], [t_b], [RO[name][1]])

            for g in range(stream, NG, 2):
                t0 = g * 512
                tsl = slice(t0, t0 + 512)
                for c in range(15):
                    raw, raw_b = nxt(raws, "raw")
                    lo = max(t0 - 1, 0)
                    hi = min(t0 + 513, S)
                    if g == 0:
                        C.op(POOL, lambda e, raw=raw: e.memset(raw[:, 0:1], 0.0), [], [raw_b])
                    if g == NG - 1:
                        C.op(POOL, lambda e, raw=raw: e.memset(raw[:, 513:514], 0.0), [], [raw_b])
                    C.dma(SP if c % 2 else ACT, raw[:, lo - (t0 - 1):hi - (t0 - 1)], prw[0][c * 128:(c + 1) * 128, lo:hi],
                          [prw[1]], [raw_b])
                    s_, s_b = sh[c]
                    _act(C, AF.Copy, s_[:], raw[:, 1:513], [raw_b, dv_b], [s_b], scale=dv[:, c:c + 1])
                    _stt(C, DVE, s_[:], raw[:, 0:512], par[:, c:c + 1], s_[:], ALU.mult, ALU.add, [raw_b, par_b, s_b], [s_b])
                    _stt(C, DVE, s_[:], raw[:, 2:514], par[:, 15 + c:16 + c], s_[:], ALU.mult, ALU.add, [raw_b, par_b, s_b], [s_b])
                    yield
                twd, twd_b = sh[12]
                sad, sad_b = sh[13]
                sgd, sgd_b = sh[14]
                _act(C, AF.Tanh, twd[:], twd[:], [twd_b], [twd_b])
                _act(C, AF.Sigmoid, sgd[:], sgd[:], [sgd_b], [sgd_b])
                for c in range(4):
                    csl = slice(c * 128, (c + 1) * 128)
                    r_, r_b = sh[c]
                    k_, k_b = sh[4 + c]
                    v_, v_b = sh[8 + c]
                    store("r", c, tsl, r_, r_b)
                    av = []
                    for d in range(2):
                        dsl = slice(d * 64, (d + 1) * 64)
                        ps, ps_b = nxt(pss, "ps")
                        _mm(C, ps[:], w2s[dsl, csl], twd[dsl, :], [w2_b, twd_b], [ps_b])
                        o, o_b = nxt(outb, "out")
                        _act(C, AF.Sigmoid, o[:], ps[:], [ps_b, par_b], [o_b], bias=par[:, 42 + 4 * d + c:43 + 4 * d + c])
                        store(f"sg{d}", c, tsl, o, o_b)
                        ps, ps_b = nxt(pss, "ps")
                        _mm(C, ps[:], a2s[dsl, csl], sad[dsl, :], [a2_b, sad_b], [ps_b])
                        o, o_b = nxt(outb, "out")
                        _act(C, AF.Sigmoid, o[:], ps[:], [ps_b, par_b], [o_b], bias=par[:, 50 + 4 * d + c:51 + 4 * d + c])
                        store(f"a{d}", c, tsl, o, o_b)
                        av.append((o, o_b))
                    ps, ps_b = nxt(pss, "ps")
                    _mm(C, ps[:], g2s[:, csl], sgd[:], [g2_b, sgd_b], [ps_b])
                    o, o_b = nxt(outb, "out")
                    _cp(C, ACT, o[:], ps[:], [ps_b], [o_b])
                    store("g", c, tsl, o, o_b)
                    yield
                    if vres is not None:
                        hv, hv_b = nxt(tmp, "tmp")
                        C.dma(SP, hv[0:32, :], vres["hv1"][0][:, tsl], [vres["hv1"][1]], [hv_b])
                        vf, vf_b = nxt(tmp, "tmp")
                        C.dma(ACT, vf[:], vres["vfirst"][0][csl, tsl], [vres["vfirst"][1]], [vf_b])
                        ps, ps_b = nxt(pss, "ps")
                        _mm(C, ps[:], vw2[:, csl], hv[0:32, :], [vw2_b, hv_b], [ps_b])
                        mx, mx_b = nxt(tmp, "tmp")
                        _act(C, AF.Sigmoid, mx[:], ps[:], [ps_b, par_b], [mx_b], bias=par[:, 58 + c:59 + c])
                        _tt(C, DVE, vf[:], vf[:], v_[:], ALU.subtract, [vf_b, v_b], [vf_b])
                        _tt(C, DVE, vf[:], vf[:], mx[:], ALU.mult, [vf_b, mx_b], [vf_b])
                        _tt(C, DVE, v_[:], v_[:], vf[:], ALU.add, [v_b, vf_b], [v_b])
                    else:
                        store("vfirst", c, tsl, v_, v_b)
                    store("v", c, tsl, v_, v_b)
                    sq, sq_b = nxt(tmp, "tmp")
                    _act(C, AF.Square, sq[:], k_[:], [k_b, par_b], [sq_b], scale=par[:, 30 + c:31 + c])
                    ps, ps_b = nxt(pss, "ps")
                    _mm(C, ps[:], blk[:], sq[:], [blk_b, sq_b], [ps_b])
                    nr, nr_b = nxt(tmp, "tmp")
                    _act(C, AF.Sqrt, nr[:], ps[:], [ps_b], [nr_b])
                    _ts(C, DVE, nr[:], nr[:], 1e-12, None, ALU.max, None, [nr_b], [nr_b])
                    C.op(DVE, lambda e, nr=nr: e.reciprocal(out=nr[:], in_=nr[:]), [nr_b], [nr_b])
                    o, o_b = nxt(outb, "out")
                    _stt(C, DVE, o[:], k_[:], par[:, 30 + c:31 + c], nr[:], ALU.mult, ALU.mult, [k_b, par_b, nr_b], [o_b])
                    store("kk", c, tsl, o, o_b)
                    yield
                    kds = []
                    for d in range(2):
                        a_, a_b = av[d]
                        o, o_b = nxt(outb, "out")
                        _ts(C, POOL, o[:], a_[:], par[:, 34 + c:35 + c], dv[:, 15 + c:16 + c], ALU.mult, ALU.add,
                            [a_b, par_b, dv_b], [o_b])
                        _tt(C, POOL, o[:], o[:], k_[:], ALU.mult, [o_b, k_b], [o_b])
                        store(f"kd{d}", c, tsl, o, o_b)
                        kds.append((o, o_b))
                    ks, ks_b = nxt(tmp, "tmp")
                    _tt(C, DVE, ks[:], kds[0][0][:], kds[1][0][:], ALU.add, [kds[0][1], kds[1][1]], [ks_b])
                    _stt(C, DVE, ks[:], r_[:], par[:, 38 + c:39 + c], ks[:], ALU.mult, ALU.mult, [r_b, par_b, ks_b], [ks_b])
                    ps, ps_b = nxt(pss, "ps")
                    _mm(C, ps[:], blk[:], ks[:], [blk_b, ks_b], [ps_b])
                    o, o_b = nxt(outb, "out")
                    _tt(C, DVE, o[:], ps[:], v_[:], ALU.mult, [ps_b, v_b], [o_b])
                    store("bonus", c, tsl, o, o_b)
                    yield

        gens = [grp_gen(0), grp_gen(1)]
        while gens:
            for gq in list(gens):
                try:
                    next(gq)
                except StopIteration:
                    gens.remove(gq)
    C.P.barrier()


RNAMES = ("r", "v", "g", "bonus", "kk", "kd0", "kd1", "a0", "a1", "sg0", "sg1")


def scan_consts():
    i = np.arange(64)
    lo = (i[None, :] < i[:, None]).astype(np.float32)
    up = (i[None, :] > i[:, None]).astype(np.float32)
    loi = (i[None, :] <= i[:, None]).astype(np.float32)
    upi = (i[None, :] >= i[:, None]).astype(np.float32)
    idn = np.eye(64, dtype=np.float32)
    return np.ascontiguousarray(np.stack([lo, up, loi, upi, idn], axis=1))


def stage_rwkv_scan(C, RO, msk_d, y_d):
    GT = 128
    with ExitStack() as st:
        msk, msk_b = C.sb(st, [64, 5, 64], F32, "msk")
        C.dma(SP, msk[:], msk_d[0], [msk_d[1]], [msk_b])
        idnb, idnb_b = C.sb(st, [64, 64], BF16, "idnb")
        _cp(C, DVE, idnb[:], msk[:, 4, :], [msk_b], [idnb_b])
        rmask, rmask_b = C.sb(st, [64, 8, GT], F32, "rmask")
        C.op(POOL, lambda e: e.memset(rmask[:], 1.0), [], [rmask_b])
        C.op(POOL, lambda e: e.memset(rmask[:].rearrange("p h (c t) -> p (h c) t", t=64)[:, :, 0:1], 0.0), [rmask_b], [rmask_b])
        pss = [C.ps(st, [64, 8, 64], F32, "sps") for _ in range(6)]
        tpss = [C.ps(st, [64, 8, 64], BF16, "tps") for _ in range(2)]
        pc = [0, 0]
        NCH = GT // 64

        def nps():
            x = pss[pc[0] % 6]
            pc[0] += 1
            return x

        def ntps():
            x = tpss[pc[1] % 2]
            pc[1] += 1
            return x

        def mm8(lhs_fn, rhs_fn, R):
            p, p_b = nps()
            for h in range(8):
                _mm(C, p[:, h, :], lhs_fn(h), rhs_fn(h), R, [p_b])
            return p, p_b

        def flat(t):
            return t[:].rearrange("p h t -> p (h t)")

        def v4(t):
            return t[:].rearrange("p h (c t) -> p (h c) t", t=64)

        bufs = []
        for d in range(2):
            H = C.sb(st, [64, 8, 64], F32, "H")
            Hb = C.sb(st, [64, 8, 64], BF16, "Hb")
            names = ["r", "v", "kk", "kd", "a", "sg", "cs", "e2", "Gi"] + (["cs2"] if d == 1 else [])
            G = {n: C.sb(st, [64, 8, GT], F32, "g_" + n) for n in names}
            Gb = {n: C.sb(st, [64, 8, GT], BF16, "gb_" + n) for n in ("At", "Bt", "Kt", "Rt", "Bh", "Kh", "vb")}
            Cb = {n: C.sb(st, [64, 8, 64], BF16, "c_" + n) for n in
                  ("Vt", "Bht", "Kht", "Pa", "PTa", "Pb", "PTb", "TT", "AakT", "ArbT", "ArkT", "W", "U")}
            Cf = {n: C.sb(st, [64, 8, 64], F32, "c_" + n) for n in ("WV", "YV", "ZV", "Yo", "Ht")}
            bufs.append((H, Hb, G, Gb, Cb, Cf))

        def dir_gen(d):
            (H, H_b), (Hb, Hb_b), G, Gb, Cb, Cf = bufs[d]
            NS = NCH * 8
            mN = msk[:, 0 if d == 0 else 1, :].unsqueeze(1).to_broadcast([64, 8, 64])
            mNT = msk[:, 1 if d == 0 else 0, :].unsqueeze(1).to_broadcast([64, 8, 64])
            mI = msk[:, 3 if d == 0 else 2, :].unsqueeze(1).to_broadcast([64, 8, 64])
            idb = msk[:, 4, :].unsqueeze(1).to_broadcast([64, 8, 64])
            dq_e = (SP, ACT) if d == 0 else (ACT, SP)
            C.op(DVE, lambda e: e.memset(H[:], 0.0), [], [H_b])
            C.op(DVE, lambda e: e.memset(Hb[:], 0.0), [], [Hb_b])
            for gi in range(S // GT):
                g = gi if d == 0 else S // GT - 1 - gi
                t0 = g * GT
                dq = 0
                for n, src in (("r", "r"), ("v", "v"), ("kk", "kk"), ("kd", f"kd{d}"), ("a", f"a{d}"), ("sg", f"sg{d}")):
                    t, t_b = G[n]
                    C.dma(dq_e[dq % 2], t[:], RO[src][0][:, t0:t0 + GT].rearrange("(h j) t -> j h t", j=64),
                          [RO[src][1]], [t_b])
                    dq += 1
                r_, r_b = G["r"]; v_, v_b = G["v"]; kk_, kk_b = G["kk"]; kd_, kd_b = G["kd"]; a_, a_b = G["a"]
                sg_, sg_b = G["sg"]; cs_, cs_b = G["cs"]; e2_, e2_b = G["e2"]; Gi_, Gi_b = G["Gi"]
                At_, At_b = Gb["At"]; Bt_, Bt_b = Gb["Bt"]; Kt_, Kt_b = Gb["Kt"]; Rt_, Rt_b = Gb["Rt"]
                Bh_, Bh_b = Gb["Bh"]; Kh_, Kh_b = Gb["Kh"]; vb_, vb_b = Gb["vb"]
                yield
                C.op(DVE, lambda e: e.tensor_tensor_scan(out=flat(cs_), data0=flat(rmask), data1=flat(sg_), initial=0.0,
                                                         op0=ALU.mult, op1=ALU.add), [rmask_b, sg_b], [cs_b])
                if d == 0:
                    c2_, c2_b = cs_, cs_b
                    tot = v4(cs_)[:, :, 63:64].to_broadcast([64, NS, 64])
                else:
                    c2_, c2_b = G["cs2"]
                    _tt(C, DVE, c2_[:], sg_[:], cs_[:], ALU.subtract, [sg_b, cs_b], [c2_b])
                    _tt(C, DVE, v4(c2_), v4(c2_), v4(cs_)[:, :, 63:64].to_broadcast([64, NS, 64]), ALU.add, [c2_b, cs_b], [c2_b])
                    tot = v4(c2_)[:, :, 0:1].to_broadcast([64, NS, 64])
                _tt(C, DVE, v4(e2_), tot, v4(c2_), ALU.subtract, [c2_b], [e2_b])
                _tt(C, POOL, sg_[:], c2_[:], sg_[:], ALU.subtract, [c2_b, sg_b], [sg_b])
                yield
                _act(C, AF.Exp, Gi_[:], c2_[:], [c2_b], [Gi_b], scale=LAM)
                _act(C, AF.Exp, c2_[:], c2_[:], [c2_b, e2_b], [c2_b], scale=-LAM)
                Gm_, Gm_b = c2_, c2_b
                _act(C, AF.Exp, sg_[:], sg_[:], [sg_b], [sg_b], scale=-LAM)
                _act(C, AF.Exp, e2_[:], e2_[:], [e2_b], [e2_b], scale=-LAM)
                _cp(C, ACT, vb_[:], v_[:], [v_b], [vb_b])
                yield
                _tt(C, POOL, a_[:], kk_[:], a_[:], ALU.mult, [kk_b, a_b], [a_b])
                _stt(C, DVE, At_[:], kk_[:], -1.0, sg_[:], ALU.mult, ALU.mult, [kk_b, sg_b], [At_b])
                _tt(C, POOL, Bt_[:], a_[:], Gi_[:], ALU.mult, [a_b, Gi_b], [Bt_b])
                _tt(C, DVE, Kt_[:], kd_[:], Gi_[:], ALU.mult, [kd_b, Gi_b], [Kt_b])
                yield
                _tt(C, POOL, Rt_[:], r_[:], Gm_[:], ALU.mult, [r_b, Gm_b], [Rt_b])
                _tt(C, DVE, Bh_[:], a_[:], e2_[:], ALU.mult, [a_b, e2_b], [Bh_b])
                _tt(C, POOL, Kh_[:], kd_[:], e2_[:], ALU.mult, [kd_b, e2_b], [Kh_b])
                yield
                for ci in range(NCH):
                    cc = ci if d == 0 else NCH - 1 - ci
                    ts_ = slice(cc * 64, cc * 64 + 64)
                    Vt, Vt_b = Cb["Vt"]; Bht, Bht_b = Cb["Bht"]; Kht, Kht_b = Cb["Kht"]
                    for src, src_b, dst, dst_b in ((vb_, vb_b, Vt, Vt_b), (Bh_, Bh_b, Bht, Bht_b), (Kh_, Kh_b, Kht, Kht_b)):
                        p, p_b = ntps()
                        for h in range(8):
                            C.op(PE, lambda e, p=p, h=h, src=src, ts_=ts_: e.transpose(out=p[:, h, :], in_=src[:, h, ts_],
                                                                                       identity=idnb[:]), [src_b, idnb_b], [p_b])
                        _cp(C, ACT, dst[:], p[:], [p_b], [dst_b])
                        yield
                    P_, P_b = Cb["Pa"]; PT_, PT_b = Cb["PTa"]; TT, TT_b = Cb["TT"]
                    p, p_b = mm8(lambda h: At_[:, h, ts_], lambda h: Bt_[:, h, ts_], [At_b, Bt_b])
                    _tt(C, DVE, P_[:], p[:], mN, ALU.mult, [p_b, msk_b], [P_b])
                    yield
                    p, p_b = mm8(lambda h: Bt_[:, h, ts_], lambda h: At_[:, h, ts_], [At_b, Bt_b])
                    _tt(C, DVE, PT_[:], p[:], mNT, ALU.mult, [p_b, msk_b], [PT_b])
                    _tt(C, DVE, TT[:], PT_[:], idb, ALU.add, [PT_b, msk_b], [TT_b])
                    yield
                    AakT, AakT_b = Cb["AakT"]; ArbT, ArbT_b = Cb["ArbT"]; ArkT, ArkT_b = Cb["ArkT"]
                    p, p_b = mm8(lambda h: Kt_[:, h, ts_], lambda h: At_[:, h, ts_], [Kt_b, At_b])
                    _tt(C, DVE, AakT[:], p[:], mNT, ALU.mult, [p_b, msk_b], [AakT_b])
                    yield
                    p, p_b = mm8(lambda h: Bt_[:, h, ts_], lambda h: Rt_[:, h, ts_], [Bt_b, Rt_b])
                    _tt(C, DVE, ArbT[:], p[:], mI, ALU.mult, [p_b, msk_b], [ArbT_b])
                    yield
                    p, p_b = mm8(lambda h: Kt_[:, h, ts_], lambda h: Rt_[:, h, ts_], [Kt_b, Rt_b])
                    _tt(C, DVE, ArkT[:], p[:], mI, ALU.mult, [p_b, msk_b], [ArkT_b])
                    yield
                    cur = (P_, P_b, PT_, PT_b)
                    for rd in range(5):
                        Pc, Pc_b, PTc, PTc_b = cur
                        Pn, Pn_b = Cb["Pb"] if rd % 2 == 0 else Cb["Pa"]
                        PTn, PTn_b = Cb["PTb"] if rd % 2 == 0 else Cb["PTa"]
                        p, p_b = mm8(lambda h: PTc[:, h, :], lambda h: Pc[:, h, :], [Pc_b, PTc_b])
                        _cp(C, ACT, Pn[:], p[:], [p_b], [Pn_b])
                        yield
                        if rd < 4:
                            p, p_b = mm8(lambda h: Pc[:, h, :], lambda h: PTc[:, h, :], [Pc_b, PTc_b])
                            _cp(C, ACT, PTn[:], p[:], [p_b], [PTn_b])
                            yield
                        p, p_b = mm8(lambda h: Pn[:, h, :], lambda h: TT[:, h, :], [Pn_b, TT_b])
                        _tt(C, DVE, TT[:], p[:], TT[:], ALU.add, [p_b, TT_b], [TT_b])
                        yield
                        cur = (Pn, Pn_b, PTn, PTn_b)
                    WV, WV_b = Cf["WV"]; YV, YV_b = Cf["YV"]; ZV, ZV_b = Cf["ZV"]
                    p, p_b = mm8(lambda h: AakT[:, h, :], lambda h: Vt[:, h, :], [AakT_b, Vt_b])
                    _cp(C, ACT, WV[:], p[:], [p_b], [WV_b])
                    yield
                    p, p_b = mm8(lambda h: ArkT[:, h, :], lambda h: Vt[:, h, :], [ArkT_b, Vt_b])
                    _cp(C, ACT, YV[:], p[:], [p_b], [YV_b])
                    yield
                    p, p_b = mm8(lambda h: Kht[:, h, :], lambda h: Vt[:, h, :], [Kht_b, Vt_b])
                    _cp(C, ACT, ZV[:], p[:], [p_b], [ZV_b])
                    yield
                    W, W_b = Cb["W"]; U, U_b = Cb["U"]; Yo, Yo_b = Cf["Yo"]; Ht, Ht_b = Cf["Ht"]
                    p, p_b = mm8(lambda h: At_[:, h, ts_], lambda h: Hb[:, h, :], [At_b, Hb_b])
                    _tt(C, DVE, W[:], p[:], WV[:], ALU.add, [p_b, WV_b], [W_b])
                    yield
                    p, p_b = mm8(lambda h: TT[:, h, :], lambda h: W[:, h, :], [TT_b, W_b])
                    _cp(C, ACT, U[:], p[:], [p_b], [U_b])
                    yield
                    p, p_b = nps()
                    for h in range(8):
                        _mm(C, p[:, h, :], Rt_[:, h, ts_], Hb[:, h, :], [Rt_b, Hb_b], [p_b], start=True, stop=False)
                        _mm(C, p[:, h, :], ArbT[:, h, :], U[:, h, :], [ArbT_b, U_b], [p_b], start=False, stop=True)
                    _tt(C, DVE, Yo[:], p[:], YV[:], ALU.add, [p_b, YV_b], [Yo_b])
                    r0 = t0 + cc * 64
                    C.dma(SP, y_d[d][0][r0:r0 + 64, :], Yo[:].rearrange("p h i -> p (h i)"), [Yo_b], [y_d[d][1]])
                    yield
                    p, p_b = mm8(lambda h: Bht[:, h, :], lambda h: U[:, h, :], [Bht_b, U_b])
                    gidx = cc * 64 + (63 if d == 0 else 0)
                    gl = Gm_[:, :, gidx:gidx + 1].to_broadcast([64, 8, 64])
                    _tt(C, POOL, Ht[:], H[:], gl, ALU.mult, [H_b, Gm_b], [Ht_b])
                    _tt(C, POOL, Ht[:], Ht[:], ZV[:], ALU.add, [Ht_b, ZV_b], [Ht_b])
                    _tt(C, DVE, H[:], p[:], Ht[:], ALU.add, [p_b, Ht_b], [H_b])
                    _cp(C, ACT, Hb[:], H[:], [H_b], [Hb_b])
                    yield

        gens = [dir_gen(0), dir_gen(1)]
        while gens:
            for gq in list(gens):
                try:
                    next(gq)
                except StopIteration:
                    gens.remove(gq)
    C.P.barrier()


def stage_rwkv_out(C, y_d, RO, par_d, yaT):
    with ExitStack() as st:
        par, par_b = C.sb(st, [128, PAR_COLS], F32, "par")
        C.dma(SP, par[:], par_d[0], [par_d[1]], [par_b])
        identf, identf_b = C.sb(st, [128, 128], F32, "identf")
        C.op(POOL, lambda e: e.memset(identf[:], 0.0), [], [identf_b])
        C.op(POOL, lambda e: e.affine_select(out=identf[:], in_=identf[:], pattern=[[-1, 128]], compare_op=ALU.not_equal,
                                             fill=1.0, base=0, channel_multiplier=1), [identf_b], [identf_b])
        y0s = [C.sb(st, [128, 8, 64], F32, "y0") for _ in range(2)]
        y1s = [C.sb(st, [128, 8, 64], F32, "y1") for _ in range(2)]
        yhs = [C.sb(st, [128, 8, 64], F32, "yh") for _ in range(8)]
        sqt, sqt_b = C.sb(st, [128, 8, 64], F32, "sqt")
        sts = [C.sb(st, [128, 3, 8], F32, "st") for _ in range(2)]
        pts = [C.ps(st, [128, 512], F32, "pt") for _ in range(4)]
        bgs = [C.sb(st, [128, 2, 512], F32, "bg") for _ in range(2)]
        fms = [C.sb(st, [128, 512], F32, "fm") for _ in range(2)]
        obs = [C.sb(st, [128, 512], BF16, "ob") for _ in range(2)]
        it = 0
        oc = 0
        for g in range(NG):
            tsl = slice(g * 512, (g + 1) * 512)
            yh4 = []
            for s in range(4):
                y0, y0_b = y0s[it % 2]
                y1, y1_b = y1s[it % 2]
                sv, sv_b = sts[it % 2]
                yh, yh_b = yhs[it % 8]
                it += 1
                r0 = g * 512 + s * 128
                C.dma(SP, y0[:], y_d[0][0][r0:r0 + 128, :].rearrange("p (h i) -> p h i", i=64), [y_d[0][1]], [y0_b])
                C.dma(ACT, y1[:], y_d[1][0][r0:r0 + 128, :].rearrange("p (h i) -> p h i", i=64), [y_d[1][1]], [y1_b])
                _tt(C, DVE, y0[:], y0[:], y1[:], ALU.add, [y0_b, y1_b], [y0_b])
                C.op(DVE, lambda e, sv=sv, y0=y0: e.tensor_reduce(out=sv[:, 0, :], in_=y0[:], axis=AX.X, op=ALU.add), [y0_b], [sv_b])
                _ts(C, DVE, sv[:, 0, :], sv[:, 0, :], 1.0 / 64, None, ALU.mult, None, [sv_b], [sv_b])
                _tt(C, DVE, y0[:], y0[:], sv[:, 0, :].unsqueeze(2).to_broadcast([128, 8, 64]), ALU.subtract, [y0_b, sv_b], [y0_b])
                _tt(C, POOL, sqt[:], y0[:], y0[:], ALU.mult, [y0_b], [sqt_b])
                C.op(DVE, lambda e, sv=sv: e.tensor_reduce(out=sv[:, 1, :], in_=sqt[:], axis=AX.X, op=ALU.add), [sqt_b], [sv_b])
                _act(C, AF.Sqrt, sv[:, 2, :], sv[:, 1, :], [sv_b], [sv_b], scale=1.0 / 64, bias=64e-5)
                C.op(DVE, lambda e, sv=sv: e.reciprocal(out=sv[:, 2, :], in_=sv[:, 2, :]), [sv_b], [sv_b])
                _tt(C, DVE, yh[:], y0[:], sv[:, 2, :].unsqueeze(2).to_broadcast([128, 8, 64]), ALU.mult, [y0_b, sv_b], [yh_b])
                yh4.append((yh, yh_b))
            for c in range(4):
                pt, pt_b = pts[oc % 4]
                bg, bg_b = bgs[oc % 2]
                fm, fm_b = fms[oc % 2]
                ob, ob_b = obs[oc % 2]
                oc += 1
                C.dma(SP, bg[:, 0, :], RO["bonus"][0][c * 128:(c + 1) * 128, tsl], [RO["bonus"][1]], [bg_b])
                C.dma(ACT, bg[:, 1, :], RO["g"][0][c * 128:(c + 1) * 128, tsl], [RO["g"][1]], [bg_b])
                for s in range(4):
                    yh, yh_b = yh4[s]
                    C.op(PE, lambda e, pt=pt, yh=yh, s=s, c=c: e.transpose(
                        out=pt[:, s * 128:(s + 1) * 128], in_=yh[:].rearrange("p h i -> p (h i)")[:, c * 128:(c + 1) * 128],
                        identity=identf[:]), [yh_b, identf_b], [pt_b])
                _act(C, AF.Identity, fm[:], pt[:], [pt_b, par_b], [fm_b], scale=par[:, 62 + c:63 + c], bias=par[:, 66 + c:67 + c])
                _tt(C, DVE, fm[:], fm[:], bg[:, 0, :], ALU.add, [fm_b, bg_b], [fm_b])
                _tt(C, DVE, ob[:], fm[:], bg[:, 1, :], ALU.mult, [fm_b, bg_b], [ob_b])
                C.dma(POOL, yaT[0][c * 128:(c + 1) * 128, tsl], ob[:], [ob_b], [yaT[1]])
    C.P.barrier()


LSTN = 2304


def moe_consts():
    t = np.arange(S)
    inv = np.zeros((4, S), np.float32)
    for gi, w in enumerate((2, 4, 8, 16)):
        lo = w // 2
        hi = w - lo - 1
        start = np.clip(t - lo, 0, S)
        end = np.clip(t + hi + 1, 0, S)
        inv[gi] = 1.0 / (end - start).astype(np.float32)
    tri = (np.arange(128)[:, None] < np.arange(128)[None, :]).astype(np.float32)
    tb = np.zeros((128, 65), np.float32)
    tb[:, :64] = 128.0 * np.arange(64, dtype=np.float32)[None, :]
    tb[:, 64] = np.arange(128, dtype=np.float32)
    eoff = np.ascontiguousarray(np.broadcast_to((np.arange(16, dtype=np.float32) * LSTN)[None, :], (128, 16)))
    blk8 = np.kron(np.eye(16, dtype=np.float32), np.ones((8, 8), np.float32))
    sel8 = np.zeros((128, 16), np.float32)
    sel8[np.arange(16) * 8, np.arange(16)] = 1.0
    return {"invcnt": inv, "tri": tri, "tbase": tb, "eoff": eoff, "blk8": blk8, "sel8": sel8}


def load_cast(C, st, eng_cast, dst, dst_b, src_ap, src_b, stg, k0):
    t, t_b = stg[k0 % len(stg)]
    a, n = src_ap.shape[1], src_ap.shape[2]
    C.dma((SP, ACT)[k0 % 2], t[:, 0:a, 0:n], src_ap, [src_b], [t_b])
    _cp(C, eng_cast, dst, t[:, 0:a, 0:n], [t_b], [dst_b])


def stage_pool(C, ppool, pw_d, par_d, inv_d, ybT):
    with ExitStack() as st:
        W = S + 32
        par, par_b = C.sb(st, [128, PAR_COLS], F32, "par")
        C.dma(SP, par[:], par_d[0], [par_d[1]], [par_b])
        zp, zp_b = C.sb(st, [128, W], F32, "zp")
        sa, sa_b = C.sb(st, [128, W], F32, "sa")
        sb_, sb_b = C.sb(st, [128, W], F32, "sb")
        inv, inv_b = C.sb(st, [128, S], F32, "inv")
        pl, pl_b = C.sb(st, [128, S], BF16, "pl")
        pwf, pwf_b = C.sb(st, [128, 128], F32, "pwf")
        pw, pw_b = C.sb(st, [128, 128], BF16, "pw")
        pss = [C.ps(st, [128, 512], F32, "pps") for _ in range(2)]
        obs = [C.sb(st, [128, 512], BF16, "pob") for _ in range(2)]
        C.op(POOL, lambda e: e.memset(zp[:], 0.0), [], [zp_b])
        C.op(POOL, lambda e: e.memset(sa[:], 0.0), [], [sa_b])
        C.op(POOL, lambda e: e.memset(sb_[:], 0.0), [], [sb_b])
        k = 0
        for gi in range(4):
            C.dma(SP, zp[:, 16:16 + S], ppool[0][gi * 128:(gi + 1) * 128, :], [ppool[1]], [zp_b])
            C.dma(ACT, inv[:], inv_d[0][gi:gi + 1, :].partition_broadcast(128) if False else inv_d[0][gi].partition_broadcast(128),
                  [inv_d[1]], [inv_b])
            C.dma(SP, pwf[:], pw_d[0][gi], [pw_d[1]], [pwf_b])
            _cp(C, POOL, pw[:], pwf[:], [pwf_b], [pw_b])
            _tt(C, DVE, sa[:, 1:W], zp[:, 1:W], zp[:, 0:W - 1], ALU.add, [zp_b], [sa_b])
            cur, cur_b, oth, oth_b = sa, sa_b, sb_, sb_b
            sh = 1
            lo_, hi_ = 1, W
            for lvl in range(gi):
                lo_, hi_ = lo_ + sh, hi_ - sh
                _tt(C, DVE, oth[:, lo_:hi_], cur[:, lo_ + sh:hi_ + sh], cur[:, lo_ - sh:hi_ - sh], ALU.add, [cur_b], [oth_b])
                cur, cur_b, oth, oth_b = oth, oth_b, cur, cur_b
                sh *= 2
            _tt(C, DVE, oth[:, 16:16 + S], cur[:, 16:16 + S], inv[:], ALU.mult, [cur_b, inv_b], [oth_b])
            _tt(C, POOL, pl[:], oth[:, 16:16 + S], zp[:, 16:16 + S], ALU.subtract, [oth_b, zp_b], [pl_b])
            for g in range(NG):
                tsl = slice(g * 512, (g + 1) * 512)
                ps, ps_b = pss[k % 2]
                ob, ob_b = obs[k % 2]
                k += 1
                _mm(C, ps[:], pw[:], pl[:, tsl], [pw_b, pl_b], [ps_b])
                _act(C, AF.Copy, ob[:], ps[:], [ps_b, par_b], [ob_b], scale=par[:, 70 + gi:71 + gi])
                C.dma(SP, ybT[0][gi * 128:(gi + 1) * 128, tsl], ob[:], [ob_b], [ybT[1]])
    C.P.barrier()


def stage_merge(C, x_in, x_tok, yT, pgate, wbr_d, wout_d, nffn_d, router_d, h_tok, aff_tok, affT):
    with ExitStack() as st:
        stg = [C.sb(st, [128, 4, 1024], F32, "mstg") for _ in range(2)]
        Wb = [C.sb(st, [128, 4, 1024], BF16, "Wb") for _ in range(3)]
        Wo, Wo_b = C.sb(st, [128, 8, 1024], BF16, "Wo")
        k0 = 0
        for b in range(3):
            load_cast(C, st, ACT, Wb[b][0][:], Wb[b][1], wbr_d[b][0].rearrange("(kc p) n -> p kc n", p=128), wbr_d[b][1], stg, k0)
            k0 += 1
        for hf in range(2):
            load_cast(C, st, ACT, Wo[:, hf * 4:(hf + 1) * 4, :], Wo_b,
                      wout_d[0][hf * 512:(hf + 1) * 512, :].rearrange("(kc p) n -> p kc n", p=128), wout_d[1], stg, k0)
            k0 += 1
        gb, gb_b = C.sb(st, [128, D], F32, "gbf")
        C.dma(SP, gb[:], nffn_d[0].partition_broadcast(128), [nffn_d[1]], [gb_b])
        rt, rt_b = C.sb(st, [128, 8, 16], F32, "rt")
        C.dma(SP, rt[:], router_d[0].rearrange("(kc p) e -> p kc e", p=128), [router_d[1]], [rt_b])
        identf, identf_b = C.sb(st, [128, 128], F32, "identf")
        C.op(POOL, lambda e: e.memset(identf[:], 0.0), [], [identf_b])
        C.op(POOL, lambda e: e.affine_select(out=identf[:], in_=identf[:], pattern=[[-1, 128]], compare_op=ALU.not_equal,
                                             fill=1.0, base=0, channel_multiplier=1), [identf_b], [identf_b])
        ys = [C.sb(st, [128, 4, 512], BF16, "ys") for _ in range(3)]
        gt, gt_b = C.sb(st, [128, 24, 512], BF16, "gt")
        mg, mg_b = C.sb(st, [128, 8, 512], BF16, "mg")
        tmps = [C.sb(st, [128, 512], F32, "mt") for _ in range(3)]
        xts = [C.sb(st, [128, D], F32, "xt") for _ in range(2)]
        hf_, hf_b = C.sb(st, [128, D], F32, "hf")
        hb, hb_b = C.sb(st, [128, D], BF16, "hb")
        sq, sq_b = C.sb(st, [128, D], BF16, "sqj")
        hT, hT_b = C.sb(st, [128, 8, 128], F32, "hT32")
        sv, sv_b = C.sb(st, [128, 8], F32, "sv")
        lg, lg_b = C.sb(st, [128, 16], F32, "lg")
        af, af_b = C.sb(st, [128, 16], F32, "af")
        aT, aT_b = C.sb(st, [16, 128], F32, "aT")
        pm = [C.ps(st, [128, 512], F32, "pm") for _ in range(3)]
        px = [C.ps(st, [128, 512], F32, "px") for _ in range(2)]
        ptr, ptr_b = C.ps(st, [128, 8, 128], F32, "ptr")
        psm, psm_b = C.ps(st, [128, 512], F32, "psm")
        it = 0
        for g in range(NG):
            tsl = slice(g * 512, (g + 1) * 512)
            for b in range(3):
                C.dma((SP, ACT, SP)[b], ys[b][0][:], yT[b][0][:, tsl].rearrange("(kc p) t -> p kc t", p=128), [yT[b][1]], [ys[b][1]])
            C.dma(ACT, gt[:], pgate[0][:, tsl].rearrange("(c p) t -> p c t", p=128), [pgate[1]], [gt_b])
            for dc in range(8):
                for b in range(3):
                    ps, ps_b = pm[b]
                    for kc in range(4):
                        _mm(C, ps[:], Wb[b][0][:, kc, dc * 128:(dc + 1) * 128], ys[b][0][:, kc, :], [Wb[b][1], ys[b][1]], [ps_b],
                            start=(kc == 0), stop=(kc == 3))
                    _tt(C, DVE, tmps[b][0][:], ps[:], gt[:, b * 8 + dc, :], ALU.mult, [ps_b, gt_b], [tmps[b][1]])
                _tt(C, POOL, tmps[0][0][:], tmps[0][0][:], tmps[1][0][:], ALU.add, [tmps[0][1], tmps[1][1]], [tmps[0][1]])
                _tt(C, POOL, mg[:, dc, :], tmps[0][0][:], tmps[2][0][:], ALU.add, [tmps[0][1], tmps[2][1]], [mg_b])
            for s in range(4):
                xt, xt_b = xts[it % 2]
                it += 1
                r0 = g * 512 + s * 128
                C.dma(SP, xt[:], x_in[0][r0:r0 + 128, :], [x_in[1]], [xt_b])
                for half in range(2):
                    ps, ps_b = px[half]
                    for kc in range(8):
                        _mm(C, ps[:], mg[:, kc, s * 128:(s + 1) * 128], Wo[:, kc, half * 512:(half + 1) * 512], [mg_b, Wo_b], [ps_b],
                            start=(kc == 0), stop=(kc == 7))
                    _tt(C, DVE, xt[:, half * 512:(half + 1) * 512], ps[:], xt[:, half * 512:(half + 1) * 512], ALU.add,
                        [ps_b, xt_b], [xt_b])
                C.dma(ACT, x_tok[0][r0:r0 + 128, :], xt[:], [xt_b], [x_tok[1]])
                _act(C, AF.Square, sq[:], xt[:], [xt_b], [sq_b, sv_b], accum_out=sv[:, 0:1])
                _act(C, AF.Sqrt, sv[:, 1:2], sv[:, 0:1], [sv_b], [sv_b], scale=1.0 / D, bias=EPS)
                C.op(DVE, lambda e: e.reciprocal(out=sv[:, 1:2], in_=sv[:, 1:2]), [sv_b], [sv_b])
                _stt(C, DVE, hf_[:], xt[:], sv[:, 1:2], gb[:], ALU.mult, ALU.mult, [xt_b, sv_b, gb_b], [hf_b])
                _cp(C, POOL, hb[:], hf_[:], [hf_b], [hb_b])
                C.dma(SP, h_tok[0][r0:r0 + 128, :], hb[:], [hb_b], [h_tok[1]])
                for kc in range(8):
                    C.op(PE, lambda e, kc=kc: e.transpose(out=ptr[:, kc, :], in_=hf_[:, kc * 128:(kc + 1) * 128], identity=identf[:]),
                         [hf_b, identf_b], [ptr_b])
                _cp(C, ACT, hT[:], ptr[:], [ptr_b], [hT_b])
                for kc in range(8):
                    _mm(C, psm[:, 0:16], hT[:, kc, :], rt[:, kc, :], [hT_b, rt_b], [psm_b], start=(kc == 0), stop=(kc == 7))
                _cp(C, DVE, lg[:], psm[:, 0:16], [psm_b], [lg_b])
                C.op(DVE, lambda e: e.tensor_reduce(out=sv[:, 2:3], in_=lg[:], axis=AX.X, op=ALU.max), [lg_b], [sv_b])
                _ts(C, DVE, sv[:, 2:3], sv[:, 2:3], -1.0, None, ALU.mult, None, [sv_b], [sv_b])
                _act(C, AF.Exp, af[:], lg[:], [lg_b, sv_b], [af_b, sv_b], bias=sv[:, 2:3], accum_out=sv[:, 3:4])
                C.op(DVE, lambda e: e.reciprocal(out=sv[:, 4:5], in_=sv[:, 3:4]), [sv_b], [sv_b])
                _ts(C, DVE, af[:], af[:], sv[:, 4:5], None, ALU.mult, None, [af_b, sv_b], [af_b])
                C.dma(SP, aff_tok[0][r0:r0 + 128, :], af[:], [af_b], [aff_tok[1]])
                C.op(PE, lambda e: e.transpose(out=psm[0:16, 128:256], in_=af[:], identity=identf[:]), [af_b, identf_b], [psm_b])
                _cp(C, ACT, aT[:], psm[0:16, 128:256], [psm_b], [aT_b])
                C.dma(ACT, affT[0][:, r0:r0 + 128], aT[:], [aT_b], [affT[1]])
    C.P.barrier()


def stage_moe(C, x_tok, h_tok, aff_tok, affT, wg_d, wu_d, wd_d, mc, msk_d, lst_d):
    CAP = 1024
    NE = 16
    with ExitStack() as st:
        thrb, thrb_b = C.sb(st, [128, 16], F32, "thrb")
        with ExitStack() as s1:
            aT, aT_b = C.sb(s1, [128, 1024], F32, "aT")
            jk, jk_b = C.sb(s1, [128, 1024], F32, "jk")
            C.dma(SP, aT[:], affT[0].rearrange("e (s t) -> (e s) t", s=8), [affT[1]], [aT_b])
            sv, sv_b = C.sb(s1, [128, 8], F32, "bs")
            blk8, blk8_b = C.sb(s1, [128, 128], F32, "blk8")
            sel8, sel8_b = C.sb(s1, [128, 16], F32, "sel8")
            dg, dg_b = C.sb(s1, [128, 16], F32, "dg")
            on, on_b = C.sb(s1, [128, 128], F32, "on128")
            pcs = [C.ps(s1, [128, 2], F32, "pc") for _ in range(2)]
            pb, pb_b = C.ps(s1, [128, 16], F32, "pb")
            C.dma(ACT, blk8[:], mc["blk8"][0], [mc["blk8"][1]], [blk8_b])
            C.dma(ACT, sel8[:], mc["sel8"][0], [mc["sel8"][1]], [sel8_b])
            C.op(POOL, lambda e: e.memset(on[:], 1.0), [], [on_b])
            C.op(DVE, lambda e: e.memset(sv[:, 0:1], 0.0), [], [sv_b])
            C.op(DVE, lambda e: e.memset(sv[:, 1:2], 1.0), [sv_b], [sv_b])
            for itn in range(34):
                pc_, pc_b = pcs[itn % 2]
                _tt(C, DVE, sv[:, 2:3], sv[:, 0:1], sv[:, 1:2], ALU.add, [sv_b], [sv_b])
                _ts(C, DVE, sv[:, 2:3], sv[:, 2:3], 0.5, None, ALU.mult, None, [sv_b], [sv_b])
                _ts(C, DVE, jk[:], aT[:], sv[:, 2:3], None, ALU.is_ge, None, [aT_b, sv_b], [jk_b])
                C.op(DVE, lambda e: e.tensor_reduce(out=sv[:, 3:4], in_=jk[:], axis=AX.X, op=ALU.add), [jk_b], [sv_b])
                _mm(C, pc_[:, 0:1], blk8[:], sv[:, 3:4], [blk8_b, sv_b], [pc_b])
                _ts(C, DVE, sv[:, 4:5], pc_[:, 0:1], CAP - 0.5, None, ALU.is_ge, None, [pc_b], [sv_b])
                _tt(C, DVE, sv[:, 5:6], sv[:, 2:3], sv[:, 0:1], ALU.subtract, [sv_b], [sv_b])
                _tt(C, DVE, sv[:, 6:7], sv[:, 1:2], sv[:, 2:3], ALU.subtract, [sv_b], [sv_b])
                _stt(C, DVE, sv[:, 0:1], sv[:, 5:6], sv[:, 4:5], sv[:, 0:1], ALU.mult, ALU.add, [sv_b], [sv_b])
                _stt(C, DVE, sv[:, 1:2], sv[:, 6:7], sv[:, 4:5], sv[:, 2:3], ALU.mult, ALU.add, [sv_b], [sv_b])
            _ts(C, DVE, dg[:], sel8[:], sv[:, 0:1], None, ALU.mult, None, [sel8_b, sv_b], [dg_b])
            _mm(C, pb[:], on[:], dg[:], [on_b, dg_b], [pb_b])
            _cp(C, DVE, thrb[:], pb[:], [pb_b], [thrb_b])
        C.P.barrier()
        af, af_b = C.sb(st, [128, 64, 16], F32, "af")
        C.dma(SP, af[:], aff_tok[0].rearrange("(i p) e -> p i e", p=128), [aff_tok[1]], [af_b])
        posi, posi_b = C.sb(st, [128, 64, 16], I32, "posi")
        srcf, src_b = C.sb(st, [128, 64 * 16 * 2 + 16], F32, "src")
        src = srcf[:, 0:2048].rearrange("p (i e t) -> p i e t", e=16, t=2)
        C.op(POOL, lambda e: e.memset(srcf[:, 2048:2064], 0.0), [], [src_b])
        identb, identb_b = C.sb(st, [128, 128], BF16, "identb")
        C.op(POOL, lambda e: e.memset(identb[:], 0.0), [], [identb_b])
        C.op(POOL, lambda e: e.affine_select(out=identb[:], in_=identb[:], pattern=[[-1, 128]], compare_op=ALU.not_equal,
                                             fill=1.0, base=0, channel_multiplier=1), [identb_b], [identb_b])
        with ExitStack() as s2:
            mk, mk_b = C.sb(s2, [128, 64, 16], F32, "mk")
            posm, posm_b = C.sb(s2, [128, 64, 16], F32, "posm")
            tri, tri_b = C.sb(s2, [128, 128], F32, "tri")
            ones, ones_b = C.sb(s2, [128, 128], F32, "ones")
            tb, tb_b = C.sb(s2, [128, 66], F32, "tb")
            eo, eo_b = C.sb(s2, [128, 16], F32, "eo")
            totT, totT_b = C.sb(s2, [128, 16, 64], F32, "totT")
            inc, inc_b = C.sb(s2, [128, 16, 64], F32, "inc")
            rm2, rm2_b = C.sb(s2, [128, 16, 64], F32, "rm2")
            pw_ = [C.ps(s2, [128, 512], F32, "pw") for _ in range(2)]
            pt_ = [C.ps(s2, [128, 512], F32, "ptt") for _ in range(2)]
            C.dma(SP, tri[:], mc["tri"][0], [mc["tri"][1]], [tri_b])
            C.dma(SP, tb[:, 0:65], mc["tbase"][0], [mc["tbase"][1]], [tb_b])
            C.dma(SP, eo[:], mc["eoff"][0], [mc["eoff"][1]], [eo_b])
            _ts(C, DVE, tb[:, 65:66], tb[:, 64:65], 2048.0, None, ALU.add, None, [tb_b], [tb_b])
            C.op(POOL, lambda e: e.memset(ones[:], 1.0), [], [ones_b])
            C.op(POOL, lambda e: e.memset(rm2[:], 1.0), [], [rm2_b])
            C.op(POOL, lambda e: e.memset(rm2[:, :, 0:1], 0.0), [rm2_b], [rm2_b])
            _tt(C, DVE, mk[:], af[:], thrb[:].unsqueeze(1).to_broadcast([128, 64, 16]), ALU.is_ge, [af_b, thrb_b], [mk_b])
            mkf = mk[:].rearrange("p i e -> p (i e)")
            for half in range(2):
                _mm(C, pw_[half][0][:], tri[:], mkf[:, half * 512:(half + 1) * 512], [tri_b, mk_b], [pw_[half][1]])
                _mm(C, pt_[half][0][:], ones[:], mkf[:, half * 512:(half + 1) * 512], [ones_b, mk_b], [pt_[half][1]])
                _cp(C, DVE, totT[:].rearrange("p e i -> p i e")[:, half * 32:(half + 1) * 32, :],
                    pt_[half][0][:].rearrange("p (i e) -> p i e", e=16), [pt_[half][1]], [totT_b])
            C.op(DVE, lambda e: e.tensor_tensor_scan(out=inc[:].rearrange("p e i -> p (e i)"), data0=rm2[:].rearrange("p e i -> p (e i)"),
                                                     data1=totT[:].rearrange("p e i -> p (e i)"), initial=0.0, op0=ALU.mult, op1=ALU.add),
                 [rm2_b, totT_b], [inc_b])
            _tt(C, DVE, inc[:], inc[:], totT[:], ALU.subtract, [inc_b, totT_b], [inc_b])
            for half in range(2):
                _tt(C, DVE, posm[:, half * 32:(half + 1) * 32, :], pw_[half][0][:].rearrange("p (i e) -> p i e", e=16),
                    inc[:].rearrange("p e i -> p i e")[:, half * 32:(half + 1) * 32, :], ALU.add, [pw_[half][1], inc_b], [posm_b])
            _ts(C, DVE, posm[:], posm[:], tb[:, 65:66], None, ALU.subtract, None, [posm_b, tb_b], [posm_b])
            _tt(C, DVE, posm[:], posm[:], mk[:], ALU.mult, [posm_b, mk_b], [posm_b])
            _ts(C, DVE, posm[:], posm[:], tb[:, 65:66], None, ALU.add, None, [posm_b, tb_b], [posm_b])
            _tt(C, DVE, posm[:], posm[:], eo[:].unsqueeze(1).to_broadcast([128, 64, 16]), ALU.add, [posm_b, eo_b], [posm_b])
            _cp(C, DVE, posi[:], posm[:], [posm_b], [posi_b])
            _ts(C, POOL, src[:, :, :, 0], tb[:, 0:64].unsqueeze(2).to_broadcast([128, 64, 16]), tb[:, 64:65], None, ALU.add, None,
                [tb_b], [src_b])
            _cp(C, POOL, src[:, :, :, 1], af[:], [af_b], [src_b])
        C.P.barrier()
        sc_bufs = [[Buf("sc") for _ in range(64)] for _ in range(NE)]

        def scatter(e_):
            for i in range(64):
                _idma(C, lambda e, e_=e_, i=i: e.indirect_dma_start(
                    out=lst_d[0], out_offset=bass.IndirectOffsetOnAxis(ap=posi[:, i, e_:e_ + 1], axis=0),
                    in_=srcf[:, (i * 16 + e_) * 2:(i * 16 + e_) * 2 + 16], in_offset=None), [posi_b, src_b], [sc_bufs[e_][i]])
        civs = [C.sb(st, [128, 8, 2], F32, "civ") for _ in range(2)]
        idxs = [C.sb(st, [128, 8], I32, "idx") for _ in range(2)]
        xg, xg_b = C.sb(st, [128, 8, 1024], BF16, "xg")
        xgT, xgT_b = C.sb(st, [128, 8, 1024], BF16, "xgT")
        hid, hid_b = C.sb(st, [128, 8, 1024], BF16, "hid")
        Wgs = [C.sb(st, [128, 8, 1024], BF16, "Wg") for _ in range(1)]
        Wus = [C.sb(st, [128, 8, 1024], BF16, "Wu") for _ in range(1)]
        Wds = [C.sb(st, [128, 8, 1024], BF16, "Wd") for _ in range(1)]
        stg = [C.sb(st, [128, 4, 1024], F32, "estg") for _ in range(3)]
        sgs = [C.sb(st, [128, 512], F32, "sg") for _ in range(2)]
        yvs = [C.sb(st, [128, 1024], F32, "yv") for _ in range(4)]
        pxt, pxt_b = C.ps(st, [128, 8, 128], BF16, "pxt")
        pgs = [C.ps(st, [128, 512], F32, "pg") for _ in range(2)]
        pus = [C.ps(st, [128, 512], F32, "pu") for _ in range(2)]
        pys = [C.ps(st, [128, 512], F32, "py") for _ in range(2)]
        kk = [0, 0]

        def load_w(W_, W_b, srcw, e_):
            for hf in range(2):
                t, t_b = stg[kk[0] % 3]
                C.dma((SP, ACT)[kk[0] % 2], t[:], srcw[0][e_, hf * 512:(hf + 1) * 512, :].rearrange("(kc p) n -> p kc n", p=128),
                      [srcw[1]], [t_b])
                _cp(C, ACT, W_[:, hf * 4:(hf + 1) * 4, :], t[:], [t_b], [W_b])
                kk[0] += 1

        def gather(e_):
            civ, civ_b = civs[e_ % 2]
            idx, idx_b = idxs[e_ % 2]
            C.dma(SP, civ[:], lst_d[0][e_ * LSTN:e_ * LSTN + 1024, 0:2].rearrange("(cb p) two -> p cb two", p=128),
                  sc_bufs[e_], [civ_b])
            _cp(C, DVE, idx[:], civ[:, :, 0], [civ_b], [idx_b])
            for cb in range(8):
                _idma(C, lambda e, cb=cb, idx=idx: e.indirect_dma_start(
                    out=xg[:, cb, :], out_offset=None, in_=h_tok[0],
                    in_offset=bass.IndirectOffsetOnAxis(ap=idx[:, cb:cb + 1], axis=0)), [idx_b, h_tok[1]], [xg_b])

        def transposes():
            for cb in range(8):
                for kc in range(8):
                    C.op(PE, lambda e, cb=cb, kc=kc: e.transpose(out=pxt[:, kc, :], in_=xg[:, cb, kc * 128:(kc + 1) * 128],
                                                                 identity=identb[:]), [xg_b, identb_b], [pxt_b])
                _cp(C, (ACT, DVE)[cb % 2], xgT[:, :, cb * 128:(cb + 1) * 128], pxt[:], [pxt_b], [xgT_b])

        load_w(*Wgs[0], wg_d, 0)
        load_w(*Wus[0], wu_d, 0)
        load_w(*Wds[0], wd_d, 0)
        scatter(0)
        scatter(1)
        scatter(2)
        gather(0)
        for e_ in range(NE):
            civ, civ_b = civs[e_ % 2]
            idx, idx_b = idxs[e_ % 2]
            Wg, Wg_b = Wgs[0]
            Wu, Wu_b = Wus[0]
            Wd, Wd_b = Wds[0]
            transposes()
            if e_ + 1 < NE:
                gather(e_ + 1)
            for fc in range(8):
                for half in range(2):
                    hsl = slice(half * 512, (half + 1) * 512)
                    pg, pg_b = pgs[kk[1] % 2]
                    pu, pu_b = pus[kk[1] % 2]
                    sg, sg_b = sgs[kk[1] % 2]
                    kk[1] += 1
                    for kc in range(8):
                        _mm(C, pg[:], Wg[:, kc, fc * 128:(fc + 1) * 128], xgT[:, kc, hsl], [Wg_b, xgT_b], [pg_b], start=(kc == 0), stop=(kc == 7))
                    for kc in range(8):
                        _mm(C, pu[:], Wu[:, kc, fc * 128:(fc + 1) * 128], xgT[:, kc, hsl], [Wu_b, xgT_b], [pu_b], start=(kc == 0), stop=(kc == 7))
                    _act(C, AF.Silu, sg[:], pg[:], [pg_b], [sg_b])
                    _tt(C, DVE, hid[:, fc, hsl], pu[:], sg[:], ALU.mult, [pu_b, sg_b], [hid_b])
            if e_ + 1 < NE:
                load_w(Wg, Wg_b, wg_d, e_ + 1)
                load_w(Wu, Wu_b, wu_d, e_ + 1)
            for cb in range(8):
                yv, yv_b = yvs[cb % 4]
                for half in range(2):
                    py, py_b = pys[half]
                    for fc in range(8):
                        _mm(C, py[:], hid[:, fc, cb * 128:(cb + 1) * 128], Wd[:, fc, half * 512:(half + 1) * 512], [hid_b, Wd_b], [py_b],
                            start=(fc == 0), stop=(fc == 7))
                    _ts(C, DVE, yv[:, half * 512:(half + 1) * 512], py[:], civ[:, cb, 1:2], None, ALU.mult, None,
                        [py_b, civ_b], [yv_b])
                _idma(C, lambda e, cb=cb, yv=yv, idx=idx: e.indirect_dma_start(
                    out=x_tok[0], out_offset=bass.IndirectOffsetOnAxis(ap=idx[:, cb:cb + 1], axis=0), in_=yv[:], in_offset=None,
                    compute_op=ALU.add), [idx_b, yv_b, x_tok[1]], [x_tok[1]])
            if e_ + 1 < NE:
                load_w(Wd, Wd_b, wd_d, e_ + 1)
            if e_ + 3 < NE:
                scatter(e_ + 3)
    C.P.barrier()


def _idma(C, fn, R, W):
    return C.P.add(POOL, fn, R, W, dma=True)


def stage_final(C, x_tok, nf_d, out_d):
    with ExitStack() as st:
        gb, gb_b = C.sb(st, [128, D], F32, "gbf")
        C.dma(SP, gb[:], nf_d[0].partition_broadcast(128), [nf_d[1]], [gb_b])
        xts = [C.sb(st, [128, D], F32, "xt") for _ in range(3)]
        sq, sq_b = C.sb(st, [128, D], BF16, "sqj")
        svs = [C.sb(st, [128, 2], F32, "sv") for _ in range(3)]
        for i in range(NT):
            xt, xt_b = xts[i % 3]
            sv, sv_b = svs[i % 3]
            C.dma(SP, xt[:], x_tok[0][i * 128:(i + 1) * 128, :], [x_tok[1]], [xt_b])
            _act(C, AF.Square, sq[:], xt[:], [xt_b], [sq_b, sv_b], accum_out=sv[:, 0:1])
            _act(C, AF.Sqrt, sv[:, 1:2], sv[:, 0:1], [sv_b], [sv_b], scale=1.0 / D, bias=EPS)
            C.op(DVE, lambda e, sv=sv: e.reciprocal(out=sv[:, 1:2], in_=sv[:, 1:2]), [sv_b], [sv_b])
            _stt(C, DVE, xt[:], xt[:], sv[:, 1:2], gb[:], ALU.mult, ALU.mult, [xt_b, sv_b, gb_b], [xt_b])
            C.dma(ACT, out_d[0][i * 128:(i + 1) * 128, :], xt[:], [xt_b], [out_d[1]])


W_NAMES = ("norm_mix", "w_in", "rwkv_w2", "rwkv_a2", "rwkv_g2", "vres_w1", "vres_w2", "pool_w", "q_norm", "k_norm",
           "w_branch_rwkv", "w_branch_pool", "w_branch_attn", "w_out", "norm_ffn", "router", "exp_gate", "exp_up", "exp_down",
           "norm_final")
W_SHAPES = {"norm_mix": [2, D], "w_in": [2, D, NPROJ], "rwkv_w2": [2, 2, 64, 512], "rwkv_a2": [2, 2, 64, 512],
            "rwkv_g2": [2, 128, 512], "vres_w1": [1, D, 32], "vres_w2": [1, 32, 512], "pool_w": [2, 4, 128, 128],
            "q_norm": [2, 64], "k_norm": [2, 64], "w_branch_rwkv": [2, 512, D], "w_branch_pool": [2, 512, D],
            "w_branch_attn": [2, 512, D], "w_out": [2, D, D], "norm_ffn": [2, D], "router": [2, D, 16],
            "exp_gate": [2, 16, D, D], "exp_up": [2, 16, D, D], "exp_down": [2, 16, D, D], "norm_final": [D]}


def build_full(nlayers=2, do_final=True):
    nc = bass.Bass("TRN2", target_bir_lowering=False)
    C = Ctx(nc)
    x = C.dram("x", [S, D], F32, kind="ExternalInput")
    Wd = {n: C.dram(n, W_SHAPES[n], F32, kind="ExternalInput") for n in W_NAMES}
    pars = [C.dram(f"par{l}", [128, PAR_COLS], F32, kind="ExternalInput") for l in range(2)]
    hc = host_consts()
    mcn = moe_consts()
    cst = {k: C.dram(k, list(v.shape), F32, kind="ExternalInput") for k, v in hc.items()}
    mc = {k: C.dram(k, list(v.shape), F32, kind="ExternalInput") for k, v in mcn.items()}
    msk = C.dram("msk", [64, 5, 64], F32, kind="ExternalInput")
    out = C.dram("out", [S, D], F32, kind="ExternalOutput")
    x_tok = C.dram("x_tok", [S, D], F32)
    outs = {"prw": C.dram("prw", [1920, S], F32), "ppool": C.dram("ppool", [512, S], F32), "patt": C.dram("patt", [768, S], F32),
            "pgate": C.dram("pgate", [3072, S], BF16), "hv1": C.dram("hv1", [32, S], F32)}
    RO = {n: C.dram("o_" + n, [512, S], F32) for n in RNAMES + ("vfirst",)}
    y_d = [C.dram(f"ydir{d}", [S, 512], F32) for d in range(2)]
    yT = [C.dram(n, [512, S], BF16) for n in ("yaT", "ybT", "ycT")]
    h_tok = C.dram("h_tok", [S, D], BF16)
    aff_tok = C.dram("aff_tok", [S, 16], F32)
    affT = C.dram("affT", [16, S], F32)
    lst_d = C.dram("ranklist", [16 * LSTN, 16], F32)

    def sub(d, *idx):
        ap = d[0]
        for i in idx:
            ap = ap[i]
        return (ap, d[1])

    for l in range(nlayers):
        x_src = x if l == 0 else x_tok
        stage_proj(C, l, x_src, sub(Wd["w_in"], l), sub(Wd["norm_mix"], l), outs, sub(Wd["vres_w1"], 0) if l == 1 else None)
        C.P.barrier()
        vres = None
        if l == 1:
            vres = {"w2": sub(Wd["vres_w2"], 0), "hv1": outs["hv1"], "vfirst": RO["vfirst"]}
        stage_rwkv_prep(C, l, outs["prw"], pars[l], sub(Wd["rwkv_w2"], l), sub(Wd["rwkv_a2"], l), sub(Wd["rwkv_g2"], l),
                        cst["blk"], RO, vres)
        stage_rwkv_scan(C, RO, msk, y_d)
        stage_rwkv_out(C, y_d, RO, pars[l], yT[0])
        stage_pool(C, outs["ppool"], sub(Wd["pool_w"], l), pars[l], mc["invcnt"], yT[1])
        stage_attn(C, outs["patt"], sub(Wd["q_norm"], l), sub(Wd["k_norm"], l), cst, yT[2])
        stage_merge(C, x_src, x_tok, yT, outs["pgate"],
                    [sub(Wd["w_branch_rwkv"], l), sub(Wd["w_branch_pool"], l), sub(Wd["w_branch_attn"], l)],
                    sub(Wd["w_out"], l), sub(Wd["norm_ffn"], l), sub(Wd["router"], l), h_tok, aff_tok, affT)
        stage_moe(C, x_tok, h_tok, aff_tok, affT, sub(Wd["exp_gate"], l), sub(Wd["exp_up"], l), sub(Wd["exp_down"], l), mc, msk, lst_d)
    if do_final:
        stage_final(C, x_tok, Wd["norm_final"], out)
    emit_program(C)
    consts = dict(hc)
    consts.update(mcn)
    consts["msk"] = scan_consts()
    return nc, consts


def make_in_maps(inputs, cores):
    inp = {k: np.asarray(v) for k, v in inputs.items()}
    shared = {n: np.ascontiguousarray(inp[n], dtype=np.float32) for n in W_NAMES}
    shared["par0"] = pack_par(inp, 0)
    shared["par1"] = pack_par(inp, 1)
    maps = []
    for b in cores:
        m = dict(shared)
        m["x"] = np.ascontiguousarray(inp["x"][b], dtype=np.float32)
        maps.append(m)
    return maps


def kernel(**inputs):
    nc, consts = build_full()
    maps = make_in_maps(inputs, list(range(8)))
    for m in maps:
        m.update(consts)
    res = run_bass_kernel_spmd(nc, maps, core_ids=list(range(8)))
    return np.stack([np.asarray(r["out"], dtype=np.float32) for r in res.results], axis=0)
```
